# Optimizing a Trainium2 kernel written in Bass

```python
import jax, jax.numpy as jnp
from jax import lax
import numpy as np

D_MODEL = 1024
BATCH = 8
SEQ = 4096
DEPTH = 2

GRID_W = 64
CTX_LEN = 256
N_EVEN = (DEPTH + 1) // 2
N_ODD = DEPTH // 2
EPS = 1e-6

SGU_GROUPS = 4
SGU_GROUP_DIM = 128
SGU_DIM = SGU_GROUPS * SGU_GROUP_DIM
SGU_CHUNK = 128
HGRN_HEADS = 4
HGRN_KDIM = 128
HGRN_VDIM = 128
HGRN_DIM = HGRN_HEADS * HGRN_KDIM
HGRN_CHUNK = 64
AB_IN = 2 * SGU_DIM + 5 * HGRN_DIM
AB_OUT = SGU_DIM + HGRN_DIM
ATTN_HEADS = 16
ATTN_KV_HEADS = 4
ATTN_GROUP = ATTN_HEADS // ATTN_KV_HEADS
HEAD_DIM = 64
ATTN_Q_DIM = ATTN_HEADS * HEAD_DIM
QKV_DIM = (ATTN_HEADS + 2 * ATTN_KV_HEADS) * HEAD_DIM
WINDOW = 128
ATTN_BLOCK = 128
ROPE_BASE = 10000.0
N_EXPERTS = 16
CAPACITY_FACTOR = 2
EXPERT_FF = 2048

kernel_name = "hybrid_sgu_hgrn2_swa_ec_moe_diffusion"


def _rmsnorm(x, g):
    xf = x.astype(jnp.float32)
    y = xf * lax.rsqrt(jnp.mean(xf * xf, axis=-1, keepdims=True) + EPS)
    return (y * g.astype(jnp.float32)).astype(x.dtype)


def _modulate(x, g, shift, scale):
    return _rmsnorm(x, g) * (1 + scale) + shift


def _chunk_sgu(zu, zv, w_s, b_s):
    B, N, _ = zu.shape
    nc = N // SGU_CHUNK
    u = jax.nn.gelu(zu, approximate=False)
    v = jax.nn.gelu(zv, approximate=False).reshape(B, nc, SGU_CHUNK, SGU_GROUPS, SGU_GROUP_DIM)
    vf = v.astype(jnp.float32)
    mu = jnp.mean(vf, axis=-1, keepdims=True)
    var = jnp.mean(jnp.square(vf - mu), axis=-1, keepdims=True)
    vn = ((vf - mu) * lax.rsqrt(var + EPS)).astype(zv.dtype)
    s = jnp.einsum('gts,bnsgc->bntgc', w_s, vn) + b_s.T[:, :, None]
    return u * s.reshape(B, N, SGU_DIM)


def _gla_scan(q, k, v, log_f, s0):
    B, H, N, K = q.shape
    nc = N // HGRN_CHUNK

    def to_chunks(a):
        return jnp.moveaxis(a.reshape(B, H, nc, HGRN_CHUNK, a.shape[-1]), 2, 0)

    tri = jnp.tril(jnp.ones((HGRN_CHUNK, HGRN_CHUNK), dtype=bool))[:, :, None]

    def step(S, inp):
        qc, kc, vc, gc = inp
        b = jnp.cumsum(gc, axis=2)
        o_inter = jnp.einsum('bhtk,bhkv->bhtv', qc * jnp.exp(b), S)
        rel = jnp.exp(jnp.where(tri, b[:, :, :, None, :] - b[:, :, None, :, :], -jnp.inf))
        a = jnp.einsum('bhtk,bhsk,bhtsk->bhts', qc, kc, rel)
        o_intra = jnp.einsum('bhts,bhsv->bhtv', a, vc)
        b_end = b[:, :, -1:, :]
        S = jnp.exp(b_end[:, :, 0, :, None]) * S + jnp.einsum('bhsk,bhsv->bhkv', kc * jnp.exp(b_end - b), vc)
        return S, o_inter + o_intra

    s_fin, o = lax.scan(step, s0, (to_chunks(q), to_chunks(k), to_chunks(v), to_chunks(log_f)))
    return jnp.moveaxis(o, 0, 2).reshape(B, H, N, v.shape[-1]), s_fin


def _hgrn2(zq, zf_fwd, zf_bwd, zi, lb, s0):
    B, N, _ = zq.shape

    def heads(a):
        return a.astype(jnp.float32).reshape(B, N, HGRN_HEADS, -1).transpose(0, 2, 1, 3)

    q = heads(jax.nn.silu(zq))
    v = heads(zi)
    outs, finals = [], []
    for d, zf in enumerate((zf_fwd, zf_bwd)):
        f = heads(lb[d] + (1.0 - lb[d]) * jax.nn.sigmoid(zf.astype(jnp.float32)))
        args = (q, 1.0 - f, v, jnp.log(f))
        if d == 1:
            args = tuple(jnp.flip(a, axis=2) for a in args)
        o, s_fin = _gla_scan(*args, s0[d])
        outs.append(jnp.flip(o, axis=2) if d == 1 else o)
        finals.append(s_fin)
    return outs[0] + outs[1], jnp.stack(finals)


def _even_mixer(h, w_in, w_out, sgu_w, sgu_b, lb, norm_g, s0):
    B, N, _ = h.shape
    z = h @ w_in
    cuts = [SGU_DIM, 2 * SGU_DIM] + [2 * SGU_DIM + i * HGRN_DIM for i in range(1, 5)]
    zu, zv, zq, zf_fwd, zf_bwd, zi, zg = jnp.split(z, cuts, axis=-1)
    y_a = _chunk_sgu(zu, zv, sgu_w, sgu_b)
    o, states = _hgrn2(zq, zf_fwd, zf_bwd, zi, lb, s0)
    o = o * lax.rsqrt(jnp.mean(o * o, axis=-1, keepdims=True) + EPS) * norm_g.astype(jnp.float32)[:, None, :]
    y_b = o.transpose(0, 2, 1, 3).reshape(B, N, HGRN_DIM).astype(h.dtype) * jax.nn.silu(zg)
    return jnp.concatenate([y_a, y_b], axis=-1) @ w_out, states


def _rope_2d(x, row, col):
    half = HEAD_DIM // 2
    quarter = half // 2
    inv = ROPE_BASE ** (-jnp.arange(quarter, dtype=jnp.float32) / quarter)

    def rot(xa, pos):
        ang = pos.astype(jnp.float32)[:, None] * inv[None, :]
        cos, sin = jnp.cos(ang), jnp.sin(ang)
        x1, x2 = xa[..., :quarter], xa[..., quarter:]
        return jnp.concatenate([x1 * cos - x2 * sin, x2 * cos + x1 * sin], axis=-1)

    xf = x.astype(jnp.float32)
    return jnp.concatenate([rot(xf[..., :half], row), rot(xf[..., half:], col)], axis=-1).astype(x.dtype)


def _sink_softmax(scores, sink):
    s_sink = jnp.broadcast_to(sink.astype(jnp.float32), scores.shape[:-1] + (1,))
    return jax.nn.softmax(jnp.concatenate([scores, s_sink], axis=-1), axis=-1)[..., :-1]


def _window_attention(h_lat, h_ctx, w_qkv, w_out, sink, need_ctx):
    B, N, _ = h_lat.shape
    Lc = h_ctx.shape[1]
    scale = HEAD_DIM ** -0.5
    sink_b = sink.reshape(ATTN_KV_HEADS, ATTN_GROUP, 1, 1)

    def split_q(zq, L):
        return zq.reshape(B, L, ATTN_KV_HEADS, ATTN_GROUP, HEAD_DIM).transpose(0, 2, 3, 1, 4)

    def split_kv(zkv, L):
        k, v = jnp.split(zkv, 2, axis=-1)
        k = k.reshape(B, L, ATTN_KV_HEADS, HEAD_DIM).transpose(0, 2, 1, 3)
        v = v.reshape(B, L, ATTN_KV_HEADS, HEAD_DIM).transpose(0, 2, 1, 3)
        return k, v

    kc, vc = split_kv(h_ctx @ w_qkv[:, ATTN_Q_DIM:], Lc)
    z = h_lat @ w_qkv
    q = split_q(z[..., :ATTN_Q_DIM], N)
    k, v = split_kv(z[..., ATTN_Q_DIM:], N)
    t = jnp.arange(N)
    row, col = t // GRID_W, t % GRID_W
    q = _rope_2d(q, row, col)
    k = _rope_2d(k, row, col)

    nb = N // ATTN_BLOCK
    pad = [(0, 0), (0, 0), (ATTN_BLOCK, ATTN_BLOCK), (0, 0)]
    kp, vp = jnp.pad(k, pad), jnp.pad(v, pad)
    r_idx = jnp.arange(ATTN_BLOCK)[:, None]
    s_idx = jnp.arange(3 * ATTN_BLOCK)[None, :]
    band = jnp.abs(s_idx - r_idx - ATTN_BLOCK) <= WINDOW

    def block(bi):
        start = bi * ATTN_BLOCK
        qb = lax.dynamic_slice_in_dim(q, start, ATTN_BLOCK, axis=3)
        kb = lax.dynamic_slice_in_dim(kp, start, 3 * ATTN_BLOCK, axis=2)
        vb = lax.dynamic_slice_in_dim(vp, start, 3 * ATTN_BLOCK, axis=2)
        j = start - ATTN_BLOCK + s_idx
        valid = band & (j >= 0) & (j < N)
        s_loc = jnp.einsum('bkgqd,bksd->bkgqs', qb, kb).astype(jnp.float32) * scale
        s_loc = jnp.where(valid, s_loc, -jnp.inf)
        s_ctx = jnp.einsum('bkgqd,bksd->bkgqs', qb, kc).astype(jnp.float32) * scale
        p = _sink_softmax(jnp.concatenate([s_loc, s_ctx], axis=-1), sink_b)
        p_loc = p[..., :3 * ATTN_BLOCK].astype(v.dtype)
        p_ctx = p[..., 3 * ATTN_BLOCK:].astype(v.dtype)
        return jnp.einsum('bkgqs,bksd->bkgqd', p_loc, vb) + jnp.einsum('bkgqs,bksd->bkgqd', p_ctx, vc)

    o = lax.map(block, jnp.arange(nb))
    y_lat = o.transpose(1, 0, 4, 2, 3, 5).reshape(B, N, ATTN_Q_DIM) @ w_out

    y_ctx = None
    if need_ctx:
        qc = split_q(h_ctx @ w_qkv[:, :ATTN_Q_DIM], Lc)
        sc = jnp.einsum('bkgqd,bksd->bkgqs', qc, kc).astype(jnp.float32) * scale
        pc = _sink_softmax(sc, sink_b).astype(vc.dtype)
        oc = jnp.einsum('bkgqs,bksd->bkgqd', pc, vc)
        y_ctx = oc.transpose(0, 3, 1, 2, 4).reshape(B, Lc, ATTN_Q_DIM) @ w_out
    return y_lat, y_ctx


def _expert_choice_ffn(h, w_r, w1, w3, w2):
    B, N, D = h.shape
    cap = CAPACITY_FACTOR * N // N_EXPERTS
    aff = jax.nn.softmax((h @ w_r).astype(jnp.float32), axis=-1)
    gate, idx = lax.top_k(jnp.swapaxes(aff, 1, 2), cap)
    xe = jax.vmap(lambda hb, ib: hb[ib])(h, idx)
    a = jnp.einsum('becd,edf->becf', xe, w1)
    u = jnp.einsum('becd,edf->becf', xe, w3)
    y = jnp.einsum('becf,efd->becd', jax.nn.silu(a) * u, w2) * gate[..., None].astype(h.dtype)
    return jax.vmap(lambda yb, ib: jnp.zeros((N, D), yb.dtype).at[ib.reshape(-1)].add(yb.reshape(-1, D)))(y, idx)


def setup_inputs(seed: int = 0) -> dict:
    key = jax.random.key(seed)
    ks = jax.random.split(key, 24)
    D = D_MODEL

    def nrm(k, shape, s):
        return jax.random.normal(k, shape, jnp.float32) * s

    return {
        "x": nrm(ks[0], (BATCH, SEQ, D), 1.0),
        "c": nrm(ks[1], (BATCH, D), 1.0),
        "ctx": nrm(ks[2], (BATCH, CTX_LEN, D), 1.0),
        "c_ctx": nrm(ks[3], (D,), 1.0),
        "mod_w": nrm(ks[4], (DEPTH, D, 6 * D), 0.5 * D ** -0.5),
        "mod_b": nrm(ks[5], (DEPTH, 6 * D), 0.02),
        "norm1_g": 1.0 + nrm(ks[6], (DEPTH, D), 0.02),
        "norm2_g": 1.0 + nrm(ks[7], (DEPTH, D), 0.02),
        "ab_w_in": nrm(ks[8], (N_EVEN, D, AB_IN), D ** -0.5),
        "ab_w_out": nrm(ks[9], (N_EVEN, AB_OUT, D), AB_OUT ** -0.5),
        "sgu_w": nrm(ks[10], (N_EVEN, SGU_GROUPS, SGU_CHUNK, SGU_CHUNK), SGU_CHUNK ** -0.5),
        "sgu_b": 1.0 + nrm(ks[11], (N_EVEN, SGU_GROUPS, SGU_CHUNK), 0.02),
        "hgrn_lb_logits": nrm(ks[12], (N_EVEN + 1, 2, HGRN_DIM), 1.0),
        "hgrn_norm_g": 1.0 + nrm(ks[13], (N_EVEN, HGRN_HEADS, HGRN_VDIM), 0.02),
        "attn_w_qkv": nrm(ks[14], (N_ODD, D, QKV_DIM), D ** -0.5),
        "attn_w_out": nrm(ks[15], (N_ODD, ATTN_Q_DIM, D), ATTN_Q_DIM ** -0.5),
        "attn_sink": nrm(ks[16], (N_ODD, ATTN_HEADS), 1.0),
        "router_w": nrm(ks[17], (DEPTH, D, N_EXPERTS), D ** -0.5),
        "expert_w1": nrm(ks[18], (DEPTH, N_EXPERTS, D, EXPERT_FF), D ** -0.5),
        "expert_w3": nrm(ks[19], (DEPTH, N_EXPERTS, D, EXPERT_FF), D ** -0.5),
        "expert_w2": nrm(ks[20], (DEPTH, N_EXPERTS, EXPERT_FF, D), EXPERT_FF ** -0.5),
        "final_g": 1.0 + nrm(ks[21], (D,), 0.02),
    }


def reference(x, c, ctx, c_ctx, mod_w, mod_b, norm1_g, norm2_g, ab_w_in, ab_w_out, sgu_w, sgu_b,
              hgrn_lb_logits, hgrn_norm_g, attn_w_qkv, attn_w_out, attn_sink, router_w,
              expert_w1, expert_w3, expert_w2, final_g):
    B = x.shape[0]
    lb_all = jnp.cumsum(jax.nn.softmax(hgrn_lb_logits.astype(jnp.float32), axis=0), axis=0)
    x_lat, x_ctx = x, ctx
    for l in range(DEPTH):
        last = l == DEPTH - 1
        mod_lat = (jax.nn.silu(c) @ mod_w[l] + mod_b[l])[:, None, :]
        mod_ctx = (jax.nn.silu(c_ctx) @ mod_w[l] + mod_b[l])[None, None, :]
        sh1, sc1, g1, sh2, sc2, g2 = jnp.split(mod_lat, 6, axis=-1)
        csh1, csc1, cg1, csh2, csc2, cg2 = jnp.split(mod_ctx, 6, axis=-1)
        h_lat = _modulate(x_lat, norm1_g[l], sh1, sc1)
        h_ctx = _modulate(x_ctx, norm1_g[l], csh1, csc1)
        if l % 2 == 0:
            e = l // 2
            params = (ab_w_in[e], ab_w_out[e], sgu_w[e], sgu_b[e], lb_all[e], hgrn_norm_g[e])
            s0 = jnp.zeros((2, B, HGRN_HEADS, HGRN_KDIM, HGRN_VDIM), jnp.float32)
            y_ctx, s_ctx = _even_mixer(h_ctx, *params, s0)
            y_lat, _ = _even_mixer(h_lat, *params, s_ctx)
        else:
            o = l // 2
            y_lat, y_ctx = _window_attention(h_lat, h_ctx, attn_w_qkv[o], attn_w_out[o], attn_sink[o], not last)
        moe = (router_w[l], expert_w1[l], expert_w3[l], expert_w2[l])
        x_lat = x_lat + g1 * y_lat
        x_lat = x_lat + g2 * _expert_choice_ffn(_modulate(x_lat, norm2_g[l], sh2, sc2), *moe)
        if not last:
            x_ctx = x_ctx + cg1 * y_ctx
            x_ctx = x_ctx + cg2 * _expert_choice_ffn(_modulate(x_ctx, norm2_g[l], csh2, csc2), *moe)
    return _rmsnorm(x_lat, final_g)
```

```python
import contextlib
import numpy as np
import ml_dtypes
import concourse.bass as bass
import concourse.mybir as mybir
from concourse.bass_utils import run_bass_kernel_spmd

F32 = mybir.dt.float32
BF16 = mybir.dt.bfloat16
I32 = mybir.dt.int32
U8 = mybir.dt.uint8
ALU = mybir.AluOpType
AF = mybir.ActivationFunctionType
AX = mybir.AxisListType
ISZ = {F32: 4, BF16: 2, I32: 4}

D = 1024
NLAT = 4096
NCTX = 256
NALL = NLAT + NCTX
NT_ALL = NALL // 128
EPS = 1e-6
NE = 16
FF = 2048

ENGS = ("pe", "act", "dve", "pool", "sp")
N_DMA_SEMS = 32
import os as _os
STRICT_SAME_ENGINE = _os.environ.get('STRICT_SAME_ENGINE', '0') == '1'


class Buf:
    def __init__(self, t, name, root=None):
        self.t = t
        self.name = name
        self.root = root if root is not None else self
        if root is None:
            self.whole = [None, []]
            self.subs = {}

    def __getitem__(self, idx):
        return self.t[idx]

    def sub(self, key):
        return (self.root, key)

    def view(self, ap):
        return Buf(ap, self.name + "_v", root=self.root)


def _nk(k):
    return (k.root, None) if isinstance(k, Buf) else (k[0].root, k[1])


class Prog:
    def __init__(self, nc):
        self.nc = nc
        self.ops = {e: [] for e in ENGS}
        self.dma_count = [0] * N_DMA_SEMS
        self.dma_last = [None] * N_DMA_SEMS
        self.dma_rr = 0
        self.dma_rr_pool = 0
        self.bar = {}

    def _states(self, key):
        buf, sk = _nk(key)
        if sk is None:
            return buf, sk, [buf.whole] + list(buf.subs.values())
        if sk not in buf.subs:
            buf.subs[sk] = [None, []]
        return buf, sk, [buf.whole, buf.subs[sk]]

    def barrier(self):
        toks = set()
        for e in ENGS:
            for i in range(len(self.ops[e]) - 1, -1, -1):
                if self.ops[e][i]["dma"] is None:
                    toks.add(("e", e, i))
                    break
        for t in self.dma_last:
            if t is not None:
                toks.add(t)
        self.bar = {e: set(toks) for e in ENGS}

    def op(self, eng, fn, reads=(), writes=(), dma=False):
        deps = set()
        if self.bar.get(eng):
            deps |= self.bar.pop(eng)
        for k in reads:
            _, _, sts = self._states(k)
            for st in sts:
                if st[0] is not None:
                    deps.add(st[0])
        for k in writes:
            _, _, sts = self._states(k)
            for st in sts:
                if st[0] is not None:
                    deps.add(("W",) + st[0])
                for r in st[1]:
                    deps.add(("W",) + r)
        idx = len(self.ops[eng])
        rec = dict(fn=fn, deps=[], sig=False, dma=None)
        if dma:
            half = N_DMA_SEMS // 2
            if eng == "pool":
                s = half + self.dma_rr_pool % half
                self.dma_rr_pool += 1
            else:
                s = self.dma_rr % half
                self.dma_rr += 1
            prev = self.dma_last[s]
            self.dma_count[s] += 16
            tok = ("d", s, self.dma_count[s])
            rec["dma"] = (s, self.dma_count[s])
            if prev is not None:
                deps.add(prev)
            self.dma_last[s] = tok
        else:
            tok = ("e", eng, idx)
        final = set()
        for d in deps:
            war = d[0] == "W"
            if war:
                d = d[1:]
            if d[0] == "e" and d[1] == eng and not dma and war and (eng == "pe" or not STRICT_SAME_ENGINE):
                continue
            if d != tok:
                final.add(d)
        for d in final:
            if d[0] == "e":
                self.ops[d[1]][d[2]]["sig"] = True
        rec["deps"] = sorted(final, key=str)
        self.ops[eng].append(rec)
        for k in reads:
            buf, sk, _ = self._states(k)
            rl = (buf.whole if sk is None else buf.subs[sk])[1]
            if tok[0] == "e":
                rl[:] = [t for t in rl if not (t[0] == "e" and t[1] == tok[1])]
            else:
                rl[:] = [t for t in rl if not (t[0] == "d" and t[1] == tok[1])]
            rl.append(tok)
        for k in writes:
            buf, sk, _ = self._states(k)
            if sk is None:
                buf.whole = [tok, []]
                buf.subs = {}
            else:
                buf.subs[sk] = [tok, []]
        return tok

    def dma(self, out, in_, reads=(), writes=(), eng="sp", **kw):
        return self.op(eng, lambda e: e.dma_start(out=out, in_=in_, **kw), reads, writes, dma=True)

    def emit(self):
        nc = self.nc
        with contextlib.ExitStack() as es:
            esem = {e: es.enter_context(nc.semaphore(f"s_{e}")) for e in ENGS}
            dsem = [es.enter_context(nc.semaphore(f"s_dma{i}")) for i in range(N_DMA_SEMS)]
            sigcnt = {}
            for e in ENGS:
                c = 0
                for i, r in enumerate(self.ops[e]):
                    if r["sig"] and r["dma"] is None:
                        c += 1
                        sigcnt[(e, i)] = c
            es.enter_context(nc.allow_non_contiguous_dma(reason="small strided loads of parameters"))
            block = es.enter_context(nc.Block())
            final_dma = [(s, self.dma_count[s]) for s in range(N_DMA_SEMS) if self.dma_count[s] > 0]

            def run(engname, eobj):
                waited = {}
                for r in self.ops[engname]:
                    for d in r["deps"]:
                        if d[0] == "e":
                            sem, val, key = esem[d[1]], sigcnt[(d[1], d[2])], ("e", d[1])
                        else:
                            sem, val, key = dsem[d[1]], d[2], ("d", d[1])
                        if waited.get(key, 0) >= val:
                            continue
                        waited[key] = val
                        eobj.wait_ge(sem, val)
                    ins = r["fn"](eobj)
                    if r["dma"] is not None:
                        ins.then_inc(dsem[r["dma"][0]], 16)
                    elif r["sig"]:
                        ins.then_inc(esem[engname], 1)
                if engname == "sp":
                    for s, v in final_dma:
                        eobj.wait_ge(dsem[s], v)

            block.sync(lambda e: run("sp", e))
            block.tensor(lambda e: run("pe", e))
            block.scalar(lambda e: run("act", e))
            block.vector(lambda e: run("dve", e))
            block.gpsimd(lambda e: run("pool", e))


ARENA_BYTES = 200 * 1024


class Ctx:
    def __init__(self, nc, debug=()):
        self.nc = nc
        self.p = Prog(nc)
        self.debug = set(debug)
        self.arena = nc.alloc_sbuf_tensor("arena", [128, ARENA_BYTES], U8)
        self.persist_top = 0
        self.off = 0
        self.cnt = 0
        self.banks = [Buf(nc.alloc_psum_tensor(f"psb{i}", [128, 512], F32)[:], f"psb{i}") for i in range(8)]
        self.bank_rr = 0
        self.din = {}
        self.dscr = {}

    def _carve(self, shape, dt, off):
        nb = int(np.prod(shape[1:])) * ISZ[dt]
        t = self.arena[0:shape[0], off:off + nb].bitcast(dt)
        if len(shape) == 3:
            t = t.rearrange("p (a b) -> p a b", a=shape[1])
        elif len(shape) == 4:
            t = t.rearrange("p (a b c) -> p a b c", a=shape[1], b=shape[2])
        return t, (nb + 63) // 64 * 64

    def tile(self, shape, dt, name=None, persist=False):
        self.cnt += 1
        name = name or f"t{self.cnt}"
        if persist:
            assert self.off == self.persist_top, "persistent tiles must be allocated at phase start"
        t, nb = self._carve(shape, dt, self.off)
        self.off += nb
        assert self.off <= ARENA_BYTES, f"SBUF arena overflow {self.off}"
        if persist:
            self.persist_top = self.off
        return Buf(t, name)

    def new_phase(self):
        self.p.barrier()
        self.off = self.persist_top

    def bank(self):
        b = self.banks[self.bank_rr % 8]
        self.bank_rr += 1
        return b

    def inp(self, name, shape, dt):
        self.din[name] = Buf(self.nc.dram_tensor(name, list(shape), dt, kind="ExternalInput").ap(), name)
        return self.din[name]

    def scr(self, name, shape, dt):
        kind = "ExternalOutput" if name in self.debug else "Internal"
        self.dscr[name] = Buf(self.nc.dram_tensor(name, list(shape), dt, kind=kind).ap(), name)
        return self.dscr[name]

    def act(self, out, in_, func, reads, writes, **kw):
        return self.p.op("act", lambda e: e.activation(out=out, in_=in_, func=func, **kw), reads, writes)

    def tt(self, out, in0, in1, op, reads, writes, eng="dve"):
        return self.p.op(eng, lambda e: e.tensor_tensor(out=out, in0=in0, in1=in1, op=op), reads, writes)

    def ts(self, out, in0, s1, s2, op0, op1, reads, writes, eng="dve"):
        if op1 is None:
            return self.p.op(eng, lambda e: e.tensor_scalar(out=out, in0=in0, scalar1=s1, scalar2=None, op0=op0), reads, writes)
        return self.p.op(eng, lambda e: e.tensor_scalar(out=out, in0=in0, scalar1=s1, scalar2=s2, op0=op0, op1=op1), reads, writes)

    def stt(self, out, in0, scalar, in1, op0, op1, reads, writes, eng="dve"):
        return self.p.op(eng, lambda e: e.scalar_tensor_tensor(out=out, in0=in0, scalar=scalar, in1=in1, op0=op0, op1=op1), reads, writes)

    def copy(self, out, in_, reads, writes, eng="dve"):
        if eng == "act":
            return self.p.op("act", lambda e: e.copy(out=out, in_=in_), reads, writes)
        return self.p.op(eng, lambda e: e.tensor_copy(out=out, in_=in_), reads, writes)

    def mm(self, out, lhsT, rhs, start, stop, reads, writes):
        return self.p.op("pe", lambda e: e.matmul(out, lhsT=lhsT, rhs=rhs, start=start, stop=stop), reads, writes)

    def tr(self, out, in_, ident, reads, writes):
        return self.p.op("pe", lambda e: e.transpose(out=out, in_=in_, identity=ident), reads, writes)

    def reduce(self, out, in_, op, reads, writes):
        return self.p.op("dve", lambda e: e.tensor_reduce(out=out, in_=in_, axis=AX.X, op=op), reads, writes)

    def recip(self, out, in_, reads, writes):
        return self.p.op("dve", lambda e: e.reciprocal(out=out, in_=in_), reads, writes)

    def memset(self, ap, val, writes, eng="pool"):
        return self.p.op(eng, lambda e: e.memset(ap, val), (), writes)

    def dma(self, out, in_, reads=(), writes=(), eng="sp", **kw):
        return self.p.dma(out, in_, reads, writes, eng, **kw)


def interleave(*gens):
    gens = [g for g in gens if g is not None]
    while gens:
        for g in list(gens):
            try:
                next(g)
            except StopIteration:
                gens.remove(g)


def bc_mid(ap2d, n):
    P, Fd = ap2d.shape
    return ap2d.unsqueeze(1).to_broadcast([P, n, Fd])


def bc_last(ap2d, n):
    P, A = ap2d.shape
    return ap2d.unsqueeze(2).to_broadcast([P, A, n])


def rms_rstd(C, x, npart, width, tmp, ss, reads):
    C.act(tmp[0:npart, 0:width], x, AF.Square, reads, [tmp, ss], accum_out=ss[0:npart, :])
    C.ts(ss[0:npart, :], ss[0:npart, :], 1.0 / width, EPS, ALU.mult, ALU.add, [ss], [ss])
    C.act(ss[0:npart, :], ss[0:npart, :], AF.Sqrt, [ss], [ss])
    C.recip(ss[0:npart, :], ss[0:npart, :], [ss], [ss])


def norm_mod_transpose(C, K, xt, gs, sh, h_bf, hT, want_T=True):
    tmp, ss = K["ntmp"], K["nss"]
    rms_rstd(C, xt[:], 128, D, tmp, ss, [xt])
    C.stt(tmp[:], xt[:], ss[:], gs[:], ALU.mult, ALU.mult, [xt, ss, gs, tmp], [tmp])
    C.tt(h_bf[:], tmp[:], sh[:], ALU.add, [tmp, sh], [h_bf])
    if want_T:
        transpose_1024(C, K, h_bf, hT)


def transpose_1024(C, K, src_bf, dstT, npart=128, col0=0):
    bk = C.bank()
    pb = bk[:].bitcast(BF16)
    for c in range(8):
        C.tr(pb[:, c * 128:c * 128 + npart], src_bf[0:npart, c * 128:(c + 1) * 128], K["identb"][0:npart, 0:npart],
             [src_bf, K["identb"]], [bk])
    src = pb.rearrange("p (c t) -> p c t", c=8)[:, :, 0:npart]
    C.copy(dstT[:, :, col0:col0 + npart], src, [bk], [dstT], eng="act")


def load_w_bf16(C, dst, w_ap, ncols, nchunks, col0=0, step=512):
    for n0 in range(0, ncols, step):
        n1 = min(ncols, n0 + step)
        C.dma(dst[:, :, col0 + n0:col0 + n1], w_ap[:, n0:n1].rearrange("(c p) n -> p c n", p=128), (), [dst], eng="pool")


def phase_prep(C, K):
    C.new_phase()
    I = C.din
    modv = C.dscr["modv"]
    cs = C.tile([128, 8, 2], F32)
    c2s = C.tile([2, D], F32)
    C.dma(c2s[:], I["c2"][:], (), [c2s])
    C.act(c2s[:], c2s[:], AF.Silu, [c2s], [c2s])
    bk = C.bank()
    for c in range(8):
        C.tr(bk[:, 2 * c:2 * c + 2], c2s[:, c * 128:(c + 1) * 128], K["identf"][0:2, 0:2], [c2s, K["identf"]], [bk])
    C.copy(cs[:].rearrange("p c r -> p (c r)"), bk[:, 0:16], [bk], [cs])
    wbuf = [C.tile([128, 8, 512], F32) for _ in range(2)]
    mv = C.tile([2, 6 * D], F32)
    mb = C.tile([2, 6 * D], F32)
    ng = C.tile([2, D], F32)
    k = 0
    for l in range(2):
        C.dma(mb[:], I["mod_b"][l, :].partition_broadcast(2), (), [mb])
        for n in range(12):
            w = wbuf[k % 2]
            k += 1
            C.dma(w[:], I["mod_w"][l, :, n * 512:(n + 1) * 512].rearrange("(c p) n -> p c n", p=128), (), [w])
            bk = C.bank()
            for c in range(8):
                C.mm(bk[0:2, :], cs[:, c, :], w[:, c, :], c == 0, c == 7, [cs, w], [bk])
            C.tt(mv[:, n * 512:(n + 1) * 512], bk[0:2, :], mb[:, n * 512:(n + 1) * 512], ALU.add, [bk, mb], [mv])
        for slot, gname in ((1, "norm1_g"), (4, "norm2_g")):
            C.dma(ng[:], I[gname][l, :].partition_broadcast(2), (), [ng])
            C.stt(mv[:, slot * D:(slot + 1) * D], mv[:, slot * D:(slot + 1) * D], 1.0, ng[:], ALU.add, ALU.mult, [mv, ng], [mv])
        C.dma(modv[l, :, :], mv[:], [mv], [modv])


def load_mod(C, dst, l, r, slot):
    C.dma(dst[:], C.dscr["modv"][l, r, slot * D:(slot + 1) * D].partition_broadcast(128), [C.dscr["modv"]], [dst])


def alloc_norm_scratch(C, K):
    K["ntmp"] = C.tile([128, D], F32)
    K["nss"] = C.tile([128, 1], F32)


def load_consts(C, K):
    I = C.din
    for name, shape, dt in (("identb", [128, 128], BF16), ("identf", [128, 128], F32), ("onesb", [128, 128], BF16),
                            ("ustrict", [128, 128], BF16)):
        K[name] = C.tile(shape, dt, name, persist=True)
        C.dma(K[name][:], I[name][:], (), [K[name]])
    K["logits"] = C.tile([128, NT_ALL, NE], F32, "logits", persist=True)


def phase_mixer_in(C, K):
    C.new_phase()
    I, S = C.din, C.dscr
    Kp = []
    for _ in range(2):
        kk_ = dict(K)
        alloc_norm_scratch(C, kk_)
        Kp.append(kk_)
    w_in = C.tile([128, 8, 3584], BF16)
    load_w_bf16(C, w_in, I["ab_w_in"][:], 3584, 8)
    ws = C.tile([128, 4, 128], F32)
    wsT = C.tile([128, 4, 128], BF16)
    C.dma(ws[:], I["sgu_w"][:].rearrange("g t s -> t g s"), (), [ws])
    bk = C.bank()
    for g in range(4):
        C.tr(bk[:, g * 128:(g + 1) * 128], ws[:, g, :], K["identf"][:], [ws, K["identf"]], [bk])
    C.copy(wsT[:].rearrange("p g t -> p (g t)"), bk[:], [bk], [wsT])
    bsT = C.tile([128, 4], F32)
    C.dma(bsT[:], I["sgu_b"][:].rearrange("g t -> t g"), (), [bsT])
    lbl = C.tile([128, 2, 2, 512], F32)
    C.dma(lbl[:].rearrange("p a b c -> p (a b c)"), I["hgrn_lb_logits"][:].rearrange("a b c -> (a b c)").partition_broadcast(128), (), [lbl])
    lb = C.tile([128, 2, 512], F32)
    oml = C.tile([128, 2, 512], F32)
    C.tt(lb[:], lbl[:, 0, :, :], lbl[:, 1, :, :], ALU.subtract, [lbl], [lb])
    C.act(lb[:], lb[:], AF.Sigmoid, [lb], [lb])
    C.ts(oml[:], lb[:], -1.0, 1.0, ALU.mult, ALU.add, [lb], [oml])
    mods = {}
    for r in range(2):
        gs = C.tile([128, D], F32)
        sh = C.tile([128, D], F32)
        load_mod(C, gs, 0, r, 1)
        load_mod(C, sh, 0, r, 0)
        mods[r] = (gs, sh)
    xt = [C.tile([128, D], F32) for _ in range(4)]
    h_bf2 = [C.tile([128, D], BF16) for _ in range(2)]
    hT2 = [C.tile([128, 8, 128], BF16) for _ in range(2)]
    u2 = [C.tile([128, 4, 128], F32) for _ in range(2)]
    v2 = [C.tile([128, 4, 128], F32) for _ in range(2)]
    sq2 = [C.tile([128, 4, 128], F32) for _ in range(2)]
    st42 = [C.tile([128, 4], F32) for _ in range(2)]
    st4b2 = [C.tile([128, 4], F32) for _ in range(2)]
    vn2 = [C.tile([128, 4, 128], BF16) for _ in range(2)]
    f32t4 = [C.tile([128, 512], F32) for _ in range(4)]
    ya = [C.tile([128, 512], BF16) for _ in range(2)]
    qo = [C.tile([128, 512], BF16) for _ in range(2)]
    kk = [C.tile([128, 512], BF16) for _ in range(4)]
    gl = [C.tile([128, 512], F32) for _ in range(4)]
    vo = [C.tile([128, 512], BF16) for _ in range(2)]
    og = [C.tile([128, 512], BF16) for _ in range(2)]

    def src_rows(i):
        if i < 2:
            return I["ctx"][i * 128:(i + 1) * 128, :]
        return I["x"][(i - 2) * 128:(i - 1) * 128, :]

    def load(i):
        C.dma(xt[i % 4][:], src_rows(i), (), [xt[i % 4]])

    def tile_body(i):
        b = i % 2
        r = 1 if i < 2 else 0
        rows = slice(i * 128, (i + 1) * 128)
        h_bf, hT, u, v, sq, st4, st4b, vn = h_bf2[b], hT2[b], u2[b], v2[b], sq2[b], st42[b], st4b2[b], vn2[b]
        norm_mod_transpose(C, Kp[b], xt[i % 4], mods[r][0], mods[r][1], h_bf, hT)
        yield
        zb = []
        for n in range(7):
            bk = C.bank()
            for c in range(8):
                C.mm(bk[:], hT[:, c, :], w_in[:, c, n * 512:(n + 1) * 512], c == 0, c == 7, [hT, w_in], [bk])
            zb.append(bk)
        C.act(u[:].rearrange("p g c -> p (g c)"), zb[0][:], AF.Gelu, [zb[0]], [u])
        C.act(v[:].rearrange("p g c -> p (g c)"), zb[1][:], AF.Gelu, [zb[1]], [v])
        C.reduce(st4[:], v[:], ALU.add, [v], [st4])
        C.ts(st4[:], st4[:], 1.0 / 128, None, ALU.mult, None, [st4], [st4])
        C.tt(v[:], v[:], bc_last(st4[:], 128), ALU.subtract, [v, st4], [v])
        C.act(sq[:], v[:], AF.Square, [v], [sq])
        C.reduce(st4b[:], sq[:], ALU.add, [sq], [st4b])
        C.ts(st4b[:], st4b[:], 1.0 / 128, EPS, ALU.mult, ALU.add, [st4b], [st4b])
        C.act(st4b[:], st4b[:], AF.Sqrt, [st4b], [st4b])
        C.recip(st4b[:], st4b[:], [st4b], [st4b])
        C.tt(vn[:], v[:], bc_last(st4b[:], 128), ALU.mult, [v, st4b], [vn])
        bk = C.bank()
        for g in range(4):
            C.mm(bk[:, g * 128:(g + 1) * 128], wsT[:, g, :], vn[:, g, :], True, True, [wsT, vn], [bk])
        for g in range(4):
            C.stt(ya[b][:, g * 128:(g + 1) * 128], bk[:, g * 128:(g + 1) * 128], bsT[:, g:g + 1], u[:, g, :],
                  ALU.add, ALU.mult, [bk, bsT, u], [ya[b]])
        C.dma(S["ya"][rows, :], ya[b][:], [ya[b]], [S["ya"].sub(i)], eng="pool")
        C.act(qo[b][:], zb[2][:], AF.Silu, [zb[2]], [qo[b]])
        C.dma(S["q"][rows, :], qo[b][:], [qo[b]], [S["q"].sub(i)], eng="pool")
        for d in range(2):
            kb_, gb_ = kk[2 * b + d], gl[2 * b + d]
            f32t = f32t4[2 * b + d]
            C.act(f32t[:], zb[3 + d][:], AF.Sigmoid, [zb[3 + d]], [f32t])
            C.tt(f32t[:], f32t[:], oml[:, d, :], ALU.mult, [f32t, oml], [f32t])
            C.tt(f32t[:], f32t[:], lb[:, d, :], ALU.add, [f32t, lb], [f32t])
            C.ts(kb_[:], f32t[:], -1.0, 1.0, ALU.mult, ALU.add, [f32t], [kb_])
            C.act(gb_[:], f32t[:], AF.Ln, [f32t], [gb_])
            C.dma(S["kk"][d, rows, :], kb_[:], [kb_], [S["kk"].sub((d, i))], eng="pool")
            C.dma(S["gl"][d, rows, :], gb_[:], [gb_], [S["gl"].sub((d, i))], eng="pool")
        C.copy(vo[b][:], zb[5][:], [zb[5]], [vo[b]], eng="act")
        C.dma(S["v"][rows, :], vo[b][:], [vo[b]], [S["v"].sub(i)], eng="pool")
        C.act(og[b][:], zb[6][:], AF.Silu, [zb[6]], [og[b]])
        C.dma(S["og"][rows, :], og[b][:], [og[b]], [S["og"].sub(i)], eng="pool")

    load(0)
    load(1)
    for i in range(0, NT_ALL, 2):
        for j in (i + 2, i + 3):
            if j < NT_ALL:
                load(j)
        interleave(tile_body(i), tile_body(i + 1))


def phase_gla(C, K):
    C.new_phase()
    I, S = C.din, C.dscr
    NCH = NALL // 64
    cst = {}
    for name, shape, dt in (("gla_m", [64, 2, 64], F32), ("gla_sel", [64, 2, 2], F32), ("gla_tri", [64, 2, 64], F32)):
        cst[name] = C.tile(shape, dt)
        C.dma(cst[name][:], I[name][:], (), [cst[name]])
    St = [C.tile([128, 4, 128], F32) for _ in range(2)]
    for d in range(2):
        C.memset(St[d][:], 0.0, [St[d]])
    NB = 2
    bufs = {}
    for d in range(2):
        for j in range(NB):
            bufs[(d, j)] = dict(
                q=C.tile([64, 512], BF16), kk=C.tile([64, 512], BF16), g=C.tile([64, 512], F32), v=C.tile([64, 512], BF16),
                ep=C.tile([64, 512], F32), em=C.tile([64, 512], F32), qb=C.tile([64, 512], BF16), kb=C.tile([64, 512], BF16),
                qkT=C.tile([128, 512], BF16), e3=C.tile([128, 4, 3], F32), ex=C.tile([128, 4, 3], F32),
                at=C.tile([64, 4, 64], BF16), ssc=C.tile([128, 4, 128], BF16), osb=C.tile([64, 512], F32),
                tmp=C.tile([128, 4, 128], F32))
    order = {0: list(range(NCH)), 1: [3, 2, 1, 0] + list(range(NCH - 1, 3, -1))}

    def load(d, step):
        ci = order[d][step]
        B = bufs[(d, step % NB)]
        rows = slice(ci * 64, (ci + 1) * 64)
        t = ci // 2
        C.dma(B["q"][:], S["q"][rows, :], [S["q"].sub(t)], [B["q"]])
        C.dma(B["kk"][:], S["kk"][d, rows, :], [S["kk"].sub((d, t))], [B["kk"]])
        C.dma(B["g"][:], S["gl"][d, rows, :], [S["gl"].sub((d, t))], [B["g"]])
        C.dma(B["v"][:], S["v"][rows, :], [S["v"].sub(t)], [B["v"]])

    def compute(d, step):
        ci = order[d][step]
        B = bufs[(d, step % NB)]
        rows = slice(ci * 64, (ci + 1) * 64)
        Sd = St[d]
        idb = K["identb"]
        bps = C.bank()
        C.mm(bps[0:64, :], cst["gla_m"][:, d, :], B["g"][:], True, True, [cst["gla_m"], B["g"]], [bps])
        C.act(B["ep"][:], bps[0:64, :], AF.Exp, [bps], [B["ep"]])
        C.act(B["em"][:], bps[0:64, :], AF.Exp, [bps], [B["em"]], scale=-1.0)
        C.tt(B["qb"][:], B["q"][:], B["ep"][:], ALU.mult, [B["q"], B["ep"]], [B["qb"]])
        C.tt(B["kb"][:], B["kk"][:], B["em"][:], ALU.mult, [B["kk"], B["em"]], [B["kb"]])
        yield
        tb = C.bank()
        pb = tb[:].bitcast(BF16)
        for h in range(4):
            C.tr(pb[:, h * 64:(h + 1) * 64], B["qb"][:, h * 128:(h + 1) * 128], idb[0:64, 0:64], [B["qb"], idb], [tb])
        for h in range(4):
            C.tr(pb[:, 256 + h * 64:256 + (h + 1) * 64], B["kb"][:, h * 128:(h + 1) * 128], idb[0:64, 0:64], [B["kb"], idb], [tb])
        C.copy(B["qkT"][:], pb[:, 0:512], [tb], [B["qkT"]], eng="act")
        yield
        sps = C.bank()
        for h in range(4):
            C.mm(sps[:, h * 2:h * 2 + 2], B["g"][:, h * 128:(h + 1) * 128], cst["gla_sel"][:, d, :], True, True,
                 [B["g"], cst["gla_sel"]], [sps])
        spv = sps[:, 0:8].rearrange("p (h c) -> p h c", c=2)
        C.copy(B["e3"][:, :, 0:2], spv, [sps], [B["e3"]])
        C.tt(B["e3"][:, :, 2], B["e3"][:, :, 1], B["e3"][:, :, 0], ALU.subtract, [B["e3"]], [B["e3"]])
        C.act(B["ex"][:], B["e3"][:], AF.Exp, [B["e3"]], [B["ex"]])
        yield
        aps = C.bank()
        for h in range(4):
            C.mm(aps[0:64, h * 64:(h + 1) * 64], B["qkT"][:, 256 + h * 64:256 + (h + 1) * 64], B["qkT"][:, h * 64:(h + 1) * 64],
                 True, True, [B["qkT"]], [aps])
        C.tt(B["at"][:], aps[0:64, 0:256].rearrange("p (h t) -> p h t", h=4), bc_mid(cst["gla_tri"][:, d, :], 4), ALU.mult,
             [aps, cst["gla_tri"]], [B["at"]])
        yield
        C.tt(B["ssc"][:], Sd[:], bc_last(B["ex"][:, :, 0], 128), ALU.mult, [Sd, B["ex"]], [B["ssc"]], eng="pool")
        ops_ = C.bank()
        for h in range(4):
            C.mm(ops_[0:64, h * 128:(h + 1) * 128], B["at"][:, h, :], B["v"][:, h * 128:(h + 1) * 128], True, False,
                 [B["at"], B["v"]], [ops_])
            C.mm(ops_[0:64, h * 128:(h + 1) * 128], B["qkT"][:, h * 64:(h + 1) * 64], B["ssc"][:, h, :], False, True,
                 [B["qkT"], B["ssc"]], [ops_])
        C.copy(B["osb"][:], ops_[0:64, :], [ops_], [B["osb"]], eng="act")
        C.dma(S["o"][d, rows, :], B["osb"][:], [B["osb"]], [S["o"].sub((d, ci))], eng="pool")
        yield
        dps = C.bank()
        for h in range(4):
            C.mm(dps[:, h * 128:(h + 1) * 128], B["kb"][:, h * 128:(h + 1) * 128], B["v"][:, h * 128:(h + 1) * 128], True, True,
                 [B["kb"], B["v"]], [dps])
        C.tt(B["tmp"][:], dps[:].rearrange("p (h v) -> p h v", h=4), bc_last(B["ex"][:, :, 2], 128), ALU.mult, [dps, B["ex"]], [B["tmp"]])
        C.tt(Sd[:], Sd[:], bc_last(B["ex"][:, :, 1], 128), ALU.mult, [Sd, B["ex"]], [Sd], eng="pool")
        C.tt(Sd[:], Sd[:], B["tmp"][:], ALU.add, [Sd, B["tmp"]], [Sd], eng="pool")

    for d in range(2):
        load(d, 0)
    for step in range(NCH):
        for d in range(2):
            if step + 1 < NCH:
                load(d, step + 1)
        interleave(compute(0, step), compute(1, step))


def residual_norm2_router(C, K, W, i, l, r, ybanks, xt, mods2, bufs):
    S = C.dscr
    g1b, gs2, sh2 = mods2
    x1, h2, h2T = bufs
    rows = slice(i * 128, (i + 1) * 128)
    if x1 is None:
        tmp = K["ntmp"]
        for n in range(2):
            C.tt(tmp[:, n * 512:(n + 1) * 512], ybanks[n][:], g1b[:, n * 512:(n + 1) * 512], ALU.mult, [ybanks[n], g1b], [tmp])
        C.tt(xt[:], tmp[:], xt[:], ALU.add, [tmp, xt], [xt])
        x1 = xt
    else:
        for n in range(2):
            C.tt(x1[:, n * 512:(n + 1) * 512], ybanks[n][:], g1b[:, n * 512:(n + 1) * 512], ALU.mult, [ybanks[n], g1b], [x1])
        C.tt(x1[:], x1[:], xt[:], ALU.add, [x1, xt], [x1])
    C.dma(S["xres"][rows, :], x1[:], [x1], [S["xres"].sub(i)], eng="pool")
    norm_mod_transpose(C, K, x1, gs2, sh2, h2, h2T)
    C.dma(S["hA"][rows, :], h2[:], [h2], [S["hA"].sub(i)], eng="pool")
    bk = C.bank()
    for c in range(8):
        C.mm(bk[:, 0:NE], h2T[:, c, :], W["wr"][:, c, :], c == 0, c == 7, [h2T, W["wr"]], [bk])
    C.copy(K["logits"][:, i, :], bk[:, 0:NE], [bk], [K["logits"].sub(i)])


def load_router_w(C, l):
    wr = C.tile([128, 8, NE], BF16)
    C.dma(wr[:], C.din["router_w"][l, :, :].rearrange("(c p) n -> p c n", p=128), (), [wr], eng="pool")
    return wr


def phase_mixer_out(C, K):
    C.new_phase()
    I, S = C.din, C.dscr
    Kp = []
    for _ in range(2):
        kk_ = dict(K)
        alloc_norm_scratch(C, kk_)
        Kp.append(kk_)
    w_out = C.tile([128, 8, D], BF16)
    load_w_bf16(C, w_out, I["ab_w_out"][:], D, 8)
    W = dict(wr=load_router_w(C, 0))
    ngb = C.tile([128, 512], F32)
    C.dma(ngb[:], I["hgrn_norm_g"][:].rearrange("h v -> (h v)").partition_broadcast(128), (), [ngb])
    mods2 = {}
    for r in range(2):
        g1b, gs2, sh2 = C.tile([128, D], F32), C.tile([128, D], F32), C.tile([128, D], F32)
        load_mod(C, g1b, 0, r, 2)
        load_mod(C, gs2, 0, r, 4)
        load_mod(C, sh2, 0, r, 3)
        mods2[r] = (g1b, gs2, sh2)
    NB = 4
    of = [C.tile([128, 4, 128], F32) for _ in range(NB)]
    ob = [C.tile([128, 4, 128], F32) for _ in range(NB)]
    ogt = [C.tile([128, 512], BF16) for _ in range(NB)]
    ycat = [C.tile([128, D], BF16) for _ in range(NB)]
    xt = [C.tile([128, D], F32) for _ in range(NB)]
    sq2 = [C.tile([128, 4, 128], F32) for _ in range(2)]
    st42 = [C.tile([128, 4], F32) for _ in range(2)]
    ycT2 = [C.tile([128, 8, 128], BF16) for _ in range(2)]
    h22 = [C.tile([128, D], BF16) for _ in range(2)]
    h2T2 = [C.tile([128, 8, 128], BF16) for _ in range(2)]

    def load(i):
        b = i % NB
        rows = slice(i * 128, (i + 1) * 128)
        C.dma(of[b][:].rearrange("p h v -> p (h v)"), S["o"][0, rows, :], [S["o"]], [of[b]])
        C.dma(ob[b][:].rearrange("p h v -> p (h v)"), S["o"][1, rows, :], [S["o"]], [ob[b]])
        C.dma(ogt[b][:], S["og"][rows, :], [S["og"]], [ogt[b]])
        C.dma(ycat[b][:, 0:512], S["ya"][rows, :], [S["ya"]], [ycat[b]])
        src = I["ctx"][i * 128:(i + 1) * 128, :] if i < 2 else I["x"][(i - 2) * 128:(i - 1) * 128, :]
        C.dma(xt[b][:], src, (), [xt[b]])

    def tile_body(i):
        b = i % NB
        pb_ = i % 2
        sq, st4, ycT, h2, h2T = sq2[pb_], st42[pb_], ycT2[pb_], h22[pb_], h2T2[pb_]
        r = 1 if i < 2 else 0
        o = of[b]
        C.tt(o[:], o[:], ob[b][:], ALU.add, [o, ob[b]], [o])
        C.act(sq[:], o[:], AF.Square, [o], [sq])
        C.reduce(st4[:], sq[:], ALU.add, [sq], [st4])
        C.ts(st4[:], st4[:], 1.0 / 128, EPS, ALU.mult, ALU.add, [st4], [st4])
        C.act(st4[:], st4[:], AF.Sqrt, [st4], [st4])
        C.recip(st4[:], st4[:], [st4], [st4])
        C.tt(o[:], o[:], bc_last(st4[:], 128), ALU.mult, [o, st4], [o])
        of2 = o[:].rearrange("p h v -> p (h v)")
        C.tt(of2, of2, ngb[:], ALU.mult, [o, ngb], [o])
        C.tt(ycat[b][:, 512:1024], of2, ogt[b][:], ALU.mult, [o, ogt[b], ycat[b]], [ycat[b]])
        yield
        transpose_1024(C, K, ycat[b], ycT)
        yield
        yb = []
        for n in range(2):
            bk = C.bank()
            for c in range(8):
                C.mm(bk[:], ycT[:, c, :], w_out[:, c, n * 512:(n + 1) * 512], c == 0, c == 7, [ycT, w_out], [bk])
            yb.append(bk)
        residual_norm2_router(C, Kp[pb_], W, i, 0, r, yb, xt[b], mods2[r], (None, h2, h2T))

    load(0)
    load(1)
    for i in range(0, NT_ALL, 2):
        for j in (i + 2, i + 3):
            if j < NT_ALL:
                load(j)
        interleave(tile_body(i), tile_body(i + 1))


def phase_route(C, K, tile0, ntl, cap, rt):
    C.new_phase()
    I = C.din
    lg = K["logits"][:, tile0:tile0 + ntl, :]
    LR = [K["logits"]]
    NF = ntl * NE
    aff = C.tile([128, ntl, NE], F32)
    t2 = C.tile([128, ntl], F32)
    C.reduce(t2[:], lg, ALU.max, LR, [t2])
    C.tt(aff[:], lg, bc_last(t2[:], NE), ALU.subtract, LR + [t2], [aff])
    C.act(aff[:], aff[:], AF.Exp, [aff], [aff])
    C.reduce(t2[:], aff[:], ALU.add, [aff], [t2])
    C.recip(t2[:], t2[:], [t2], [t2])
    C.tt(aff[:], aff[:], bc_last(t2[:], NE), ALU.mult, [aff, t2], [aff])
    lo, hi, mid = C.tile([128, NE], F32), C.tile([128, NE], F32), C.tile([128, NE], F32)
    cnt, ge, dlt = C.tile([128, NE], F32), C.tile([128, NE], F32), C.tile([128, NE], F32)
    cmpb = C.tile([128, ntl, NE], BF16)
    C.memset(lo[:], 0.0, [lo])
    C.memset(hi[:], 1.0, [hi])
    C.memset(mid[:], 0.5, [mid])
    onesb = K["onesb"]
    for it in range(30):
        C.tt(cmpb[:], aff[:], bc_mid(mid[:], ntl), ALU.is_gt, [aff, mid], [cmpb])
        bk = C.bank()
        C.mm(bk[:, 0:NF], onesb[:], cmpb[:].rearrange("p i e -> p (i e)"), True, True, [onesb, cmpb], [bk])
        C.reduce(cnt[:], bk[:, 0:NF].rearrange("p (i e) -> p e i", e=NE), ALU.add, [bk], [cnt])
        C.ts(ge[:], cnt[:], float(cap), None, ALU.is_ge, None, [cnt], [ge])
        C.tt(dlt[:], mid[:], lo[:], ALU.subtract, [mid, lo], [dlt])
        C.tt(dlt[:], dlt[:], ge[:], ALU.mult, [dlt, ge], [dlt])
        C.tt(lo[:], lo[:], dlt[:], ALU.add, [lo, dlt], [lo])
        C.tt(dlt[:], hi[:], mid[:], ALU.subtract, [hi, mid], [dlt])
        C.tt(dlt[:], dlt[:], ge[:], ALU.mult, [dlt, ge], [dlt])
        C.tt(hi[:], mid[:], dlt[:], ALU.add, [mid, dlt], [hi])
        C.tt(mid[:], lo[:], hi[:], ALU.add, [lo, hi], [mid])
        C.ts(mid[:], mid[:], 0.5, None, ALU.mult, None, [mid], [mid])
    Ae = C.tile([128, NE, ntl], F32)
    inc = C.tile([128, NE, ntl], F32)
    rmask = C.tile([128, NE, ntl], F32)
    Aeb = C.tile([128, NE, ntl], BF16)
    excb = C.tile([128, NE, ntl], BF16)
    posm = C.tile([128, NE, ntl], F32)
    C.tt(Ae[:], aff[:].rearrange("p i e -> p e i"), bc_last(lo[:], ntl), ALU.is_gt, [aff, lo], [Ae])
    C.memset(rmask[:], 1.0, [rmask])
    C.memset(rmask[:, :, 0:1], 0.0, [rmask])
    flat = lambda t: t[:].rearrange("p e i -> p (e i)")
    C.p.op("dve", lambda e: e.tensor_tensor_scan(out=flat(inc), data0=flat(rmask), data1=flat(Ae), initial=0.0,
                                                 op0=ALU.mult, op1=ALU.add), [rmask, Ae], [inc])
    C.tt(inc[:], inc[:], Ae[:], ALU.subtract, [inc, Ae], [inc])
    C.copy(Aeb[:], Ae[:], [Ae], [Aeb])
    C.copy(excb[:], inc[:], [inc], [excb])
    bk = C.bank()
    C.mm(bk[:, 0:NF], K["ustrict"][:], flat(Aeb), True, False, [K["ustrict"], Aeb], [bk])
    C.mm(bk[:, 0:NF], onesb[:], flat(excb), False, True, [onesb, excb], [bk])
    C.stt(flat(posm), bk[:, 0:NF], 1.0, flat(Ae), ALU.add, ALU.mult, [bk, Ae], [posm])
    C.ts(posm[:], posm[:], -1.0, None, ALU.add, None, [posm], [posm])
    vals = C.tile([128, ntl, NE, 4], BF16)
    tv = C.tile([128, NT_ALL, 2], BF16)
    C.dma(tv[:], I["tvals"][:], (), [tv])
    affb = C.tile([128, ntl, NE], BF16)
    afr = C.tile([128, ntl, NE], F32)
    C.copy(affb[:], aff[:], [aff], [affb])
    C.tt(afr[:], aff[:], affb[:], ALU.subtract, [aff, affb], [afr])
    for j in range(2):
        C.copy(vals[:, :, :, j], bc_last(tv[:, tile0:tile0 + ntl, j], NE), [tv], [vals])
    C.copy(vals[:, :, :, 2], affb[:], [affb], [vals])
    C.copy(vals[:, :, :, 3], afr[:], [afr], [vals])
    iota = C.tile([128, 512], F32)
    C.dma(iota[:], I["iota"][:], (), [iota])
    rows = min(cap, 128)
    njt = cap // rows
    Pm = [C.tile([128, cap], BF16) for _ in range(4)]
    Rsb = [C.tile([4, cap], F32) for _ in range(2)]
    tq = C.tile([128, 4], F32)
    idf = C.tile([128, 1], F32)
    k = 0
    for e in range(NE):
        rb = C.bank()
        for i in range(ntl):
            Pt = Pm[k % 4]
            k += 1
            C.ts(Pt[:], iota[:, 0:cap], posm[:, e, i:i + 1], None, ALU.is_equal, None, [iota, posm], [Pt])
            C.mm(rb[0:4, 0:cap], vals[:, i, e, :], Pt[:], i == 0, i == ntl - 1, [vals, Pt], [rb])
        R = Rsb[e % 2]
        C.copy(R[:], rb[0:4, 0:cap], [rb], [R], eng="act")
        for jt in range(njt):
            tb = C.bank()
            C.tr(tb[0:rows, 0:4], R[:, jt * rows:(jt + 1) * rows], K["identf"][0:4, 0:4], [R, K["identf"]], [tb])
            C.copy(tq[0:rows, :], tb[0:rows, 0:4], [tb], [tq])
            C.stt(idf[0:rows, :], tq[0:rows, 0:1], 64.0, tq[0:rows, 1:2], ALU.mult, ALU.add, [tq], [idf])
            C.copy(rt["idx"][0:rows, e, jt:jt + 1], idf[0:rows, :], [idf], [rt["idx"]])
            C.tt(rt["gate"][0:rows, e, jt:jt + 1], tq[0:rows, 2:3], tq[0:rows, 3:4], ALU.add, [tq], [rt["gate"]])


def phase_experts(C, K, l, groups):
    C.new_phase()
    I, S = C.din, C.dscr
    ttiles, segs = [], []
    col = 0
    for r, cap, rt in groups:
        g2b = C.tile([128, D], F32)
        load_mod(C, g2b, l, r, 5)
        rows = min(cap, 128)
        for jt in range(cap // rows):
            ttiles.append((rt, jt, rows, col + jt * rows, g2b))
        segs.append((col, cap))
        col += cap
    NTOK = col
    ntt = len(ttiles)
    NBA, NBY = 3, 2
    W1 = [C.tile([128, 8, 512], BF16) for _ in range(NBA)]
    W3 = [C.tile([128, 8, 512], BF16) for _ in range(NBA)]
    W2 = [C.tile([128, 16, 512], BF16) for _ in range(NBY)]
    xe = [C.tile([128, D], BF16) for _ in range(2)]
    xeT = [C.tile([128, 8, NTOK], BF16) for _ in range(2)]
    gT = [C.tile([128, 16, NTOK], BF16) for _ in range(2)]
    sa = [C.tile([128, NTOK], BF16) for _ in range(2)]
    yo = [C.tile([128, ntt, D], F32) for _ in range(2)]
    pieces = []
    for e in range(NE):
        pieces += [(e, "a", fq) for fq in range(4)] + [(e, "y", dh) for dh in range(2)]
    cnt = {"a": 0, "y": 0}
    slot = {}

    def load_piece(pi):
        e, kind, j = pieces[pi]
        if kind == "a":
            s_ = cnt["a"] % NBA
            cnt["a"] += 1
            slot[pi] = s_
            C.dma(W1[s_][:], I["expert_w1"][l, e, :, j * 512:(j + 1) * 512].rearrange("(c p) n -> p c n", p=128), (), [W1[s_]], eng="pool")
            C.dma(W3[s_][:], I["expert_w3"][l, e, :, j * 512:(j + 1) * 512].rearrange("(c p) n -> p c n", p=128), (), [W3[s_]], eng="pool")
        else:
            s_ = cnt["y"] % NBY
            cnt["y"] += 1
            slot[pi] = s_
            C.dma(W2[s_][:], I["expert_w2"][l, e, :, j * 512:(j + 1) * 512].rearrange("(c p) n -> p c n", p=128), (), [W2[s_]], eng="pool")

    def gather(e):
        xT = xeT[e % 2]
        for ti, (rt, jt, rows, col0, _) in enumerate(ttiles):
            xg = xe[ti % 2]
            C.p.op("pool", lambda en, xg=xg, jt=jt, rt=rt, rows=rows: en.indirect_dma_start(
                out=xg[0:rows, :], out_offset=None, in_=S["hA"][:, :],
                in_offset=bass.IndirectOffsetOnAxis(ap=rt["idx"][0:rows, e, jt:jt + 1], axis=0)),
                [rt["idx"], S["hA"]], [xg], dma=True)
            transpose_1024(C, K, xg, xT, npart=rows, col0=col0)

    load_piece(0)
    load_piece(1)
    gather(0)
    for pi in range(len(pieces)):
        if pi + 2 < len(pieces):
            load_piece(pi + 2)
        e, kind, j = pieces[pi]
        s_ = slot[pi]
        xT, g_, y_ = xeT[e % 2], gT[e % 2], yo[e % 2]
        if kind == "a":
            if j == 1 and e + 1 < NE:
                gather(e + 1)
            for ft in range(4):
                f = j * 4 + ft
                sb_ = sa[f % 2]
                for c0, nc_ in segs:
                    ab, ub = C.bank(), C.bank()
                    for c in range(8):
                        C.mm(ab[:, 0:nc_], W1[s_][:, c, ft * 128:(ft + 1) * 128], xT[:, c, c0:c0 + nc_], c == 0, c == 7, [W1[s_], xT], [ab])
                    for c in range(8):
                        C.mm(ub[:, 0:nc_], W3[s_][:, c, ft * 128:(ft + 1) * 128], xT[:, c, c0:c0 + nc_], c == 0, c == 7, [W3[s_], xT], [ub])
                    C.act(sb_[:, c0:c0 + nc_], ab[:, 0:nc_], AF.Silu, [ab], [sb_])
                    C.tt(g_[:, f, c0:c0 + nc_], sb_[:, c0:c0 + nc_], ub[:, 0:nc_], ALU.mult, [sb_, ub], [g_])
        else:
            for ti, (rt, jt, rows, col0, g2b) in enumerate(ttiles):
                yb = C.bank()
                for fc in range(16):
                    C.mm(yb[0:rows, :], g_[:, fc, col0:col0 + rows], W2[s_][:, fc, :], fc == 0, fc == 15, [g_, W2[s_]], [yb])
                C.stt(y_[0:rows, ti, j * 512:(j + 1) * 512], yb[0:rows, :], rt["gate"][0:rows, e, jt:jt + 1], g2b[0:rows, j * 512:(j + 1) * 512],
                      ALU.mult, ALU.mult, [yb, rt["gate"], g2b], [y_])
            if j == 1:
                for ti, (rt, jt, rows, col0, g2b) in enumerate(ttiles):
                    C.p.op("pool", lambda en, y_=y_, jt=jt, e=e, rt=rt, rows=rows, ti=ti: en.indirect_dma_start(
                        out=S["xres"][:, :], out_offset=bass.IndirectOffsetOnAxis(ap=rt["idx"][0:rows, e, jt:jt + 1], axis=0),
                        in_=y_[0:rows, ti, :], in_offset=None, compute_op=ALU.add),
                        [rt["idx"], y_], [S["xres"]], dma=True)


def alloc_route(C, cap):
    rows = min(cap, 128)
    njt = cap // rows
    return dict(idx=C.tile([128, NE, njt], I32, persist=True), gate=C.tile([128, NE, njt], F32, persist=True))


import os
ATT_LEVEL = int(os.environ.get('ATT_LEVEL', '9'))
ATT_TILES = int(os.environ.get('ATT_TILES', str(NT_ALL)))


def phase_attention(C, K):
    C.new_phase()
    I, S = C.din, C.dscr
    alloc_norm_scratch(C, K)
    wqkv = C.tile([128, 8, 1536], BF16)
    load_w_bf16(C, wqkv, I["attn_w_qkv"][:], 1536, 8)
    w_out = C.tile([128, 8, D], BF16)
    load_w_bf16(C, w_out, I["attn_w_out"][:], D, 8)
    W = dict(wr=load_router_w(C, 1))
    idb = K["identb"]
    kT = C.tile([64, 4, NALL], BF16)
    vaug = C.tile([128, NT_ALL, 4, 80], BF16)
    C.memset(vaug[:], 1.0, [vaug])
    masks = C.tile([128, 2, 128], BF16)
    C.dma(masks[:], I["amask"][:], (), [masks])
    esink = C.tile([128, 16], F32)
    C.dma(esink[:], I["attn_sink"][:].partition_broadcast(128), (), [esink])
    C.act(esink[:], esink[:], AF.Exp, [esink], [esink])
    gs1, sh1 = C.tile([128, D], F32), C.tile([128, D], F32)
    load_mod(C, gs1, 1, 1, 1)
    load_mod(C, sh1, 1, 1, 0)
    g1b, gs2, sh2 = C.tile([128, D], F32), C.tile([128, D], F32), C.tile([128, D], F32)
    load_mod(C, g1b, 1, 0, 2)
    load_mod(C, gs2, 1, 0, 4)
    load_mod(C, sh2, 1, 0, 3)
    xt = [C.tile([128, D], F32) for _ in range(2)]
    xt2 = [C.tile([128, D], F32) for _ in range(1)]
    rc = [C.tile([128, 512], F32) for _ in range(2)]
    rs = [C.tile([128, 512], F32) for _ in range(2)]
    h_bf = C.tile([128, D], BF16)
    hT = C.tile([128, 8, 128], BF16)
    zs = C.tile([128, 1280], F32)
    t1 = C.tile([128, 20, 64], F32)
    t2 = C.tile([128, 20, 64], F32)
    qk = C.tile([128, 20, 64], BF16)
    qT = [C.tile([64, 16, 128], BF16) for _ in range(3)]
    pT = [C.tile([128, 512], BF16) for _ in range(5)]
    oat2 = [C.tile([128, 16, 64], BF16) for _ in range(2)]
    den = C.tile([128, 4], F32)
    oT = C.tile([128, 8, 128], BF16)
    h2 = C.tile([128, D], BF16)
    h2T = oT
    K2 = dict(K)
    K2["ntmp"] = C.tile([128, D], F32)
    K2["nss"] = C.tile([128, 1], F32)

    def load(i):
        b = i % 2
        C.dma(xt[b][:], S["xres"][i * 128:(i + 1) * 128, :], [S["xres"].sub(i)], [xt[b]])
        if i >= 2:
            C.dma(rc[b][:], I["rope_c"][(i - 2) * 128:(i - 1) * 128, :], (), [rc[b]])
            C.dma(rs[b][:], I["rope_s"][(i - 2) * 128:(i - 1) * 128, :], (), [rs[b]])

    def qkv(i):
        b = i % 2
        if i == 2:
            load_mod(C, gs1, 1, 0, 1)
            load_mod(C, sh1, 1, 0, 0)
        norm_mod_transpose(C, K, xt[b], gs1, sh1, h_bf, hT)
        yield
        zb = []
        for n in range(3):
            if i < 2 and n < 2:
                zb.append(None)
                continue
            bk = C.bank()
            for c in range(8):
                C.mm(bk[:], hT[:, c, :], wqkv[:, c, n * 512:(n + 1) * 512], c == 0, c == 7, [hT, wqkv], [bk])
            zb.append(bk)
        kvb = zb[2]
        if ATT_LEVEL < 1:
            return
        C.copy(vaug[:, i, :, 0:64], kvb[:, 256:512].rearrange("p (k d) -> p k d", k=4), [kvb], [vaug.sub(i)], eng="act")
        if ATT_LEVEL < 2:
            return
        if i < 2:
            C.copy(qk[:, 16:20, :], kvb[:, 0:256].rearrange("p (k d) -> p k d", k=4), [kvb], [qk], eng="act")
        else:
            srcs = [(zb[0], 512, 0), (zb[1], 512, 8), (kvb, 256, 16)]
            for bkk, w, h0 in srcs:
                nh = w // 64
                zsv = zs[:, h0 * 64:h0 * 64 + w]
                C.copy(zsv, bkk[:, 0:w], [bkk], [zs], eng="act")
                t1v = t1[:, h0:h0 + nh, :].rearrange("p h d -> p (h d)")
                C.tt(t1v, zsv, rc[b][:, 0:w], ALU.mult, [zs, rc[b]], [t1])
                s5 = zsv.rearrange("p (h u a w) -> p h u a w", u=2, a=2, w=16)
                d5 = t2[:, h0:h0 + nh, :].rearrange("p h (u a w) -> p h u a w", u=2, a=2)
                sn = rs[b][:, 0:w].rearrange("p (h u a w) -> p h u a w", u=2, a=2, w=16)
                for a in range(2):
                    C.tt(d5[:, :, :, a, :], s5[:, :, :, 1 - a, :], sn[:, :, :, a, :], ALU.mult, [zs, rs[b]], [t2])
            C.tt(qk[:], t1[:], t2[:], ALU.add, [t1, t2], [qk])
        yield
        if ATT_LEVEL < 3:
            return
        tb = C.bank()
        pb = tb[:].bitcast(BF16)
        for kv in range(4):
            C.tr(pb[0:64, kv * 128:(kv + 1) * 128], qk[:, 16 + kv, :], idb[:], [qk, idb], [tb])
        C.copy(kT[:, :, i * 128:(i + 1) * 128], pb[0:64, 0:512].rearrange("p (k t) -> p k t", k=4), [tb], [kT.sub(i)], eng="act")
        if i >= 2:
            q_ = qT[i % 3]
            for half in range(2):
                tb = C.bank()
                pb = tb[:].bitcast(BF16)
                for hh in range(8):
                    C.tr(pb[0:64, hh * 128:(hh + 1) * 128], qk[:, half * 8 + hh, :], idb[:], [qk, idb], [tb])
                C.copy(q_[:, half * 8:(half + 1) * 8, :], pb[0:64, :].rearrange("p (h t) -> p h t", h=8), [tb], [q_], eng="act")

    def attend(i):
        if ATT_LEVEL < 4:
            return
        b = i % 2
        q_ = qT[i % 3]
        oat = oat2[i % 2]
        oat_flat = oat.view(oat[:].rearrange("p h d -> p (h d)"))
        C.dma(xt2[0][:], S["xres"][i * 128:(i + 1) * 128, :], [S["xres"].sub(i)], [xt2[0]])
        kbl = [(0, None), (1, None)]
        if i - 1 >= 2:
            kbl.append((i - 1, 0))
        kbl.append((i, None))
        if i + 1 < NT_ALL:
            kbl.append((i + 1, 1))
        for kv in range(4):
            pts = []
            for j, (kb, mk) in enumerate(kbl):
                sb_ = C.bank()
                C.mm(sb_[:], kT[:, kv, kb * 128:(kb + 1) * 128], q_[:, 4 * kv:4 * kv + 4, :].rearrange("p h t -> p (h t)"), True, True,
                     [kT.sub(kb), q_], [sb_])
                pt = pT[j]
                C.act(pt[:], sb_[:], AF.Exp, [sb_], [pt], scale=0.125)
                if mk is not None:
                    C.tt(pt[:].rearrange("p (h t) -> p h t", h=4), pt[:].rearrange("p (h t) -> p h t", h=4),
                         bc_mid(masks[:, mk, :], 4), ALU.mult, [pt, masks], [pt])
                pts.append(pt)
            yield
            if ATT_LEVEL < 5:
                continue
            ob_ = C.bank()
            for g in range(4):
                for j, (kb, mk) in enumerate(kbl):
                    C.mm(ob_[:, g * 128:g * 128 + 65], pts[j][:, g * 128:(g + 1) * 128], vaug[:, kb, kv, 0:65], j == 0, j == len(kbl) - 1,
                         [pts[j], vaug.sub(kb)], [ob_])
            ov = ob_[:, :].rearrange("p (g d) -> p g d", g=4)
            C.tt(den[:], ov[:, :, 64], esink[:, 4 * kv:4 * kv + 4], ALU.add, [ob_, esink], [den])
            C.recip(den[:], den[:], [den], [den])
            C.tt(oat[:, 4 * kv:4 * kv + 4, :], ov[:, :, 0:64], bc_last(den[:], 64), ALU.mult, [ob_, den], [oat])
            yield
        if ATT_LEVEL < 6:
            return
        transpose_1024(C, K, oat_flat, oT)
        yield
        yb = []
        for n in range(2):
            bk = C.bank()
            for c in range(8):
                C.mm(bk[:], oT[:, c, :], w_out[:, c, n * 512:(n + 1) * 512], c == 0, c == 7, [oT, w_out], [bk])
            yb.append(bk)
        residual_norm2_router(C, K2, W, i, 1, 0, yb, xt2[0], (g1b, gs2, sh2), (None, h2, h2T))

    load(0)
    for i in range(ATT_TILES):
        if i + 1 < ATT_TILES:
            load(i + 1)
        interleave(qkv(i), attend(i - 2) if i - 2 >= 2 else None)
    if ATT_TILES == NT_ALL:
        interleave(attend(NT_ALL - 2))
        interleave(attend(NT_ALL - 1))


def phase_final(C, K):
    C.new_phase()
    I, S = C.din, C.dscr
    alloc_norm_scratch(C, K)
    gb = C.tile([128, D], F32)
    C.dma(gb[:], I["final_g"][:].partition_broadcast(128), (), [gb])
    xt = [C.tile([128, D], F32) for _ in range(2)]
    yt = [C.tile([128, D], F32) for _ in range(2)]
    out = C.dout

    def load(i):
        C.dma(xt[i % 2][:], S["xres"][(i + 2) * 128:(i + 3) * 128, :], [S["xres"]], [xt[i % 2]])

    load(0)
    for i in range(NLAT // 128):
        if i + 1 < NLAT // 128:
            load(i + 1)
        b = i % 2
        rms_rstd(C, xt[b][:], 128, D, K["ntmp"], K["nss"], [xt[b]])
        C.stt(yt[b][:], xt[b][:], K["nss"][:], gb[:], ALU.mult, ALU.mult, [xt[b], K["nss"], gb], [yt[b]])
        C.dma(out[i * 128:(i + 1) * 128, :], yt[b][:], [yt[b]], [C.dout_buf], eng="pool")


INPUT_SPECS = [
    ("x", [NLAT, D], F32), ("ctx", [NCTX, D], F32), ("c2", [2, D], F32),
    ("mod_w", [2, D, 6 * D], F32), ("mod_b", [2, 6 * D], F32), ("norm1_g", [2, D], F32), ("norm2_g", [2, D], F32),
    ("ab_w_in", [D, 3584], F32), ("ab_w_out", [D, D], F32), ("sgu_w", [4, 128, 128], F32), ("sgu_b", [4, 128], F32),
    ("hgrn_lb_logits", [2, 2, 512], F32), ("hgrn_norm_g", [4, 128], F32),
    ("attn_w_qkv", [D, 1536], F32), ("attn_w_out", [D, D], F32), ("attn_sink", [16], F32),
    ("router_w", [2, D, NE], F32), ("expert_w1", [2, NE, D, FF], F32), ("expert_w3", [2, NE, D, FF], F32),
    ("expert_w2", [2, NE, FF, D], F32), ("final_g", [D], F32),
    ("identb", [128, 128], BF16), ("identf", [128, 128], F32), ("onesb", [128, 128], BF16), ("ustrict", [128, 128], BF16),
    ("gla_m", [64, 2, 64], F32), ("gla_sel", [64, 2, 2], F32), ("gla_tri", [64, 2, 64], F32),
    ("amask", [128, 2, 128], BF16), ("rope_c", [NLAT, 512], F32), ("rope_s", [NLAT, 512], F32),
    ("tvals", [128, NT_ALL, 2], BF16), ("iota", [128, 512], F32),
]


def host_constants():
    bf = ml_dtypes.bfloat16
    c = {}
    c["identb"] = np.eye(128, dtype=np.float32).astype(bf)
    c["identf"] = np.eye(128, dtype=np.float32)
    c["onesb"] = np.ones((128, 128), np.float32).astype(bf)
    pp = np.arange(128)
    c["ustrict"] = (pp[:, None] < pp[None, :]).astype(np.float32).astype(bf)
    s = np.arange(64)[:, None]
    t = np.arange(64)[None, :]
    m = np.zeros((64, 2, 64), np.float32)
    m[:, 0, :] = (s <= t).astype(np.float32) - (s <= 31).astype(np.float32)
    m[:, 1, :] = (s >= t).astype(np.float32) - (s >= 32).astype(np.float32)
    c["gla_m"] = m
    sel = np.zeros((64, 2, 2), np.float32)
    sel[:, 0, 0] = (np.arange(64) <= 31)
    sel[:, 1, 0] = (np.arange(64) >= 32)
    sel[:, :, 1] = 1.0
    c["gla_sel"] = sel
    tri = np.zeros((64, 2, 64), np.float32)
    tri[:, 0, :] = (s <= t)
    tri[:, 1, :] = (s >= t)
    c["gla_tri"] = tri
    j = np.arange(128)[:, None]
    i = np.arange(128)[None, :]
    am = np.zeros((128, 2, 128), np.float32)
    am[:, 0, :] = (i <= j)
    am[:, 1, :] = (j <= i)
    c["amask"] = am.astype(bf)
    tt = np.arange(NLAT)
    inv = (10000.0 ** (-np.arange(16, dtype=np.float32) / 16)).astype(np.float32)
    ar = (tt // 64).astype(np.float32)[:, None] * inv[None, :]
    ac = (tt % 64).astype(np.float32)[:, None] * inv[None, :]
    c["rope_c"] = np.tile(np.concatenate([np.cos(ar), np.cos(ar), np.cos(ac), np.cos(ac)], axis=1).astype(np.float32), (1, 8))
    c["rope_s"] = np.tile(np.concatenate([-np.sin(ar), np.sin(ar), -np.sin(ac), np.sin(ac)], axis=1).astype(np.float32), (1, 8))
    rowid = np.arange(NT_ALL)[None, :] * 128 + np.arange(128)[:, None]
    c["tvals"] = np.stack([rowid // 64, rowid % 64], axis=-1).astype(np.float32).astype(bf)
    c["iota"] = np.broadcast_to(np.arange(512, dtype=np.float32), (128, 512)).copy()
    return c


SCRATCH_SPECS = [
    ("modv", [2, 2, 6 * D], F32), ("xres", [NALL, D], F32), ("hA", [NALL, D], BF16),
    ("ya", [NALL, 512], BF16), ("q", [NALL, 512], BF16), ("kk", [2, NALL, 512], BF16), ("gl", [2, NALL, 512], F32),
    ("v", [NALL, 512], BF16), ("og", [NALL, 512], BF16), ("o", [2, NALL, 512], F32),
]


def build_program(debug=(), stop_after=None, only=None):
    nc = bass.Bass("TRN2", target_bir_lowering=False)
    C = Ctx(nc, debug)
    K = {}
    for name, shape, dt in INPUT_SPECS:
        C.inp(name, shape, dt)
    for name, shape, dt in SCRATCH_SPECS:
        C.scr(name, shape, dt)
    C.dout_buf = Buf(nc.dram_tensor("out", [NLAT, D], F32, kind="ExternalOutput").ap(), "out")
    C.dout = C.dout_buf.t
    load_consts(C, K)
    rt_ctx = alloc_route(C, 32)
    rt_lat = alloc_route(C, 512)
    phases = [
        ("prep", lambda: phase_prep(C, K)),
        ("mixer_in", lambda: phase_mixer_in(C, K)),
        ("gla", lambda: phase_gla(C, K)),
        ("mixer_out", lambda: phase_mixer_out(C, K)),
        ("route_ctx0", lambda: phase_route(C, K, 0, 2, 32, rt_ctx)),
        ("route_lat0", lambda: phase_route(C, K, 2, 32, 512, rt_lat)),
        ("experts_lat0", lambda: phase_experts(C, K, 0, [(0, 512, rt_lat), (1, 32, rt_ctx)])),
        ("attention", lambda: phase_attention(C, K)),
        ("route_lat1", lambda: phase_route(C, K, 2, 32, 512, rt_lat)),
        ("experts_lat1", lambda: phase_experts(C, K, 1, [(0, 512, rt_lat)])),
        ("final", lambda: phase_final(C, K)),
    ]
    for name, fn in phases:
        if only is not None and name not in only:
            continue
        fn()
        if stop_after == name:
            break
    C.p.emit()
    return nc


def make_in_maps(inputs, cores):
    consts = host_constants()
    f = lambda a: np.ascontiguousarray(np.asarray(a, dtype=np.float32))
    shared = {
        "mod_w": f(inputs["mod_w"]), "mod_b": f(inputs["mod_b"]), "norm1_g": f(inputs["norm1_g"]), "norm2_g": f(inputs["norm2_g"]),
        "ab_w_in": f(inputs["ab_w_in"][0]), "ab_w_out": f(inputs["ab_w_out"][0]), "sgu_w": f(inputs["sgu_w"][0]), "sgu_b": f(inputs["sgu_b"][0]),
        "hgrn_lb_logits": f(inputs["hgrn_lb_logits"]), "hgrn_norm_g": f(inputs["hgrn_norm_g"][0]),
        "attn_w_qkv": f(inputs["attn_w_qkv"][0]), "attn_w_out": f(inputs["attn_w_out"][0]), "attn_sink": f(inputs["attn_sink"][0]),
        "router_w": f(inputs["router_w"]), "expert_w1": f(inputs["expert_w1"]), "expert_w3": f(inputs["expert_w3"]),
        "expert_w2": f(inputs["expert_w2"]), "final_g": f(inputs["final_g"]),
    }
    shared.update(consts)
    maps = []
    for b in cores:
        m = dict(shared)
        m["x"] = f(inputs["x"][b])
        m["ctx"] = f(inputs["ctx"][b])
        m["c2"] = np.ascontiguousarray(np.stack([f(inputs["c"][b]), f(inputs["c_ctx"])], axis=0))
        maps.append(m)
    return maps


def kernel(**inputs):
    nc = build_program()
    maps = make_in_maps(inputs, list(range(8)))
    res = run_bass_kernel_spmd(nc, maps, core_ids=list(range(8)))
    return np.stack([np.asarray(r["out"], dtype=np.float32) for r in res.results], axis=0)
```

```python
import contextlib
import numpy as np
import ml_dtypes
import concourse.bass as bass
import concourse.mybir as mybir
from concourse.bass_utils import run_bass_kernel_spmd

F32 = mybir.dt.float32
BF16 = mybir.dt.bfloat16
I32 = mybir.dt.int32
U8 = mybir.dt.uint8
ALU = mybir.AluOpType
AF = mybir.ActivationFunctionType
AX = mybir.AxisListType
ISZ = {F32: 4, BF16: 2, I32: 4}

D = 1024
NLAT = 4096
NCTX = 256
NALL = NLAT + NCTX
NT_ALL = NALL // 128
EPS = 1e-6
NE = 16
FF = 2048

ENGS = ("pe", "act", "dve", "pool", "sp")
N_DMA_SEMS = 32
import os as _os
STRICT_SAME_ENGINE = _os.environ.get('STRICT_SAME_ENGINE', '0') == '1'


class Buf:
    def __init__(self, t, name, root=None):
        self.t = t
        self.name = name
        self.root = root if root is not None else self
        if root is None:
            self.whole = [None, []]
            self.subs = {}

    def __getitem__(self, idx):
        return self.t[idx]

    def sub(self, key):
        return (self.root, key)

    def view(self, ap):
        return Buf(ap, self.name + "_v", root=self.root)


def _nk(k):
    return (k.root, None) if isinstance(k, Buf) else (k[0].root, k[1])


class Prog:
    def __init__(self, nc):
        self.nc = nc
        self.ops = {e: [] for e in ENGS}
        self.dma_count = [0] * N_DMA_SEMS
        self.dma_last = [None] * N_DMA_SEMS
        self.dma_rr = 0
        self.dma_rr_pool = 0
        self.bar = {}

    def _states(self, key):
        buf, sk = _nk(key)
        if sk is None:
            return buf, sk, [buf.whole] + list(buf.subs.values())
        if sk not in buf.subs:
            buf.subs[sk] = [None, []]
        return buf, sk, [buf.whole, buf.subs[sk]]

    def barrier(self):
        toks = set()
        for e in ENGS:
            for i in range(len(self.ops[e]) - 1, -1, -1):
                if self.ops[e][i]["dma"] is None:
                    toks.add(("e", e, i))
                    break
        for t in self.dma_last:
            if t is not None:
                toks.add(t)
        self.bar = {e: set(toks) for e in ENGS}

    def op(self, eng, fn, reads=(), writes=(), dma=False):
        deps = set()
        if self.bar.get(eng):
            deps |= self.bar.pop(eng)
        for k in reads:
            _, _, sts = self._states(k)
            for st in sts:
                if st[0] is not None:
                    deps.add(st[0])
        for k in writes:
            _, _, sts = self._states(k)
            for st in sts:
                if st[0] is not None:
                    deps.add(("W",) + st[0])
                for r in st[1]:
                    deps.add(("W",) + r)
        idx = len(self.ops[eng])
        rec = dict(fn=fn, deps=[], sig=False, dma=None)
        if dma:
            half = N_DMA_SEMS // 2
            if eng == "pool":
                s = half + self.dma_rr_pool % half
                self.dma_rr_pool += 1
            else:
                s = self.dma_rr % half
                self.dma_rr += 1
            prev = self.dma_last[s]
            self.dma_count[s] += 16
            tok = ("d", s, self.dma_count[s])
            rec["dma"] = (s, self.dma_count[s])
            if prev is not None:
                deps.add(prev)
            self.dma_last[s] = tok
        else:
            tok = ("e", eng, idx)
        final = set()
        for d in deps:
            war = d[0] == "W"
            if war:
                d = d[1:]
            if d[0] == "e" and d[1] == eng and not dma and war and (eng == "pe" or not STRICT_SAME_ENGINE):
                continue
            if d != tok:
                final.add(d)
        for d in final:
            if d[0] == "e":
                self.ops[d[1]][d[2]]["sig"] = True
        rec["deps"] = sorted(final, key=str)
        self.ops[eng].append(rec)
        for k in reads:
            buf, sk, _ = self._states(k)
            rl = (buf.whole if sk is None else buf.subs[sk])[1]
            if tok[0] == "e":
                rl[:] = [t for t in rl if not (t[0] == "e" and t[1] == tok[1])]
            else:
                rl[:] = [t for t in rl if not (t[0] == "d" and t[1] == tok[1])]
            rl.append(tok)
        for k in writes:
            buf, sk, _ = self._states(k)
            if sk is None:
                buf.whole = [tok, []]
                buf.subs = {}
            else:
                buf.subs[sk] = [tok, []]
        return tok

    def dma(self, out, in_, reads=(), writes=(), eng="sp", **kw):
        return self.op(eng, lambda e: e.dma_start(out=out, in_=in_, **kw), reads, writes, dma=True)

    def emit(self):
        nc = self.nc
        with contextlib.ExitStack() as es:
            esem = {e: es.enter_context(nc.semaphore(f"s_{e}")) for e in ENGS}
            dsem = [es.enter_context(nc.semaphore(f"s_dma{i}")) for i in range(N_DMA_SEMS)]
            sigcnt = {}
            for e in ENGS:
                c = 0
                for i, r in enumerate(self.ops[e]):
                    if r["sig"] and r["dma"] is None:
                        c += 1
                        sigcnt[(e, i)] = c
            es.enter_context(nc.allow_non_contiguous_dma(reason="small strided loads of parameters"))
            block = es.enter_context(nc.Block())
            final_dma = [(s, self.dma_count[s]) for s in range(N_DMA_SEMS) if self.dma_count[s] > 0]

            def run(engname, eobj):
                waited = {}
                for r in self.ops[engname]:
                    for d in r["deps"]:
                        if d[0] == "e":
                            sem, val, key = esem[d[1]], sigcnt[(d[1], d[2])], ("e", d[1])
                        else:
                            sem, val, key = dsem[d[1]], d[2], ("d", d[1])
                        if waited.get(key, 0) >= val:
                            continue
                        waited[key] = val
                        eobj.wait_ge(sem, val)
                    ins = r["fn"](eobj)
                    if r["dma"] is not None:
                        ins.then_inc(dsem[r["dma"][0]], 16)
                    elif r["sig"]:
                        ins.then_inc(esem[engname], 1)
                if engname == "sp":
                    for s, v in final_dma:
                        eobj.wait_ge(dsem[s], v)

            block.sync(lambda e: run("sp", e))
            block.tensor(lambda e: run("pe", e))
            block.scalar(lambda e: run("act", e))
            block.vector(lambda e: run("dve", e))
            block.gpsimd(lambda e: run("pool", e))


ARENA_BYTES = 200 * 1024


class Ctx:
    def __init__(self, nc, debug=()):
        self.nc = nc
        self.p = Prog(nc)
        self.debug = set(debug)
        self.arena = nc.alloc_sbuf_tensor("arena", [128, ARENA_BYTES], U8)
        self.persist_top = 0
        self.off = 0
        self.cnt = 0
        self.banks = [Buf(nc.alloc_psum_tensor(f"psb{i}", [128, 512], F32)[:], f"psb{i}") for i in range(8)]
        self.bank_rr = 0
        self.din = {}
        self.dscr = {}

    def _carve(self, shape, dt, off):
        nb = int(np.prod(shape[1:])) * ISZ[dt]
        t = self.arena[0:shape[0], off:off + nb].bitcast(dt)
        if len(shape) == 3:
            t = t.rearrange("p (a b) -> p a b", a=shape[1])
        elif len(shape) == 4:
            t = t.rearrange("p (a b c) -> p a b c", a=shape[1], b=shape[2])
        return t, (nb + 63) // 64 * 64

    def tile(self, shape, dt, name=None, persist=False):
        self.cnt += 1
        name = name or f"t{self.cnt}"
        if persist:
            assert self.off == self.persist_top, "persistent tiles must be allocated at phase start"
        t, nb = self._carve(shape, dt, self.off)
        self.off += nb
        assert self.off <= ARENA_BYTES, f"SBUF arena overflow {self.off}"
        if persist:
            self.persist_top = self.off
        return Buf(t, name)

    def new_phase(self):
        self.p.barrier()
        self.off = self.persist_top

    def bank(self):
        b = self.banks[self.bank_rr % 8]
        self.bank_rr += 1
        return b

    def inp(self, name, shape, dt):
        self.din[name] = Buf(self.nc.dram_tensor(name, list(shape), dt, kind="ExternalInput").ap(), name)
        return self.din[name]

    def scr(self, name, shape, dt):
        kind = "ExternalOutput" if name in self.debug else "Internal"
        self.dscr[name] = Buf(self.nc.dram_tensor(name, list(shape), dt, kind=kind).ap(), name)
        return self.dscr[name]

    def act(self, out, in_, func, reads, writes, **kw):
        return self.p.op("act", lambda e: e.activation(out=out, in_=in_, func=func, **kw), reads, writes)

    def tt(self, out, in0, in1, op, reads, writes, eng="dve"):
        return self.p.op(eng, lambda e: e.tensor_tensor(out=out, in0=in0, in1=in1, op=op), reads, writes)

    def ts(self, out, in0, s1, s2, op0, op1, reads, writes, eng="dve"):
        if op1 is None:
            return self.p.op(eng, lambda e: e.tensor_scalar(out=out, in0=in0, scalar1=s1, scalar2=None, op0=op0), reads, writes)
        return self.p.op(eng, lambda e: e.tensor_scalar(out=out, in0=in0, scalar1=s1, scalar2=s2, op0=op0, op1=op1), reads, writes)

    def stt(self, out, in0, scalar, in1, op0, op1, reads, writes, eng="dve"):
        return self.p.op(eng, lambda e: e.scalar_tensor_tensor(out=out, in0=in0, scalar=scalar, in1=in1, op0=op0, op1=op1), reads, writes)

    def copy(self, out, in_, reads, writes, eng="dve"):
        if eng == "act":
            return self.p.op("act", lambda e: e.copy(out=out, in_=in_), reads, writes)
        return self.p.op(eng, lambda e: e.tensor_copy(out=out, in_=in_), reads, writes)

    def mm(self, out, lhsT, rhs, start, stop, reads, writes):
        return self.p.op("pe", lambda e: e.matmul(out, lhsT=lhsT, rhs=rhs, start=start, stop=stop), reads, writes)

    def tr(self, out, in_, ident, reads, writes):
        return self.p.op("pe", lambda e: e.transpose(out=out, in_=in_, identity=ident), reads, writes)

    def reduce(self, out, in_, op, reads, writes):
        return self.p.op("dve", lambda e: e.tensor_reduce(out=out, in_=in_, axis=AX.X, op=op), reads, writes)

    def recip(self, out, in_, reads, writes):
        return self.p.op("dve", lambda e: e.reciprocal(out=out, in_=in_), reads, writes)

    def memset(self, ap, val, writes, eng="pool"):
        return self.p.op(eng, lambda e: e.memset(ap, val), (), writes)

    def dma(self, out, in_, reads=(), writes=(), eng="sp", **kw):
        return self.p.dma(out, in_, reads, writes, eng, **kw)


def interleave(*gens):
    gens = [g for g in gens if g is not None]
    while gens:
        for g in list(gens):
            try:
                next(g)
            except StopIteration:
                gens.remove(g)


def bc_mid(ap2d, n):
    P, Fd = ap2d.shape
    return ap2d.unsqueeze(1).to_broadcast([P, n, Fd])


def bc_last(ap2d, n):
    P, A = ap2d.shape
    return ap2d.unsqueeze(2).to_broadcast([P, A, n])


def rms_rstd(C, x, npart, width, tmp, ss, reads):
    C.act(tmp[0:npart, 0:width], x, AF.Square, reads, [tmp, ss], accum_out=ss[0:npart, :])
    C.ts(ss[0:npart, :], ss[0:npart, :], 1.0 / width, EPS, ALU.mult, ALU.add, [ss], [ss])
    C.act(ss[0:npart, :], ss[0:npart, :], AF.Sqrt, [ss], [ss])
    C.recip(ss[0:npart, :], ss[0:npart, :], [ss], [ss])


def norm_mod_transpose(C, K, xt, gs, sh, h_bf, hT, want_T=True):
    tmp, ss = K["ntmp"], K["nss"]
    rms_rstd(C, xt[:], 128, D, tmp, ss, [xt])
    C.stt(tmp[:], xt[:], ss[:], gs[:], ALU.mult, ALU.mult, [xt, ss, gs, tmp], [tmp])
    C.tt(h_bf[:], tmp[:], sh[:], ALU.add, [tmp, sh], [h_bf])
    if want_T:
        transpose_1024(C, K, h_bf, hT)


def transpose_1024(C, K, src_bf, dstT, npart=128, col0=0):
    bk = C.bank()
    pb = bk[:].bitcast(BF16)
    for c in range(8):
        C.tr(pb[:, c * 128:c * 128 + npart], src_bf[0:npart, c * 128:(c + 1) * 128], K["identb"][0:npart, 0:npart],
             [src_bf, K["identb"]], [bk])
    src = pb.rearrange("p (c t) -> p c t", c=8)[:, :, 0:npart]
    C.copy(dstT[:, :, col0:col0 + npart], src, [bk], [dstT], eng="act")


def load_w_bf16(C, dst, w_ap, ncols, nchunks, col0=0, step=512):
    for n0 in range(0, ncols, step):
        n1 = min(ncols, n0 + step)
        C.dma(dst[:, :, col0 + n0:col0 + n1], w_ap[:, n0:n1].rearrange("(c p) n -> p c n", p=128), (), [dst], eng="pool")


def phase_prep(C, K):
    C.new_phase()
    I = C.din
    modv = C.dscr["modv"]
    cs = C.tile([128, 8, 2], F32)
    c2s = C.tile([2, D], F32)
    C.dma(c2s[:], I["c2"][:], (), [c2s])
    C.act(c2s[:], c2s[:], AF.Silu, [c2s], [c2s])
    bk = C.bank()
    for c in range(8):
        C.tr(bk[:, 2 * c:2 * c + 2], c2s[:, c * 128:(c + 1) * 128], K["identf"][0:2, 0:2], [c2s, K["identf"]], [bk])
    C.copy(cs[:].rearrange("p c r -> p (c r)"), bk[:, 0:16], [bk], [cs])
    wbuf = [C.tile([128, 8, 512], F32) for _ in range(2)]
    mv = C.tile([2, 6 * D], F32)
    mb = C.tile([2, 6 * D], F32)
    ng = C.tile([2, D], F32)
    k = 0
    for l in range(2):
        C.dma(mb[:], I["mod_b"][l, :].partition_broadcast(2), (), [mb])
        for n in range(12):
            w = wbuf[k % 2]
            k += 1
            C.dma(w[:], I["mod_w"][l, :, n * 512:(n + 1) * 512].rearrange("(c p) n -> p c n", p=128), (), [w])
            bk = C.bank()
            for c in range(8):
                C.mm(bk[0:2, :], cs[:, c, :], w[:, c, :], c == 0, c == 7, [cs, w], [bk])
            C.tt(mv[:, n * 512:(n + 1) * 512], bk[0:2, :], mb[:, n * 512:(n + 1) * 512], ALU.add, [bk, mb], [mv])
        for slot, gname in ((1, "norm1_g"), (4, "norm2_g")):
            C.dma(ng[:], I[gname][l, :].partition_broadcast(2), (), [ng])
            C.stt(mv[:, slot * D:(slot + 1) * D], mv[:, slot * D:(slot + 1) * D], 1.0, ng[:], ALU.add, ALU.mult, [mv, ng], [mv])
        C.dma(modv[l, :, :], mv[:], [mv], [modv])


def load_mod(C, dst, l, r, slot):
    C.dma(dst[:], C.dscr["modv"][l, r, slot * D:(slot + 1) * D].partition_broadcast(128), [C.dscr["modv"]], [dst])


def alloc_norm_scratch(C, K):
    K["ntmp"] = C.tile([128, D], F32)
    K["nss"] = C.tile([128, 1], F32)


def load_consts(C, K):
    I = C.din
    for name, shape, dt in (("identb", [128, 128], BF16), ("identf", [128, 128], F32), ("onesb", [128, 128], BF16),
                            ("ustrict", [128, 128], BF16)):
        K[name] = C.tile(shape, dt, name, persist=True)
        C.dma(K[name][:], I[name][:], (), [K[name]])
    K["logits"] = C.tile([128, NT_ALL, NE], F32, "logits", persist=True)


def phase_mixer_in(C, K):
    C.new_phase()
    I, S = C.din, C.dscr
    Kp = []
    for _ in range(2):
        kk_ = dict(K)
        alloc_norm_scratch(C, kk_)
        Kp.append(kk_)
    w_in = C.tile([128, 8, 3584], BF16)
    load_w_bf16(C, w_in, I["ab_w_in"][:], 3584, 8)
    ws = C.tile([128, 4, 128], F32)
    wsT = C.tile([128, 4, 128], BF16)
    C.dma(ws[:], I["sgu_w"][:].rearrange("g t s -> t g s"), (), [ws])
    bk = C.bank()
    for g in range(4):
        C.tr(bk[:, g * 128:(g + 1) * 128], ws[:, g, :], K["identf"][:], [ws, K["identf"]], [bk])
    C.copy(wsT[:].rearrange("p g t -> p (g t)"), bk[:], [bk], [wsT])
    bsT = C.tile([128, 4], F32)
    C.dma(bsT[:], I["sgu_b"][:].rearrange("g t -> t g"), (), [bsT])
    lbl = C.tile([128, 2, 2, 512], F32)
    C.dma(lbl[:].rearrange("p a b c -> p (a b c)"), I["hgrn_lb_logits"][:].rearrange("a b c -> (a b c)").partition_broadcast(128), (), [lbl])
    lb = C.tile([128, 2, 512], F32)
    oml = C.tile([128, 2, 512], F32)
    C.tt(lb[:], lbl[:, 0, :, :], lbl[:, 1, :, :], ALU.subtract, [lbl], [lb])
    C.act(lb[:], lb[:], AF.Sigmoid, [lb], [lb])
    C.ts(oml[:], lb[:], -1.0, 1.0, ALU.mult, ALU.add, [lb], [oml])
    mods = {}
    for r in range(2):
        gs = C.tile([128, D], F32)
        sh = C.tile([128, D], F32)
        load_mod(C, gs, 0, r, 1)
        load_mod(C, sh, 0, r, 0)
        mods[r] = (gs, sh)
    xt = [C.tile([128, D], F32) for _ in range(4)]
    h_bf2 = [C.tile([128, D], BF16) for _ in range(2)]
    hT2 = [C.tile([128, 8, 128], BF16) for _ in range(2)]
    u2 = [C.tile([128, 4, 128], F32) for _ in range(2)]
    v2 = [C.tile([128, 4, 128], F32) for _ in range(2)]
    sq2 = [C.tile([128, 4, 128], F32) for _ in range(2)]
    st42 = [C.tile([128, 4], F32) for _ in range(2)]
    st4b2 = [C.tile([128, 4], F32) for _ in range(2)]
    vn2 = [C.tile([128, 4, 128], BF16) for _ in range(2)]
    f32t4 = [C.tile([128, 512], F32) for _ in range(4)]
    ya = [C.tile([128, 512], BF16) for _ in range(2)]
    qo = [C.tile([128, 512], BF16) for _ in range(2)]
    kk = [C.tile([128, 512], BF16) for _ in range(4)]
    gl = [C.tile([128, 512], F32) for _ in range(4)]
    vo = [C.tile([128, 512], BF16) for _ in range(2)]
    og = [C.tile([128, 512], BF16) for _ in range(2)]

    def src_rows(i):
        if i < 2:
            return I["ctx"][i * 128:(i + 1) * 128, :]
        return I["x"][(i - 2) * 128:(i - 1) * 128, :]

    def load(i):
        C.dma(xt[i % 4][:], src_rows(i), (), [xt[i % 4]])

    def tile_body(i):
        b = i % 2
        r = 1 if i < 2 else 0
        rows = slice(i * 128, (i + 1) * 128)
        h_bf, hT, u, v, sq, st4, st4b, vn = h_bf2[b], hT2[b], u2[b], v2[b], sq2[b], st42[b], st4b2[b], vn2[b]
        norm_mod_transpose(C, Kp[b], xt[i % 4], mods[r][0], mods[r][1], h_bf, hT)
        yield
        zb = []
        for n in range(7):
            bk = C.bank()
            for c in range(8):
                C.mm(bk[:], hT[:, c, :], w_in[:, c, n * 512:(n + 1) * 512], c == 0, c == 7, [hT, w_in], [bk])
            zb.append(bk)
        C.act(u[:].rearrange("p g c -> p (g c)"), zb[0][:], AF.Gelu, [zb[0]], [u])
        C.act(v[:].rearrange("p g c -> p (g c)"), zb[1][:], AF.Gelu, [zb[1]], [v])
        C.reduce(st4[:], v[:], ALU.add, [v], [st4])
        C.ts(st4[:], st4[:], 1.0 / 128, None, ALU.mult, None, [st4], [st4])
        C.tt(v[:], v[:], bc_last(st4[:], 128), ALU.subtract, [v, st4], [v])
        C.act(sq[:], v[:], AF.Square, [v], [sq])
        C.reduce(st4b[:], sq[:], ALU.add, [sq], [st4b])
        C.ts(st4b[:], st4b[:], 1.0 / 128, EPS, ALU.mult, ALU.add, [st4b], [st4b])
        C.act(st4b[:], st4b[:], AF.Sqrt, [st4b], [st4b])
        C.recip(st4b[:], st4b[:], [st4b], [st4b])
        C.tt(vn[:], v[:], bc_last(st4b[:], 128), ALU.mult, [v, st4b], [vn])
        bk = C.bank()
        for g in range(4):
            C.mm(bk[:, g * 128:(g + 1) * 128], wsT[:, g, :], vn[:, g, :], True, True, [wsT, vn], [bk])
        for g in range(4):
            C.stt(ya[b][:, g * 128:(g + 1) * 128], bk[:, g * 128:(g + 1) * 128], bsT[:, g:g + 1], u[:, g, :],
                  ALU.add, ALU.mult, [bk, bsT, u], [ya[b]])
        C.dma(S["ya"][rows, :], ya[b][:], [ya[b]], [S["ya"].sub(i)], eng="pool")
        C.act(qo[b][:], zb[2][:], AF.Silu, [zb[2]], [qo[b]])
        C.dma(S["q"][rows, :], qo[b][:], [qo[b]], [S["q"].sub(i)], eng="pool")
        for d in range(2):
            kb_, gb_ = kk[2 * b + d], gl[2 * b + d]
            f32t = f32t4[2 * b + d]
            C.act(f32t[:], zb[3 + d][:], AF.Sigmoid, [zb[3 + d]], [f32t])
            C.tt(f32t[:], f32t[:], oml[:, d, :], ALU.mult, [f32t, oml], [f32t])
            C.tt(f32t[:], f32t[:], lb[:, d, :], ALU.add, [f32t, lb], [f32t])
            C.ts(kb_[:], f32t[:], -1.0, 1.0, ALU.mult, ALU.add, [f32t], [kb_])
            C.act(gb_[:], f32t[:], AF.Ln, [f32t], [gb_])
            C.dma(S["kk"][d, rows, :], kb_[:], [kb_], [S["kk"].sub((d, i))], eng="pool")
            C.dma(S["gl"][d, rows, :], gb_[:], [gb_], [S["gl"].sub((d, i))], eng="pool")
        C.copy(vo[b][:], zb[5][:], [zb[5]], [vo[b]], eng="act")
        C.dma(S["v"][rows, :], vo[b][:], [vo[b]], [S["v"].sub(i)], eng="pool")
        C.act(og[b][:], zb[6][:], AF.Silu, [zb[6]], [og[b]])
        C.dma(S["og"][rows, :], og[b][:], [og[b]], [S["og"].sub(i)], eng="pool")

    load(0)
    load(1)
    for i in range(0, NT_ALL, 2):
        for j in (i + 2, i + 3):
            if j < NT_ALL:
                load(j)
        interleave(tile_body(i), tile_body(i + 1))


def phase_gla(C, K):
    C.new_phase()
    I, S = C.din, C.dscr
    NCH = NALL // 64
    cst = {}
    for name, shape, dt in (("gla_m", [64, 2, 64], F32), ("gla_sel", [64, 2, 2], F32), ("gla_tri", [64, 2, 64], F32)):
        cst[name] = C.tile(shape, dt)
        C.dma(cst[name][:], I[name][:], (), [cst[name]])
    St = [C.tile([128, 4, 128], F32) for _ in range(2)]
    for d in range(2):
        C.memset(St[d][:], 0.0, [St[d]])
    NB = 2
    bufs = {}
    for d in range(2):
        for j in range(NB):
            bufs[(d, j)] = dict(
                q=C.tile([64, 512], BF16), kk=C.tile([64, 512], BF16), g=C.tile([64, 512], F32), v=C.tile([64, 512], BF16),
                ep=C.tile([64, 512], F32), em=C.tile([64, 512], F32), qb=C.tile([64, 512], BF16), kb=C.tile([64, 512], BF16),
                qkT=C.tile([128, 512], BF16), e3=C.tile([128, 4, 3], F32), ex=C.tile([128, 4, 3], F32),
                at=C.tile([64, 4, 64], BF16), ssc=C.tile([128, 4, 128], BF16), osb=C.tile([64, 512], F32),
                tmp=C.tile([128, 4, 128], F32))
    order = {0: list(range(NCH)), 1: [3, 2, 1, 0] + list(range(NCH - 1, 3, -1))}

    def load(d, step):
        ci = order[d][step]
        B = bufs[(d, step % NB)]
        rows = slice(ci * 64, (ci + 1) * 64)
        t = ci // 2
        C.dma(B["q"][:], S["q"][rows, :], [S["q"].sub(t)], [B["q"]])
        C.dma(B["kk"][:], S["kk"][d, rows, :], [S["kk"].sub((d, t))], [B["kk"]])
        C.dma(B["g"][:], S["gl"][d, rows, :], [S["gl"].sub((d, t))], [B["g"]])
        C.dma(B["v"][:], S["v"][rows, :], [S["v"].sub(t)], [B["v"]])

    def compute(d, step):
        ci = order[d][step]
        B = bufs[(d, step % NB)]
        rows = slice(ci * 64, (ci + 1) * 64)
        Sd = St[d]
        idb = K["identb"]
        bps = C.bank()
        C.mm(bps[0:64, :], cst["gla_m"][:, d, :], B["g"][:], True, True, [cst["gla_m"], B["g"]], [bps])
        C.act(B["ep"][:], bps[0:64, :], AF.Exp, [bps], [B["ep"]])
        C.act(B["em"][:], bps[0:64, :], AF.Exp, [bps], [B["em"]], scale=-1.0)
        C.tt(B["qb"][:], B["q"][:], B["ep"][:], ALU.mult, [B["q"], B["ep"]], [B["qb"]])
        C.tt(B["kb"][:], B["kk"][:], B["em"][:], ALU.mult, [B["kk"], B["em"]], [B["kb"]])
        yield
        tb = C.bank()
        pb = tb[:].bitcast(BF16)
        for h in range(4):
            C.tr(pb[:, h * 64:(h + 1) * 64], B["qb"][:, h * 128:(h + 1) * 128], idb[0:64, 0:64], [B["qb"], idb], [tb])
        for h in range(4):
            C.tr(pb[:, 256 + h * 64:256 + (h + 1) * 64], B["kb"][:, h * 128:(h + 1) * 128], idb[0:64, 0:64], [B["kb"], idb], [tb])
        C.copy(B["qkT"][:], pb[:, 0:512], [tb], [B["qkT"]], eng="act")
        yield
        sps = C.bank()
        for h in range(4):
            C.mm(sps[:, h * 2:h * 2 + 2], B["g"][:, h * 128:(h + 1) * 128], cst["gla_sel"][:, d, :], True, True,
                 [B["g"], cst["gla_sel"]], [sps])
        spv = sps[:, 0:8].rearrange("p (h c) -> p h c", c=2)
        C.copy(B["e3"][:, :, 0:2], spv, [sps], [B["e3"]])
        C.tt(B["e3"][:, :, 2], B["e3"][:, :, 1], B["e3"][:, :, 0], ALU.subtract, [B["e3"]], [B["e3"]])
        C.act(B["ex"][:], B["e3"][:], AF.Exp, [B["e3"]], [B["ex"]])
        yield
        aps = C.bank()
        for h in range(4):
            C.mm(aps[0:64, h * 64:(h + 1) * 64], B["qkT"][:, 256 + h * 64:256 + (h + 1) * 64], B["qkT"][:, h * 64:(h + 1) * 64],
                 True, True, [B["qkT"]], [aps])
        C.tt(B["at"][:], aps[0:64, 0:256].rearrange("p (h t) -> p h t", h=4), bc_mid(cst["gla_tri"][:, d, :], 4), ALU.mult,
             [aps, cst["gla_tri"]], [B["at"]])
        yield
        C.tt(B["ssc"][:], Sd[:], bc_last(B["ex"][:, :, 0], 128), ALU.mult, [Sd, B["ex"]], [B["ssc"]], eng="pool")
        ops_ = C.bank()
        for h in range(4):
            C.mm(ops_[0:64, h * 128:(h + 1) * 128], B["at"][:, h, :], B["v"][:, h * 128:(h + 1) * 128], True, False,
                 [B["at"], B["v"]], [ops_])
            C.mm(ops_[0:64, h * 128:(h + 1) * 128], B["qkT"][:, h * 64:(h + 1) * 64], B["ssc"][:, h, :], False, True,
                 [B["qkT"], B["ssc"]], [ops_])
        C.copy(B["osb"][:], ops_[0:64, :], [ops_], [B["osb"]], eng="act")
        C.dma(S["o"][d, rows, :], B["osb"][:], [B["osb"]], [S["o"].sub((d, ci))], eng="pool")
        yield
        dps = C.bank()
        for h in range(4):
            C.mm(dps[:, h * 128:(h + 1) * 128], B["kb"][:, h * 128:(h + 1) * 128], B["v"][:, h * 128:(h + 1) * 128], True, True,
                 [B["kb"], B["v"]], [dps])
        C.tt(B["tmp"][:], dps[:].rearrange("p (h v) -> p h v", h=4), bc_last(B["ex"][:, :, 2], 128), ALU.mult, [dps, B["ex"]], [B["tmp"]])
        C.tt(Sd[:], Sd[:], bc_last(B["ex"][:, :, 1], 128), ALU.mult, [Sd, B["ex"]], [Sd], eng="pool")
        C.tt(Sd[:], Sd[:], B["tmp"][:], ALU.add, [Sd, B["tmp"]], [Sd], eng="pool")

    for d in range(2):
        load(d, 0)
    for step in range(NCH):
        for d in range(2):
            if step + 1 < NCH:
                load(d, step + 1)
        interleave(compute(0, step), compute(1, step))


def residual_norm2_router(C, K, W, i, l, r, ybanks, xt, mods2, bufs):
    S = C.dscr
    g1b, gs2, sh2 = mods2
    x1, h2, h2T = bufs
    rows = slice(i * 128, (i + 1) * 128)
    if x1 is None:
        tmp = K["ntmp"]
        for n in range(2):
            C.tt(tmp[:, n * 512:(n + 1) * 512], ybanks[n][:], g1b[:, n * 512:(n + 1) * 512], ALU.mult, [ybanks[n], g1b], [tmp])
        C.tt(xt[:], tmp[:], xt[:], ALU.add, [tmp, xt], [xt])
        x1 = xt
    else:
        for n in range(2):
            C.tt(x1[:, n * 512:(n + 1) * 512], ybanks[n][:], g1b[:, n * 512:(n + 1) * 512], ALU.mult, [ybanks[n], g1b], [x1])
        C.tt(x1[:], x1[:], xt[:], ALU.add, [x1, xt], [x1])
    C.dma(S["xres"][rows, :], x1[:], [x1], [S["xres"].sub(i)], eng="pool")
    norm_mod_transpose(C, K, x1, gs2, sh2, h2, h2T)
    C.dma(S["hA"][rows, :], h2[:], [h2], [S["hA"].sub(i)], eng="pool")
    bk = C.bank()
    for c in range(8):
        C.mm(bk[:, 0:NE], h2T[:, c, :], W["wr"][:, c, :], c == 0, c == 7, [h2T, W["wr"]], [bk])
    C.copy(K["logits"][:, i, :], bk[:, 0:NE], [bk], [K["logits"].sub(i)])


def load_router_w(C, l):
    wr = C.tile([128, 8, NE], BF16)
    C.dma(wr[:], C.din["router_w"][l, :, :].rearrange("(c p) n -> p c n", p=128), (), [wr], eng="pool")
    return wr


def phase_mixer_out(C, K):
    C.new_phase()
    I, S = C.din, C.dscr
    Kp = []
    for _ in range(2):
        kk_ = dict(K)
        alloc_norm_scratch(C, kk_)
        Kp.append(kk_)
    w_out = C.tile([128, 8, D], BF16)
    load_w_bf16(C, w_out, I["ab_w_out"][:], D, 8)
    W = dict(wr=load_router_w(C, 0))
    ngb = C.tile([128, 512], F32)
    C.dma(ngb[:], I["hgrn_norm_g"][:].rearrange("h v -> (h v)").partition_broadcast(128), (), [ngb])
    mods2 = {}
    for r in range(2):
        g1b, gs2, sh2 = C.tile([128, D], F32), C.tile([128, D], F32), C.tile([128, D], F32)
        load_mod(C, g1b, 0, r, 2)
        load_mod(C, gs2, 0, r, 4)
        load_mod(C, sh2, 0, r, 3)
        mods2[r] = (g1b, gs2, sh2)
    NB = 4
    of = [C.tile([128, 4, 128], F32) for _ in range(NB)]
    ob = [C.tile([128, 4, 128], F32) for _ in range(NB)]
    ogt = [C.tile([128, 512], BF16) for _ in range(NB)]
    ycat = [C.tile([128, D], BF16) for _ in range(NB)]
    xt = [C.tile([128, D], F32) for _ in range(NB)]
    sq2 = [C.tile([128, 4, 128], F32) for _ in range(2)]
    st42 = [C.tile([128, 4], F32) for _ in range(2)]
    ycT2 = [C.tile([128, 8, 128], BF16) for _ in range(2)]
    h22 = [C.tile([128, D], BF16) for _ in range(2)]
    h2T2 = [C.tile([128, 8, 128], BF16) for _ in range(2)]

    def load(i):
        b = i % NB
        rows = slice(i * 128, (i + 1) * 128)
        C.dma(of[b][:].rearrange("p h v -> p (h v)"), S["o"][0, rows, :], [S["o"]], [of[b]])
        C.dma(ob[b][:].rearrange("p h v -> p (h v)"), S["o"][1, rows, :], [S["o"]], [ob[b]])
        C.dma(ogt[b][:], S["og"][rows, :], [S["og"]], [ogt[b]])
        C.dma(ycat[b][:, 0:512], S["ya"][rows, :], [S["ya"]], [ycat[b]])
        src = I["ctx"][i * 128:(i + 1) * 128, :] if i < 2 else I["x"][(i - 2) * 128:(i - 1) * 128, :]
        C.dma(xt[b][:], src, (), [xt[b]])

    def tile_body(i):
        b = i % NB
        pb_ = i % 2
        sq, st4, ycT, h2, h2T = sq2[pb_], st42[pb_], ycT2[pb_], h22[pb_], h2T2[pb_]
        r = 1 if i < 2 else 0
        o = of[b]
        C.tt(o[:], o[:], ob[b][:], ALU.add, [o, ob[b]], [o])
        C.act(sq[:], o[:], AF.Square, [o], [sq])
        C.reduce(st4[:], sq[:], ALU.add, [sq], [st4])
        C.ts(st4[:], st4[:], 1.0 / 128, EPS, ALU.mult, ALU.add, [st4], [st4])
        C.act(st4[:], st4[:], AF.Sqrt, [st4], [st4])
        C.recip(st4[:], st4[:], [st4], [st4])
        C.tt(o[:], o[:], bc_last(st4[:], 128), ALU.mult, [o, st4], [o])
        of2 = o[:].rearrange("p h v -> p (h v)")
        C.tt(of2, of2, ngb[:], ALU.mult, [o, ngb], [o])
        C.tt(ycat[b][:, 512:1024], of2, ogt[b][:], ALU.mult, [o, ogt[b], ycat[b]], [ycat[b]])
        yield
        transpose_1024(C, K, ycat[b], ycT)
        yield
        yb = []
        for n in range(2):
            bk = C.bank()
            for c in range(8):
                C.mm(bk[:], ycT[:, c, :], w_out[:, c, n * 512:(n + 1) * 512], c == 0, c == 7, [ycT, w_out], [bk])
            yb.append(bk)
        residual_norm2_router(C, Kp[pb_], W, i, 0, r, yb, xt[b], mods2[r], (None, h2, h2T))

    load(0)
    load(1)
    for i in range(0, NT_ALL, 2):
        for j in (i + 2, i + 3):
            if j < NT_ALL:
                load(j)
        interleave(tile_body(i), tile_body(i + 1))


def phase_route(C, K, tile0, ntl, cap, rt):
    C.new_phase()
    I = C.din
    lg = K["logits"][:, tile0:tile0 + ntl, :]
    LR = [K["logits"]]
    NF = ntl * NE
    aff = C.tile([128, ntl, NE], F32)
    t2 = C.tile([128, ntl], F32)
    C.reduce(t2[:], lg, ALU.max, LR, [t2])
    C.tt(aff[:], lg, bc_last(t2[:], NE), ALU.subtract, LR + [t2], [aff])
    C.act(aff[:], aff[:], AF.Exp, [aff], [aff])
    C.reduce(t2[:], aff[:], ALU.add, [aff], [t2])
    C.recip(t2[:], t2[:], [t2], [t2])
    C.tt(aff[:], aff[:], bc_last(t2[:], NE), ALU.mult, [aff, t2], [aff])
    lo, hi, mid = C.tile([128, NE], F32), C.tile([128, NE], F32), C.tile([128, NE], F32)
    cnt, ge, dlt = C.tile([128, NE], F32), C.tile([128, NE], F32), C.tile([128, NE], F32)
    cmpb = C.tile([128, ntl, NE], BF16)
    C.memset(lo[:], 0.0, [lo])
    C.memset(hi[:], 1.0, [hi])
    C.memset(mid[:], 0.5, [mid])
    onesb = K["onesb"]
    for it in range(30):
        C.tt(cmpb[:], aff[:], bc_mid(mid[:], ntl), ALU.is_gt, [aff, mid], [cmpb])
        bk = C.bank()
        C.mm(bk[:, 0:NF], onesb[:], cmpb[:].rearrange("p i e -> p (i e)"), True, True, [onesb, cmpb], [bk])
        C.reduce(cnt[:], bk[:, 0:NF].rearrange("p (i e) -> p e i", e=NE), ALU.add, [bk], [cnt])
        C.ts(ge[:], cnt[:], float(cap), None, ALU.is_ge, None, [cnt], [ge])
        C.tt(dlt[:], mid[:], lo[:], ALU.subtract, [mid, lo], [dlt])
        C.tt(dlt[:], dlt[:], ge[:], ALU.mult, [dlt, ge], [dlt])
        C.tt(lo[:], lo[:], dlt[:], ALU.add, [lo, dlt], [lo])
        C.tt(dlt[:], hi[:], mid[:], ALU.subtract, [hi, mid], [dlt])
        C.tt(dlt[:], dlt[:], ge[:], ALU.mult, [dlt, ge], [dlt])
        C.tt(hi[:], mid[:], dlt[:], ALU.add, [mid, dlt], [hi])
        C.tt(mid[:], lo[:], hi[:], ALU.add, [lo, hi], [mid])
        C.ts(mid[:], mid[:], 0.5, None, ALU.mult, None, [mid], [mid])
    Ae = C.tile([128, NE, ntl], F32)
    inc = C.tile([128, NE, ntl], F32)
    rmask = C.tile([128, NE, ntl], F32)
    Aeb = C.tile([128, NE, ntl], BF16)
    excb = C.tile([128, NE, ntl], BF16)
    posm = C.tile([128, NE, ntl], F32)
    C.tt(Ae[:], aff[:].rearrange("p i e -> p e i"), bc_last(lo[:], ntl), ALU.is_gt, [aff, lo], [Ae])
    C.memset(rmask[:], 1.0, [rmask])
    C.memset(rmask[:, :, 0:1], 0.0, [rmask])
    flat = lambda t: t[:].rearrange("p e i -> p (e i)")
    C.p.op("dve", lambda e: e.tensor_tensor_scan(out=flat(inc), data0=flat(rmask), data1=flat(Ae), initial=0.0,
                                                 op0=ALU.mult, op1=ALU.add), [rmask, Ae], [inc])
    C.tt(inc[:], inc[:], Ae[:], ALU.subtract, [inc, Ae], [inc])
    C.copy(Aeb[:], Ae[:], [Ae], [Aeb])
    C.copy(excb[:], inc[:], [inc], [excb])
    bk = C.bank()
    C.mm(bk[:, 0:NF], K["ustrict"][:], flat(Aeb), True, False, [K["ustrict"], Aeb], [bk])
    C.mm(bk[:, 0:NF], onesb[:], flat(excb), False, True, [onesb, excb], [bk])
    C.stt(flat(posm), bk[:, 0:NF], 1.0, flat(Ae), ALU.add, ALU.mult, [bk, Ae], [posm])
    C.ts(posm[:], posm[:], -1.0, None, ALU.add, None, [posm], [posm])
    vals = C.tile([128, ntl, NE, 4], BF16)
    tv = C.tile([128, NT_ALL, 2], BF16)
    C.dma(tv[:], I["tvals"][:], (), [tv])
    affb = C.tile([128, ntl, NE], BF16)
    afr = C.tile([128, ntl, NE], F32)
    C.copy(affb[:], aff[:], [aff], [affb])
    C.tt(afr[:], aff[:], affb[:], ALU.subtract, [aff, affb], [afr])
    for j in range(2):
        C.copy(vals[:, :, :, j], bc_last(tv[:, tile0:tile0 + ntl, j], NE), [tv], [vals])
    C.copy(vals[:, :, :, 2], affb[:], [affb], [vals])
    C.copy(vals[:, :, :, 3], afr[:], [afr], [vals])
    iota = C.tile([128, 512], F32)
    C.dma(iota[:], I["iota"][:], (), [iota])
    rows = min(cap, 128)
    njt = cap // rows
    Pm = [C.tile([128, cap], BF16) for _ in range(4)]
    Rsb = [C.tile([4, cap], F32) for _ in range(2)]
    tq = C.tile([128, 4], F32)
    idf = C.tile([128, 1], F32)
    k = 0
    for e in range(NE):
        rb = C.bank()
        for i in range(ntl):
            Pt = Pm[k % 4]
            k += 1
            C.ts(Pt[:], iota[:, 0:cap], posm[:, e, i:i + 1], None, ALU.is_equal, None, [iota, posm], [Pt])
            C.mm(rb[0:4, 0:cap], vals[:, i, e, :], Pt[:], i == 0, i == ntl - 1, [vals, Pt], [rb])
        R = Rsb[e % 2]
        C.copy(R[:], rb[0:4, 0:cap], [rb], [R], eng="act")
        for jt in range(njt):
            tb = C.bank()
            C.tr(tb[0:rows, 0:4], R[:, jt * rows:(jt + 1) * rows], K["identf"][0:4, 0:4], [R, K["identf"]], [tb])
            C.copy(tq[0:rows, :], tb[0:rows, 0:4], [tb], [tq])
            C.stt(idf[0:rows, :], tq[0:rows, 0:1], 64.0, tq[0:rows, 1:2], ALU.mult, ALU.add, [tq], [idf])
            C.copy(rt["idx"][0:rows, e, jt:jt + 1], idf[0:rows, :], [idf], [rt["idx"]])
            C.tt(rt["gate"][0:rows, e, jt:jt + 1], tq[0:rows, 2:3], tq[0:rows, 3:4], ALU.add, [tq], [rt["gate"]])


def phase_experts(C, K, l, groups):
    C.new_phase()
    I, S = C.din, C.dscr
    ttiles, segs = [], []
    col = 0
    for r, cap, rt in groups:
        g2b = C.tile([128, D], F32)
        load_mod(C, g2b, l, r, 5)
        rows = min(cap, 128)
        for jt in range(cap // rows):
            ttiles.append((rt, jt, rows, col + jt * rows, g2b))
        segs.append((col, cap))
        col += cap
    NTOK = col
    ntt = len(ttiles)
    NBA, NBY = 3, 2
    W1 = [C.tile([128, 8, 512], BF16) for _ in range(NBA)]
    W3 = [C.tile([128, 8, 512], BF16) for _ in range(NBA)]
    W2 = [C.tile([128, 16, 512], BF16) for _ in range(NBY)]
    xe = [C.tile([128, D], BF16) for _ in range(2)]
    xeT = [C.tile([128, 8, NTOK], BF16) for _ in range(2)]
    gT = [C.tile([128, 16, NTOK], BF16) for _ in range(2)]
    sa = [C.tile([128, NTOK], BF16) for _ in range(2)]
    yo = [C.tile([128, ntt, D], F32) for _ in range(2)]
    pieces = []
    for e in range(NE):
        pieces += [(e, "a", fq) for fq in range(4)] + [(e, "y", dh) for dh in range(2)]
    cnt = {"a": 0, "y": 0}
    slot = {}

    def load_piece(pi):
        e, kind, j = pieces[pi]
        if kind == "a":
            s_ = cnt["a"] % NBA
            cnt["a"] += 1
            slot[pi] = s_
            C.dma(W1[s_][:], I["expert_w1"][l, e, :, j * 512:(j + 1) * 512].rearrange("(c p) n -> p c n", p=128), (), [W1[s_]], eng="pool")
            C.dma(W3[s_][:], I["expert_w3"][l, e, :, j * 512:(j + 1) * 512].rearrange("(c p) n -> p c n", p=128), (), [W3[s_]], eng="pool")
        else:
            s_ = cnt["y"] % NBY
            cnt["y"] += 1
            slot[pi] = s_
            C.dma(W2[s_][:], I["expert_w2"][l, e, :, j * 512:(j + 1) * 512].rearrange("(c p) n -> p c n", p=128), (), [W2[s_]], eng="pool")

    def gather(e):
        xT = xeT[e % 2]
        for ti, (rt, jt, rows, col0, _) in enumerate(ttiles):
            xg = xe[ti % 2]
            C.p.op("pool", lambda en, xg=xg, jt=jt, rt=rt, rows=rows: en.indirect_dma_start(
                out=xg[0:rows, :], out_offset=None, in_=S["hA"][:, :],
                in_offset=bass.IndirectOffsetOnAxis(ap=rt["idx"][0:rows, e, jt:jt + 1], axis=0)),
                [rt["idx"], S["hA"]], [xg], dma=True)
            transpose_1024(C, K, xg, xT, npart=rows, col0=col0)

    def scatter(e):
        y_ = yo[e % 2]
        for ti, (rt, jt, rows, col0, g2b) in enumerate(ttiles):
            C.p.op("pool", lambda en, y_=y_, jt=jt, e=e, rt=rt, rows=rows, ti=ti: en.indirect_dma_start(
                out=S["xres"][:, :], out_offset=bass.IndirectOffsetOnAxis(ap=rt["idx"][0:rows, e, jt:jt + 1], axis=0),
                in_=y_[0:rows, ti, :], in_offset=None, compute_op=ALU.add),
                [rt["idx"], y_], [S["xres"]], dma=True)

    load_piece(0)
    load_piece(1)
    gather(0)
    for pi in range(len(pieces)):
        if pi + 2 < len(pieces):
            load_piece(pi + 2)
        e, kind, j = pieces[pi]
        s_ = slot[pi]
        xT, g_, y_ = xeT[e % 2], gT[e % 2], yo[e % 2]
        if kind == "a":
            if j == 1 and e >= 1:
                scatter(e - 1)
            if j == 1 and e + 1 < NE:
                gather(e + 1)
            for ft in range(4):
                f = j * 4 + ft
                sb_ = sa[f % 2]
                for c0, nc_ in segs:
                    ab, ub = C.bank(), C.bank()
                    for c in range(8):
                        C.mm(ab[:, 0:nc_], W1[s_][:, c, ft * 128:(ft + 1) * 128], xT[:, c, c0:c0 + nc_], c == 0, c == 7, [W1[s_], xT], [ab])
                    for c in range(8):
                        C.mm(ub[:, 0:nc_], W3[s_][:, c, ft * 128:(ft + 1) * 128], xT[:, c, c0:c0 + nc_], c == 0, c == 7, [W3[s_], xT], [ub])
                    C.act(sb_[:, c0:c0 + nc_], ab[:, 0:nc_], AF.Silu, [ab], [sb_])
                    C.tt(g_[:, f, c0:c0 + nc_], sb_[:, c0:c0 + nc_], ub[:, 0:nc_], ALU.mult, [sb_, ub], [g_])
        else:
            for ti, (rt, jt, rows, col0, g2b) in enumerate(ttiles):
                yb = C.bank()
                for fc in range(16):
                    C.mm(yb[0:rows, :], g_[:, fc, col0:col0 + rows], W2[s_][:, fc, :], fc == 0, fc == 15, [g_, W2[s_]], [yb])
                C.stt(y_[0:rows, ti, j * 512:(j + 1) * 512], yb[0:rows, :], rt["gate"][0:rows, e, jt:jt + 1], g2b[0:rows, j * 512:(j + 1) * 512],
                      ALU.mult, ALU.mult, [yb, rt["gate"], g2b], [y_])
            if j == 1 and e == NE - 1:
                scatter(e)


def alloc_route(C, cap):
    rows = min(cap, 128)
    njt = cap // rows
    return dict(idx=C.tile([128, NE, njt], I32, persist=True), gate=C.tile([128, NE, njt], F32, persist=True))


import os
ATT_LEVEL = int(os.environ.get('ATT_LEVEL', '9'))
ATT_TILES = int(os.environ.get('ATT_TILES', str(NT_ALL)))


def phase_attention(C, K):
    C.new_phase()
    I, S = C.din, C.dscr
    alloc_norm_scratch(C, K)
    wqkv = C.tile([128, 8, 1536], BF16)
    load_w_bf16(C, wqkv, I["attn_w_qkv"][:], 1536, 8)
    w_out = C.tile([128, 8, D], BF16)
    load_w_bf16(C, w_out, I["attn_w_out"][:], D, 8)
    W = dict(wr=load_router_w(C, 1))
    idb = K["identb"]
    kT = C.tile([64, 4, NALL], BF16)
    vaug = C.tile([128, NT_ALL, 4, 80], BF16)
    C.memset(vaug[:], 1.0, [vaug])
    masks = C.tile([128, 2, 128], BF16)
    C.dma(masks[:], I["amask"][:], (), [masks])
    esink = C.tile([128, 16], F32)
    C.dma(esink[:], I["attn_sink"][:].partition_broadcast(128), (), [esink])
    C.act(esink[:], esink[:], AF.Exp, [esink], [esink])
    gs1, sh1 = C.tile([128, D], F32), C.tile([128, D], F32)
    load_mod(C, gs1, 1, 1, 1)
    load_mod(C, sh1, 1, 1, 0)
    g1b, gs2, sh2 = C.tile([128, D], F32), C.tile([128, D], F32), C.tile([128, D], F32)
    load_mod(C, g1b, 1, 0, 2)
    load_mod(C, gs2, 1, 0, 4)
    load_mod(C, sh2, 1, 0, 3)
    xt = [C.tile([128, D], F32) for _ in range(2)]
    xt2 = [C.tile([128, D], F32) for _ in range(1)]
    rc = [C.tile([128, 512], F32) for _ in range(2)]
    rs = [C.tile([128, 512], F32) for _ in range(2)]
    h_bf = C.tile([128, D], BF16)
    hT = C.tile([128, 8, 128], BF16)
    zs = C.tile([128, 1280], F32)
    t1 = C.tile([128, 20, 64], F32)
    t2 = C.tile([128, 20, 64], F32)
    qk = C.tile([128, 20, 64], BF16)
    qT = [C.tile([64, 16, 128], BF16) for _ in range(3)]
    pT = [C.tile([128, 512], BF16) for _ in range(5)]
    oat2 = [C.tile([128, 16, 64], BF16) for _ in range(2)]
    den = C.tile([128, 4], F32)
    oT = C.tile([128, 8, 128], BF16)
    h2 = C.tile([128, D], BF16)
    h2T = oT
    K2 = dict(K)
    K2["ntmp"] = C.tile([128, D], F32)
    K2["nss"] = C.tile([128, 1], F32)

    def load(i):
        b = i % 2
        C.dma(xt[b][:], S["xres"][i * 128:(i + 1) * 128, :], [S["xres"].sub(i)], [xt[b]])
        if i >= 2:
            C.dma(rc[b][:], I["rope_c"][(i - 2) * 128:(i - 1) * 128, :], (), [rc[b]])
            C.dma(rs[b][:], I["rope_s"][(i - 2) * 128:(i - 1) * 128, :], (), [rs[b]])

    def qkv(i):
        b = i % 2
        if i == 2:
            load_mod(C, gs1, 1, 0, 1)
            load_mod(C, sh1, 1, 0, 0)
        norm_mod_transpose(C, K, xt[b], gs1, sh1, h_bf, hT)
        yield
        zb = []
        for n in range(3):
            if i < 2 and n < 2:
                zb.append(None)
                continue
            bk = C.bank()
            for c in range(8):
                C.mm(bk[:], hT[:, c, :], wqkv[:, c, n * 512:(n + 1) * 512], c == 0, c == 7, [hT, wqkv], [bk])
            zb.append(bk)
        kvb = zb[2]
        if ATT_LEVEL < 1:
            return
        C.copy(vaug[:, i, :, 0:64], kvb[:, 256:512].rearrange("p (k d) -> p k d", k=4), [kvb], [vaug.sub(i)], eng="act")
        if ATT_LEVEL < 2:
            return
        if i < 2:
            C.copy(qk[:, 16:20, :], kvb[:, 0:256].rearrange("p (k d) -> p k d", k=4), [kvb], [qk], eng="act")
        else:
            srcs = [(zb[0], 512, 0), (zb[1], 512, 8), (kvb, 256, 16)]
            for bkk, w, h0 in srcs:
                nh = w // 64
                zsv = zs[:, h0 * 64:h0 * 64 + w]
                C.copy(zsv, bkk[:, 0:w], [bkk], [zs], eng="act")
                t1v = t1[:, h0:h0 + nh, :].rearrange("p h d -> p (h d)")
                C.tt(t1v, zsv, rc[b][:, 0:w], ALU.mult, [zs, rc[b]], [t1])
                s5 = zsv.rearrange("p (h u a w) -> p h u a w", u=2, a=2, w=16)
                d5 = t2[:, h0:h0 + nh, :].rearrange("p h (u a w) -> p h u a w", u=2, a=2)
                sn = rs[b][:, 0:w].rearrange("p (h u a w) -> p h u a w", u=2, a=2, w=16)
                for a in range(2):
                    C.tt(d5[:, :, :, a, :], s5[:, :, :, 1 - a, :], sn[:, :, :, a, :], ALU.mult, [zs, rs[b]], [t2])
            C.tt(qk[:], t1[:], t2[:], ALU.add, [t1, t2], [qk])
        yield
        if ATT_LEVEL < 3:
            return
        tb = C.bank()
        pb = tb[:].bitcast(BF16)
        for kv in range(4):
            C.tr(pb[0:64, kv * 128:(kv + 1) * 128], qk[:, 16 + kv, :], idb[:], [qk, idb], [tb])
        C.copy(kT[:, :, i * 128:(i + 1) * 128], pb[0:64, 0:512].rearrange("p (k t) -> p k t", k=4), [tb], [kT.sub(i)], eng="act")
        if i >= 2:
            q_ = qT[i % 3]
            for half in range(2):
                tb = C.bank()
                pb = tb[:].bitcast(BF16)
                for hh in range(8):
                    C.tr(pb[0:64, hh * 128:(hh + 1) * 128], qk[:, half * 8 + hh, :], idb[:], [qk, idb], [tb])
                C.copy(q_[:, half * 8:(half + 1) * 8, :], pb[0:64, :].rearrange("p (h t) -> p h t", h=8), [tb], [q_], eng="act")

    def attend(i):
        if ATT_LEVEL < 4:
            return
        b = i % 2
        q_ = qT[i % 3]
        oat = oat2[i % 2]
        oat_flat = oat.view(oat[:].rearrange("p h d -> p (h d)"))
        C.dma(xt2[0][:], S["xres"][i * 128:(i + 1) * 128, :], [S["xres"].sub(i)], [xt2[0]])
        kbl = [(0, None), (1, None)]
        if i - 1 >= 2:
            kbl.append((i - 1, 0))
        kbl.append((i, None))
        if i + 1 < NT_ALL:
            kbl.append((i + 1, 1))
        for kv in range(4):
            pts = []
            for j, (kb, mk) in enumerate(kbl):
                sb_ = C.bank()
                C.mm(sb_[:], kT[:, kv, kb * 128:(kb + 1) * 128], q_[:, 4 * kv:4 * kv + 4, :].rearrange("p h t -> p (h t)"), True, True,
                     [kT.sub(kb), q_], [sb_])
                pt = pT[j]
                C.act(pt[:], sb_[:], AF.Exp, [sb_], [pt], scale=0.125)
                if mk is not None:
                    C.tt(pt[:].rearrange("p (h t) -> p h t", h=4), pt[:].rearrange("p (h t) -> p h t", h=4),
                         bc_mid(masks[:, mk, :], 4), ALU.mult, [pt, masks], [pt])
                pts.append(pt)
            yield
            if ATT_LEVEL < 5:
                continue
            ob_ = C.bank()
            for g in range(4):
                for j, (kb, mk) in enumerate(kbl):
                    C.mm(ob_[:, g * 128:g * 128 + 65], pts[j][:, g * 128:(g + 1) * 128], vaug[:, kb, kv, 0:65], j == 0, j == len(kbl) - 1,
                         [pts[j], vaug.sub(kb)], [ob_])
            ov = ob_[:, :].rearrange("p (g d) -> p g d", g=4)
            C.tt(den[:], ov[:, :, 64], esink[:, 4 * kv:4 * kv + 4], ALU.add, [ob_, esink], [den])
            C.recip(den[:], den[:], [den], [den])
            C.tt(oat[:, 4 * kv:4 * kv + 4, :], ov[:, :, 0:64], bc_last(den[:], 64), ALU.mult, [ob_, den], [oat])
            yield
        if ATT_LEVEL < 6:
            return
        transpose_1024(C, K, oat_flat, oT)
        yield
        yb = []
        for n in range(2):
            bk = C.bank()
            for c in range(8):
                C.mm(bk[:], oT[:, c, :], w_out[:, c, n * 512:(n + 1) * 512], c == 0, c == 7, [oT, w_out], [bk])
            yb.append(bk)
        residual_norm2_router(C, K2, W, i, 1, 0, yb, xt2[0], (g1b, gs2, sh2), (None, h2, h2T))

    load(0)
    for i in range(ATT_TILES):
        if i + 1 < ATT_TILES:
            load(i + 1)
        interleave(qkv(i), attend(i - 2) if i - 2 >= 2 else None)
    if ATT_TILES == NT_ALL:
        interleave(attend(NT_ALL - 2))
        interleave(attend(NT_ALL - 1))


def phase_final(C, K):
    C.new_phase()
    I, S = C.din, C.dscr
    alloc_norm_scratch(C, K)
    gb = C.tile([128, D], F32)
    C.dma(gb[:], I["final_g"][:].partition_broadcast(128), (), [gb])
    xt = [C.tile([128, D], F32) for _ in range(2)]
    yt = [C.tile([128, D], F32) for _ in range(2)]
    out = C.dout

    def load(i):
        C.dma(xt[i % 2][:], S["xres"][(i + 2) * 128:(i + 3) * 128, :], [S["xres"]], [xt[i % 2]])

    load(0)
    for i in range(NLAT // 128):
        if i + 1 < NLAT // 128:
            load(i + 1)
        b = i % 2
        rms_rstd(C, xt[b][:], 128, D, K["ntmp"], K["nss"], [xt[b]])
        C.stt(yt[b][:], xt[b][:], K["nss"][:], gb[:], ALU.mult, ALU.mult, [xt[b], K["nss"], gb], [yt[b]])
        C.dma(out[i * 128:(i + 1) * 128, :], yt[b][:], [yt[b]], [C.dout_buf], eng="pool")


INPUT_SPECS = [
    ("x", [NLAT, D], F32), ("ctx", [NCTX, D], F32), ("c2", [2, D], F32),
    ("mod_w", [2, D, 6 * D], F32), ("mod_b", [2, 6 * D], F32), ("norm1_g", [2, D], F32), ("norm2_g", [2, D], F32),
    ("ab_w_in", [D, 3584], F32), ("ab_w_out", [D, D], F32), ("sgu_w", [4, 128, 128], F32), ("sgu_b", [4, 128], F32),
    ("hgrn_lb_logits", [2, 2, 512], F32), ("hgrn_norm_g", [4, 128], F32),
    ("attn_w_qkv", [D, 1536], F32), ("attn_w_out", [D, D], F32), ("attn_sink", [16], F32),
    ("router_w", [2, D, NE], F32), ("expert_w1", [2, NE, D, FF], F32), ("expert_w3", [2, NE, D, FF], F32),
    ("expert_w2", [2, NE, FF, D], F32), ("final_g", [D], F32),
    ("identb", [128, 128], BF16), ("identf", [128, 128], F32), ("onesb", [128, 128], BF16), ("ustrict", [128, 128], BF16),
    ("gla_m", [64, 2, 64], F32), ("gla_sel", [64, 2, 2], F32), ("gla_tri", [64, 2, 64], F32),
    ("amask", [128, 2, 128], BF16), ("rope_c", [NLAT, 512], F32), ("rope_s", [NLAT, 512], F32),
    ("tvals", [128, NT_ALL, 2], BF16), ("iota", [128, 512], F32),
]


def host_constants():
    bf = ml_dtypes.bfloat16
    c = {}
    c["identb"] = np.eye(128, dtype=np.float32).astype(bf)
    c["identf"] = np.eye(128, dtype=np.float32)
    c["onesb"] = np.ones((128, 128), np.float32).astype(bf)
    pp = np.arange(128)
    c["ustrict"] = (pp[:, None] < pp[None, :]).astype(np.float32).astype(bf)
    s = np.arange(64)[:, None]
    t = np.arange(64)[None, :]
    m = np.zeros((64, 2, 64), np.float32)
    m[:, 0, :] = (s <= t).astype(np.float32) - (s <= 31).astype(np.float32)
    m[:, 1, :] = (s >= t).astype(np.float32) - (s >= 32).astype(np.float32)
    c["gla_m"] = m
    sel = np.zeros((64, 2, 2), np.float32)
    sel[:, 0, 0] = (np.arange(64) <= 31)
    sel[:, 1, 0] = (np.arange(64) >= 32)
    sel[:, :, 1] = 1.0
    c["gla_sel"] = sel
    tri = np.zeros((64, 2, 64), np.float32)
    tri[:, 0, :] = (s <= t)
    tri[:, 1, :] = (s >= t)
    c["gla_tri"] = tri
    j = np.arange(128)[:, None]
    i = np.arange(128)[None, :]
    am = np.zeros((128, 2, 128), np.float32)
    am[:, 0, :] = (i <= j)
    am[:, 1, :] = (j <= i)
    c["amask"] = am.astype(bf)
    tt = np.arange(NLAT)
    inv = (10000.0 ** (-np.arange(16, dtype=np.float32) / 16)).astype(np.float32)
    ar = (tt // 64).astype(np.float32)[:, None] * inv[None, :]
    ac = (tt % 64).astype(np.float32)[:, None] * inv[None, :]
    c["rope_c"] = np.tile(np.concatenate([np.cos(ar), np.cos(ar), np.cos(ac), np.cos(ac)], axis=1).astype(np.float32), (1, 8))
    c["rope_s"] = np.tile(np.concatenate([-np.sin(ar), np.sin(ar), -np.sin(ac), np.sin(ac)], axis=1).astype(np.float32), (1, 8))
    rowid = np.arange(NT_ALL)[None, :] * 128 + np.arange(128)[:, None]
    c["tvals"] = np.stack([rowid // 64, rowid % 64], axis=-1).astype(np.float32).astype(bf)
    c["iota"] = np.broadcast_to(np.arange(512, dtype=np.float32), (128, 512)).copy()
    return c


SCRATCH_SPECS = [
    ("modv", [2, 2, 6 * D], F32), ("xres", [NALL, D], F32), ("hA", [NALL, D], BF16),
    ("ya", [NALL, 512], BF16), ("q", [NALL, 512], BF16), ("kk", [2, NALL, 512], BF16), ("gl", [2, NALL, 512], F32),
    ("v", [NALL, 512], BF16), ("og", [NALL, 512], BF16), ("o", [2, NALL, 512], F32),
]


def build_program(debug=(), stop_after=None, only=None):
    nc = bass.Bass("TRN2", target_bir_lowering=False)
    C = Ctx(nc, debug)
    K = {}
    for name, shape, dt in INPUT_SPECS:
        C.inp(name, shape, dt)
    for name, shape, dt in SCRATCH_SPECS:
        C.scr(name, shape, dt)
    C.dout_buf = Buf(nc.dram_tensor("out", [NLAT, D], F32, kind="ExternalOutput").ap(), "out")
    C.dout = C.dout_buf.t
    load_consts(C, K)
    rt_ctx = alloc_route(C, 32)
    rt_lat = alloc_route(C, 512)
    phases = [
        ("prep", lambda: phase_prep(C, K)),
        ("mixer_in", lambda: phase_mixer_in(C, K)),
        ("gla", lambda: phase_gla(C, K)),
        ("mixer_out", lambda: phase_mixer_out(C, K)),
        ("route_ctx0", lambda: phase_route(C, K, 0, 2, 32, rt_ctx)),
        ("route_lat0", lambda: phase_route(C, K, 2, 32, 512, rt_lat)),
        ("experts_lat0", lambda: phase_experts(C, K, 0, [(0, 512, rt_lat), (1, 32, rt_ctx)])),
        ("attention", lambda: phase_attention(C, K)),
        ("route_lat1", lambda: phase_route(C, K, 2, 32, 512, rt_lat)),
        ("experts_lat1", lambda: phase_experts(C, K, 1, [(0, 512, rt_lat)])),
        ("final", lambda: phase_final(C, K)),
    ]
    for name, fn in phases:
        if only is not None and name not in only:
            continue
        fn()
        if stop_after == name:
            break
    C.p.emit()
    return nc


def make_in_maps(inputs, cores):
    consts = host_constants()
    f = lambda a: np.ascontiguousarray(np.asarray(a, dtype=np.float32))
    shared = {
        "mod_w": f(inputs["mod_w"]), "mod_b": f(inputs["mod_b"]), "norm1_g": f(inputs["norm1_g"]), "norm2_g": f(inputs["norm2_g"]),
        "ab_w_in": f(inputs["ab_w_in"][0]), "ab_w_out": f(inputs["ab_w_out"][0]), "sgu_w": f(inputs["sgu_w"][0]), "sgu_b": f(inputs["sgu_b"][0]),
        "hgrn_lb_logits": f(inputs["hgrn_lb_logits"]), "hgrn_norm_g": f(inputs["hgrn_norm_g"][0]),
        "attn_w_qkv": f(inputs["attn_w_qkv"][0]), "attn_w_out": f(inputs["attn_w_out"][0]), "attn_sink": f(inputs["attn_sink"][0]),
        "router_w": f(inputs["router_w"]), "expert_w1": f(inputs["expert_w1"]), "expert_w3": f(inputs["expert_w3"]),
        "expert_w2": f(inputs["expert_w2"]), "final_g": f(inputs["final_g"]),
    }
    shared.update(consts)
    maps = []
    for b in cores:
        m = dict(shared)
        m["x"] = f(inputs["x"][b])
        m["ctx"] = f(inputs["ctx"][b])
        m["c2"] = np.ascontiguousarray(np.stack([f(inputs["c"][b]), f(inputs["c_ctx"])], axis=0))
        maps.append(m)
    return maps


def kernel(**inputs):
    nc = build_program()
    maps = make_in_maps(inputs, list(range(8)))
    res = run_bass_kernel_spmd(nc, maps, core_ids=list(range(8)))
    return np.stack([np.asarray(r["out"], dtype=np.float32) for r in res.results], axis=0)
```

```python
import contextlib
import numpy as np
import ml_dtypes
import concourse.bass as bass
import concourse.mybir as mybir
from concourse.bass_utils import run_bass_kernel_spmd

F32 = mybir.dt.float32
BF16 = mybir.dt.bfloat16
I32 = mybir.dt.int32
U8 = mybir.dt.uint8
ALU = mybir.AluOpType
AF = mybir.ActivationFunctionType
AX = mybir.AxisListType
ISZ = {F32: 4, BF16: 2, I32: 4}

D = 1024
NLAT = 4096
NCTX = 256
NALL = NLAT + NCTX
NT_ALL = NALL // 128
EPS = 1e-6
NE = 16
FF = 2048

ENGS = ("pe", "act", "dve", "pool", "sp")
N_DMA_SEMS = 32
import os as _os
STRICT_SAME_ENGINE = _os.environ.get('STRICT_SAME_ENGINE', '0') == '1'


class Buf:
    def __init__(self, t, name, root=None):
        self.t = t
        self.name = name
        self.root = root if root is not None else self
        if root is None:
            self.whole = [None, []]
            self.subs = {}

    def __getitem__(self, idx):
        return self.t[idx]

    def sub(self, key):
        return (self.root, key)

    def view(self, ap):
        return Buf(ap, self.name + "_v", root=self.root)


def _nk(k):
    return (k.root, None) if isinstance(k, Buf) else (k[0].root, k[1])


class Prog:
    def __init__(self, nc):
        self.nc = nc
        self.ops = {e: [] for e in ENGS}
        self.dma_count = [0] * N_DMA_SEMS
        self.dma_last = [None] * N_DMA_SEMS
        self.dma_rr = 0
        self.dma_rr_pool = 0
        self.bar = {}

    def _states(self, key):
        buf, sk = _nk(key)
        if sk is None:
            return buf, sk, [buf.whole] + list(buf.subs.values())
        if sk not in buf.subs:
            buf.subs[sk] = [None, []]
        return buf, sk, [buf.whole, buf.subs[sk]]

    def barrier(self):
        toks = set()
        for e in ENGS:
            for i in range(len(self.ops[e]) - 1, -1, -1):
                if self.ops[e][i]["dma"] is None:
                    toks.add(("e", e, i))
                    break
        for t in self.dma_last:
            if t is not None:
                toks.add(t)
        self.bar = {e: set(toks) for e in ENGS}

    def op(self, eng, fn, reads=(), writes=(), dma=False):
        deps = set()
        if self.bar.get(eng):
            deps |= self.bar.pop(eng)
        for k in reads:
            _, _, sts = self._states(k)
            for st in sts:
                if st[0] is not None:
                    deps.add(st[0])
        for k in writes:
            _, _, sts = self._states(k)
            for st in sts:
                if st[0] is not None:
                    deps.add(("W",) + st[0])
                for r in st[1]:
                    deps.add(("W",) + r)
        idx = len(self.ops[eng])
        rec = dict(fn=fn, deps=[], sig=False, dma=None)
        if dma:
            half = N_DMA_SEMS // 2
            if eng == "pool":
                s = half + self.dma_rr_pool % half
                self.dma_rr_pool += 1
            else:
                s = self.dma_rr % half
                self.dma_rr += 1
            prev = self.dma_last[s]
            self.dma_count[s] += 16
            tok = ("d", s, self.dma_count[s])
            rec["dma"] = (s, self.dma_count[s])
            if prev is not None:
                deps.add(prev)
            self.dma_last[s] = tok
        else:
            tok = ("e", eng, idx)
        final = set()
        for d in deps:
            war = d[0] == "W"
            if war:
                d = d[1:]
            if d[0] == "e" and d[1] == eng and not dma and war and (eng == "pe" or not STRICT_SAME_ENGINE):
                continue
            if d != tok:
                final.add(d)
        for d in final:
            if d[0] == "e":
                self.ops[d[1]][d[2]]["sig"] = True
        rec["deps"] = sorted(final, key=str)
        self.ops[eng].append(rec)
        for k in reads:
            buf, sk, _ = self._states(k)
            rl = (buf.whole if sk is None else buf.subs[sk])[1]
            if tok[0] == "e":
                rl[:] = [t for t in rl if not (t[0] == "e" and t[1] == tok[1])]
            else:
                rl[:] = [t for t in rl if not (t[0] == "d" and t[1] == tok[1])]
            rl.append(tok)
        for k in writes:
            buf, sk, _ = self._states(k)
            if sk is None:
                buf.whole = [tok, []]
                buf.subs = {}
            else:
                buf.subs[sk] = [tok, []]
        return tok

    def dma(self, out, in_, reads=(), writes=(), eng="sp", **kw):
        return self.op(eng, lambda e: e.dma_start(out=out, in_=in_, **kw), reads, writes, dma=True)

    def emit(self):
        nc = self.nc
        with contextlib.ExitStack() as es:
            esem = {e: es.enter_context(nc.semaphore(f"s_{e}")) for e in ENGS}
            dsem = [es.enter_context(nc.semaphore(f"s_dma{i}")) for i in range(N_DMA_SEMS)]
            sigcnt = {}
            for e in ENGS:
                c = 0
                for i, r in enumerate(self.ops[e]):
                    if r["sig"] and r["dma"] is None:
                        c += 1
                        sigcnt[(e, i)] = c
            es.enter_context(nc.allow_non_contiguous_dma(reason="small strided loads of parameters"))
            block = es.enter_context(nc.Block())
            final_dma = [(s, self.dma_count[s]) for s in range(N_DMA_SEMS) if self.dma_count[s] > 0]

            def run(engname, eobj):
                waited = {}
                for r in self.ops[engname]:
                    for d in r["deps"]:
                        if d[0] == "e":
                            sem, val, key = esem[d[1]], sigcnt[(d[1], d[2])], ("e", d[1])
                        else:
                            sem, val, key = dsem[d[1]], d[2], ("d", d[1])
                        if waited.get(key, 0) >= val:
                            continue
                        waited[key] = val
                        eobj.wait_ge(sem, val)
                    ins = r["fn"](eobj)
                    if r["dma"] is not None:
                        ins.then_inc(dsem[r["dma"][0]], 16)
                    elif r["sig"]:
                        ins.then_inc(esem[engname], 1)
                if engname == "sp":
                    for s, v in final_dma:
                        eobj.wait_ge(dsem[s], v)

            block.sync(lambda e: run("sp", e))
            block.tensor(lambda e: run("pe", e))
            block.scalar(lambda e: run("act", e))
            block.vector(lambda e: run("dve", e))
            block.gpsimd(lambda e: run("pool", e))


ARENA_BYTES = 200 * 1024


class Ctx:
    def __init__(self, nc, debug=()):
        self.nc = nc
        self.p = Prog(nc)
        self.debug = set(debug)
        self.arena = nc.alloc_sbuf_tensor("arena", [128, ARENA_BYTES], U8)
        self.persist_top = 0
        self.off = 0
        self.cnt = 0
        self.banks = [Buf(nc.alloc_psum_tensor(f"psb{i}", [128, 512], F32)[:], f"psb{i}") for i in range(8)]
        self.bank_rr = 0
        self.din = {}
        self.dscr = {}

    def _carve(self, shape, dt, off):
        nb = int(np.prod(shape[1:])) * ISZ[dt]
        t = self.arena[0:shape[0], off:off + nb].bitcast(dt)
        if len(shape) == 3:
            t = t.rearrange("p (a b) -> p a b", a=shape[1])
        elif len(shape) == 4:
            t = t.rearrange("p (a b c) -> p a b c", a=shape[1], b=shape[2])
        return t, (nb + 63) // 64 * 64

    def tile(self, shape, dt, name=None, persist=False):
        self.cnt += 1
        name = name or f"t{self.cnt}"
        if persist:
            assert self.off == self.persist_top, "persistent tiles must be allocated at phase start"
        t, nb = self._carve(shape, dt, self.off)
        self.off += nb
        assert self.off <= ARENA_BYTES, f"SBUF arena overflow {self.off}"
        if persist:
            self.persist_top = self.off
        return Buf(t, name)

    def new_phase(self):
        self.p.barrier()
        self.off = self.persist_top

    def bank(self):
        b = self.banks[self.bank_rr % 8]
        self.bank_rr += 1
        return b

    def inp(self, name, shape, dt):
        self.din[name] = Buf(self.nc.dram_tensor(name, list(shape), dt, kind="ExternalInput").ap(), name)
        return self.din[name]

    def scr(self, name, shape, dt):
        kind = "ExternalOutput" if name in self.debug else "Internal"
        self.dscr[name] = Buf(self.nc.dram_tensor(name, list(shape), dt, kind=kind).ap(), name)
        return self.dscr[name]

    def act(self, out, in_, func, reads, writes, **kw):
        return self.p.op("act", lambda e: e.activation(out=out, in_=in_, func=func, **kw), reads, writes)

    def tt(self, out, in0, in1, op, reads, writes, eng="dve"):
        return self.p.op(eng, lambda e: e.tensor_tensor(out=out, in0=in0, in1=in1, op=op), reads, writes)

    def ts(self, out, in0, s1, s2, op0, op1, reads, writes, eng="dve"):
        if op1 is None:
            return self.p.op(eng, lambda e: e.tensor_scalar(out=out, in0=in0, scalar1=s1, scalar2=None, op0=op0), reads, writes)
        return self.p.op(eng, lambda e: e.tensor_scalar(out=out, in0=in0, scalar1=s1, scalar2=s2, op0=op0, op1=op1), reads, writes)

    def stt(self, out, in0, scalar, in1, op0, op1, reads, writes, eng="dve"):
        return self.p.op(eng, lambda e: e.scalar_tensor_tensor(out=out, in0=in0, scalar=scalar, in1=in1, op0=op0, op1=op1), reads, writes)

    def copy(self, out, in_, reads, writes, eng="dve"):
        if eng == "act":
            return self.p.op("act", lambda e: e.copy(out=out, in_=in_), reads, writes)
        return self.p.op(eng, lambda e: e.tensor_copy(out=out, in_=in_), reads, writes)

    def mm(self, out, lhsT, rhs, start, stop, reads, writes):
        return self.p.op("pe", lambda e: e.matmul(out, lhsT=lhsT, rhs=rhs, start=start, stop=stop), reads, writes)

    def tr(self, out, in_, ident, reads, writes):
        return self.p.op("pe", lambda e: e.transpose(out=out, in_=in_, identity=ident), reads, writes)

    def reduce(self, out, in_, op, reads, writes):
        return self.p.op("dve", lambda e: e.tensor_reduce(out=out, in_=in_, axis=AX.X, op=op), reads, writes)

    def recip(self, out, in_, reads, writes):
        return self.p.op("dve", lambda e: e.reciprocal(out=out, in_=in_), reads, writes)

    def memset(self, ap, val, writes, eng="pool"):
        return self.p.op(eng, lambda e: e.memset(ap, val), (), writes)

    def dma(self, out, in_, reads=(), writes=(), eng="sp", **kw):
        return self.p.dma(out, in_, reads, writes, eng, **kw)


def interleave(*gens):
    gens = [g for g in gens if g is not None]
    while gens:
        for g in list(gens):
            try:
                next(g)
            except StopIteration:
                gens.remove(g)


def bc_mid(ap2d, n):
    P, Fd = ap2d.shape
    return ap2d.unsqueeze(1).to_broadcast([P, n, Fd])


def bc_last(ap2d, n):
    P, A = ap2d.shape
    return ap2d.unsqueeze(2).to_broadcast([P, A, n])


def rms_rstd(C, x, npart, width, tmp, ss, reads):
    C.act(tmp[0:npart, 0:width], x, AF.Square, reads, [tmp, ss], accum_out=ss[0:npart, :])
    C.ts(ss[0:npart, :], ss[0:npart, :], 1.0 / width, EPS, ALU.mult, ALU.add, [ss], [ss])
    C.act(ss[0:npart, :], ss[0:npart, :], AF.Sqrt, [ss], [ss])
    C.recip(ss[0:npart, :], ss[0:npart, :], [ss], [ss])


def norm_mod_transpose(C, K, xt, gs, sh, h_bf, hT, want_T=True):
    tmp, ss = K["ntmp"], K["nss"]
    rms_rstd(C, xt[:], 128, D, tmp, ss, [xt])
    C.stt(tmp[:], xt[:], ss[:], gs[:], ALU.mult, ALU.mult, [xt, ss, gs, tmp], [tmp])
    C.tt(h_bf[:], tmp[:], sh[:], ALU.add, [tmp, sh], [h_bf])
    if want_T:
        transpose_1024(C, K, h_bf, hT)


def transpose_1024(C, K, src_bf, dstT, npart=128, col0=0):
    bk = C.bank()
    pb = bk[:].bitcast(BF16)
    for c in range(8):
        C.tr(pb[:, c * 128:c * 128 + npart], src_bf[0:npart, c * 128:(c + 1) * 128], K["identb"][0:npart, 0:npart],
             [src_bf, K["identb"]], [bk])
    src = pb.rearrange("p (c t) -> p c t", c=8)[:, :, 0:npart]
    C.copy(dstT[:, :, col0:col0 + npart], src, [bk], [dstT], eng="act")


def load_w_bf16(C, dst, w_ap, ncols, nchunks, col0=0, step=512):
    for n0 in range(0, ncols, step):
        n1 = min(ncols, n0 + step)
        C.dma(dst[:, :, col0 + n0:col0 + n1], w_ap[:, n0:n1].rearrange("(c p) n -> p c n", p=128), (), [dst], eng="pool")


def phase_prep(C, K):
    C.new_phase()
    I = C.din
    modv = C.dscr["modv"]
    cs = C.tile([128, 8, 2], F32)
    c2s = C.tile([2, D], F32)
    C.dma(c2s[:], I["c2"][:], (), [c2s])
    C.act(c2s[:], c2s[:], AF.Silu, [c2s], [c2s])
    bk = C.bank()
    for c in range(8):
        C.tr(bk[:, 2 * c:2 * c + 2], c2s[:, c * 128:(c + 1) * 128], K["identf"][0:2, 0:2], [c2s, K["identf"]], [bk])
    C.copy(cs[:].rearrange("p c r -> p (c r)"), bk[:, 0:16], [bk], [cs])
    wbuf = [C.tile([128, 8, 512], F32) for _ in range(2)]
    mv = C.tile([2, 6 * D], F32)
    mb = C.tile([2, 6 * D], F32)
    ng = C.tile([2, D], F32)
    k = 0
    for l in range(2):
        C.dma(mb[:], I["mod_b"][l, :].partition_broadcast(2), (), [mb])
        for n in range(12):
            w = wbuf[k % 2]
            k += 1
            C.dma(w[:], I["mod_w"][l, :, n * 512:(n + 1) * 512].rearrange("(c p) n -> p c n", p=128), (), [w])
            bk = C.bank()
            for c in range(8):
                C.mm(bk[0:2, :], cs[:, c, :], w[:, c, :], c == 0, c == 7, [cs, w], [bk])
            C.tt(mv[:, n * 512:(n + 1) * 512], bk[0:2, :], mb[:, n * 512:(n + 1) * 512], ALU.add, [bk, mb], [mv])
        for slot, gname in ((1, "norm1_g"), (4, "norm2_g")):
            C.dma(ng[:], I[gname][l, :].partition_broadcast(2), (), [ng])
            C.stt(mv[:, slot * D:(slot + 1) * D], mv[:, slot * D:(slot + 1) * D], 1.0, ng[:], ALU.add, ALU.mult, [mv, ng], [mv])
        C.dma(modv[l, :, :], mv[:], [mv], [modv])


def load_mod(C, dst, l, r, slot):
    C.dma(dst[:], C.dscr["modv"][l, r, slot * D:(slot + 1) * D].partition_broadcast(128), [C.dscr["modv"]], [dst])


def alloc_norm_scratch(C, K):
    K["ntmp"] = C.tile([128, D], F32)
    K["nss"] = C.tile([128, 1], F32)


def load_consts(C, K):
    I = C.din
    for name, shape, dt in (("identb", [128, 128], BF16), ("identf", [128, 128], F32), ("onesb", [128, 128], BF16),
                            ("ustrict", [128, 128], BF16)):
        K[name] = C.tile(shape, dt, name, persist=True)
        C.dma(K[name][:], I[name][:], (), [K[name]])
    K["logits"] = C.tile([128, NT_ALL, NE], F32, "logits", persist=True)


def phase_mixer_in(C, K):
    C.new_phase()
    I, S = C.din, C.dscr
    Kp = []
    for _ in range(2):
        kk_ = dict(K)
        alloc_norm_scratch(C, kk_)
        Kp.append(kk_)
    w_in = C.tile([128, 8, 3584], BF16)
    load_w_bf16(C, w_in, I["ab_w_in"][:], 3584, 8)
    ws = C.tile([128, 4, 128], F32)
    wsT = C.tile([128, 4, 128], BF16)
    C.dma(ws[:], I["sgu_w"][:].rearrange("g t s -> t g s"), (), [ws])
    bk = C.bank()
    for g in range(4):
        C.tr(bk[:, g * 128:(g + 1) * 128], ws[:, g, :], K["identf"][:], [ws, K["identf"]], [bk])
    C.copy(wsT[:].rearrange("p g t -> p (g t)"), bk[:], [bk], [wsT])
    bsT = C.tile([128, 4], F32)
    C.dma(bsT[:], I["sgu_b"][:].rearrange("g t -> t g"), (), [bsT])
    lbl = C.tile([128, 2, 2, 512], F32)
    C.dma(lbl[:].rearrange("p a b c -> p (a b c)"), I["hgrn_lb_logits"][:].rearrange("a b c -> (a b c)").partition_broadcast(128), (), [lbl])
    lb = C.tile([128, 2, 512], F32)
    oml = C.tile([128, 2, 512], F32)
    C.tt(lb[:], lbl[:, 0, :, :], lbl[:, 1, :, :], ALU.subtract, [lbl], [lb])
    C.act(lb[:], lb[:], AF.Sigmoid, [lb], [lb])
    C.ts(oml[:], lb[:], -1.0, 1.0, ALU.mult, ALU.add, [lb], [oml])
    mods = {}
    for r in range(2):
        gs = C.tile([128, D], F32)
        sh = C.tile([128, D], F32)
        load_mod(C, gs, 0, r, 1)
        load_mod(C, sh, 0, r, 0)
        mods[r] = (gs, sh)
    xt = [C.tile([128, D], F32) for _ in range(4)]
    h_bf2 = [C.tile([128, D], BF16) for _ in range(2)]
    hT2 = [C.tile([128, 8, 128], BF16) for _ in range(2)]
    u2 = [C.tile([128, 4, 128], F32) for _ in range(2)]
    v2 = [C.tile([128, 4, 128], F32) for _ in range(2)]
    sq2 = [C.tile([128, 4, 128], F32) for _ in range(2)]
    st42 = [C.tile([128, 4], F32) for _ in range(2)]
    st4b2 = [C.tile([128, 4], F32) for _ in range(2)]
    vn2 = [C.tile([128, 4, 128], BF16) for _ in range(2)]
    f32t4 = [C.tile([128, 512], F32) for _ in range(4)]
    ya = [C.tile([128, 512], BF16) for _ in range(2)]
    qo = [C.tile([128, 512], BF16) for _ in range(2)]
    kk = [C.tile([128, 512], BF16) for _ in range(4)]
    gl = [C.tile([128, 512], F32) for _ in range(4)]
    vo = [C.tile([128, 512], BF16) for _ in range(2)]
    og = [C.tile([128, 512], BF16) for _ in range(2)]

    def src_rows(i):
        if i < 2:
            return I["ctx"][i * 128:(i + 1) * 128, :]
        return I["x"][(i - 2) * 128:(i - 1) * 128, :]

    def load(i):
        C.dma(xt[i % 4][:], src_rows(i), (), [xt[i % 4]])

    def tile_body(i):
        b = i % 2
        r = 1 if i < 2 else 0
        rows = slice(i * 128, (i + 1) * 128)
        h_bf, hT, u, v, sq, st4, st4b, vn = h_bf2[b], hT2[b], u2[b], v2[b], sq2[b], st42[b], st4b2[b], vn2[b]
        norm_mod_transpose(C, Kp[b], xt[i % 4], mods[r][0], mods[r][1], h_bf, hT)
        yield
        zb = []
        for n in range(7):
            bk = C.bank()
            for c in range(8):
                C.mm(bk[:], hT[:, c, :], w_in[:, c, n * 512:(n + 1) * 512], c == 0, c == 7, [hT, w_in], [bk])
            zb.append(bk)
        C.act(u[:].rearrange("p g c -> p (g c)"), zb[0][:], AF.Gelu, [zb[0]], [u])
        C.act(v[:].rearrange("p g c -> p (g c)"), zb[1][:], AF.Gelu, [zb[1]], [v])
        C.reduce(st4[:], v[:], ALU.add, [v], [st4])
        C.ts(st4[:], st4[:], 1.0 / 128, None, ALU.mult, None, [st4], [st4])
        C.tt(v[:], v[:], bc_last(st4[:], 128), ALU.subtract, [v, st4], [v])
        C.act(sq[:], v[:], AF.Square, [v], [sq])
        C.reduce(st4b[:], sq[:], ALU.add, [sq], [st4b])
        C.ts(st4b[:], st4b[:], 1.0 / 128, EPS, ALU.mult, ALU.add, [st4b], [st4b])
        C.act(st4b[:], st4b[:], AF.Sqrt, [st4b], [st4b])
        C.recip(st4b[:], st4b[:], [st4b], [st4b])
        C.tt(vn[:], v[:], bc_last(st4b[:], 128), ALU.mult, [v, st4b], [vn])
        bk = C.bank()
        for g in range(4):
            C.mm(bk[:, g * 128:(g + 1) * 128], wsT[:, g, :], vn[:, g, :], True, True, [wsT, vn], [bk])
        for g in range(4):
            C.stt(ya[b][:, g * 128:(g + 1) * 128], bk[:, g * 128:(g + 1) * 128], bsT[:, g:g + 1], u[:, g, :],
                  ALU.add, ALU.mult, [bk, bsT, u], [ya[b]])
        C.dma(S["ya"][rows, :], ya[b][:], [ya[b]], [S["ya"].sub(i)], eng="pool")
        C.act(qo[b][:], zb[2][:], AF.Silu, [zb[2]], [qo[b]])
        C.dma(S["q"][rows, :], qo[b][:], [qo[b]], [S["q"].sub(i)], eng="pool")
        for d in range(2):
            kb_, gb_ = kk[2 * b + d], gl[2 * b + d]
            f32t = f32t4[2 * b + d]
            C.act(f32t[:], zb[3 + d][:], AF.Sigmoid, [zb[3 + d]], [f32t])
            C.tt(f32t[:], f32t[:], oml[:, d, :], ALU.mult, [f32t, oml], [f32t])
            C.tt(f32t[:], f32t[:], lb[:, d, :], ALU.add, [f32t, lb], [f32t])
            C.ts(kb_[:], f32t[:], -1.0, 1.0, ALU.mult, ALU.add, [f32t], [kb_])
            C.act(gb_[:], f32t[:], AF.Ln, [f32t], [gb_])
            C.dma(S["kk"][d, rows, :], kb_[:], [kb_], [S["kk"].sub((d, i))], eng="pool")
            C.dma(S["gl"][d, rows, :], gb_[:], [gb_], [S["gl"].sub((d, i))], eng="pool")
        C.copy(vo[b][:], zb[5][:], [zb[5]], [vo[b]], eng="act")
        C.dma(S["v"][rows, :], vo[b][:], [vo[b]], [S["v"].sub(i)], eng="pool")
        C.act(og[b][:], zb[6][:], AF.Silu, [zb[6]], [og[b]])
        C.dma(S["og"][rows, :], og[b][:], [og[b]], [S["og"].sub(i)], eng="pool")

    load(0)
    load(1)
    for i in range(0, NT_ALL, 2):
        for j in (i + 2, i + 3):
            if j < NT_ALL:
                load(j)
        interleave(tile_body(i), tile_body(i + 1))


def phase_gla(C, K):
    C.new_phase()
    I, S = C.din, C.dscr
    NCH = NALL // 64
    cst = {}
    for name, shape, dt in (("gla_m", [64, 2, 64], F32), ("gla_sel", [64, 2, 2], F32), ("gla_tri", [64, 2, 64], F32)):
        cst[name] = C.tile(shape, dt)
        C.dma(cst[name][:], I[name][:], (), [cst[name]])
    St = [C.tile([128, 4, 128], F32) for _ in range(2)]
    for d in range(2):
        C.memset(St[d][:], 0.0, [St[d]])
    NB = 2
    bufs = {}
    for d in range(2):
        for j in range(NB):
            bufs[(d, j)] = dict(
                q=C.tile([64, 512], BF16), kk=C.tile([64, 512], BF16), g=C.tile([64, 512], F32), v=C.tile([64, 512], BF16),
                ep=C.tile([64, 512], F32), em=C.tile([64, 512], F32), qb=C.tile([64, 512], BF16), kb=C.tile([64, 512], BF16),
                qkT=C.tile([128, 512], BF16), e3=C.tile([128, 4, 3], F32), ex=C.tile([128, 4, 3], F32),
                at=C.tile([64, 4, 64], BF16), ssc=C.tile([128, 4, 128], BF16), osb=C.tile([64, 512], F32),
                tmp=C.tile([128, 4, 128], F32))
    order = {0: list(range(NCH)), 1: [3, 2, 1, 0] + list(range(NCH - 1, 3, -1))}

    def load(d, step):
        ci = order[d][step]
        B = bufs[(d, step % NB)]
        rows = slice(ci * 64, (ci + 1) * 64)
        t = ci // 2
        C.dma(B["q"][:], S["q"][rows, :], [S["q"].sub(t)], [B["q"]])
        C.dma(B["kk"][:], S["kk"][d, rows, :], [S["kk"].sub((d, t))], [B["kk"]])
        C.dma(B["g"][:], S["gl"][d, rows, :], [S["gl"].sub((d, t))], [B["g"]])
        C.dma(B["v"][:], S["v"][rows, :], [S["v"].sub(t)], [B["v"]])

    def compute(d, step):
        ci = order[d][step]
        B = bufs[(d, step % NB)]
        rows = slice(ci * 64, (ci + 1) * 64)
        Sd = St[d]
        idb = K["identb"]
        bps = C.bank()
        C.mm(bps[0:64, :], cst["gla_m"][:, d, :], B["g"][:], True, True, [cst["gla_m"], B["g"]], [bps])
        C.act(B["ep"][:], bps[0:64, :], AF.Exp, [bps], [B["ep"]])
        C.act(B["em"][:], bps[0:64, :], AF.Exp, [bps], [B["em"]], scale=-1.0)
        C.tt(B["qb"][:], B["q"][:], B["ep"][:], ALU.mult, [B["q"], B["ep"]], [B["qb"]])
        C.tt(B["kb"][:], B["kk"][:], B["em"][:], ALU.mult, [B["kk"], B["em"]], [B["kb"]])
        yield
        tb = C.bank()
        pb = tb[:].bitcast(BF16)
        for h in range(4):
            C.tr(pb[:, h * 64:(h + 1) * 64], B["qb"][:, h * 128:(h + 1) * 128], idb[0:64, 0:64], [B["qb"], idb], [tb])
        for h in range(4):
            C.tr(pb[:, 256 + h * 64:256 + (h + 1) * 64], B["kb"][:, h * 128:(h + 1) * 128], idb[0:64, 0:64], [B["kb"], idb], [tb])
        C.copy(B["qkT"][:], pb[:, 0:512], [tb], [B["qkT"]], eng="act")
        yield
        sps = C.bank()
        for h in range(4):
            C.mm(sps[:, h * 2:h * 2 + 2], B["g"][:, h * 128:(h + 1) * 128], cst["gla_sel"][:, d, :], True, True,
                 [B["g"], cst["gla_sel"]], [sps])
        spv = sps[:, 0:8].rearrange("p (h c) -> p h c", c=2)
        C.copy(B["e3"][:, :, 0:2], spv, [sps], [B["e3"]])
        C.tt(B["e3"][:, :, 2], B["e3"][:, :, 1], B["e3"][:, :, 0], ALU.subtract, [B["e3"]], [B["e3"]])
        C.act(B["ex"][:], B["e3"][:], AF.Exp, [B["e3"]], [B["ex"]])
        yield
        aps = C.bank()
        for h in range(4):
            C.mm(aps[0:64, h * 64:(h + 1) * 64], B["qkT"][:, 256 + h * 64:256 + (h + 1) * 64], B["qkT"][:, h * 64:(h + 1) * 64],
                 True, True, [B["qkT"]], [aps])
        C.tt(B["at"][:], aps[0:64, 0:256].rearrange("p (h t) -> p h t", h=4), bc_mid(cst["gla_tri"][:, d, :], 4), ALU.mult,
             [aps, cst["gla_tri"]], [B["at"]])
        yield
        C.tt(B["ssc"][:], Sd[:], bc_last(B["ex"][:, :, 0], 128), ALU.mult, [Sd, B["ex"]], [B["ssc"]], eng="pool")
        ops_ = C.bank()
        for h in range(4):
            C.mm(ops_[0:64, h * 128:(h + 1) * 128], B["at"][:, h, :], B["v"][:, h * 128:(h + 1) * 128], True, False,
                 [B["at"], B["v"]], [ops_])
            C.mm(ops_[0:64, h * 128:(h + 1) * 128], B["qkT"][:, h * 64:(h + 1) * 64], B["ssc"][:, h, :], False, True,
                 [B["qkT"], B["ssc"]], [ops_])
        C.copy(B["osb"][:], ops_[0:64, :], [ops_], [B["osb"]], eng="act")
        C.dma(S["o"][d, rows, :], B["osb"][:], [B["osb"]], [S["o"].sub((d, ci))], eng="pool")
        yield
        dps = C.bank()
        for h in range(4):
            C.mm(dps[:, h * 128:(h + 1) * 128], B["kb"][:, h * 128:(h + 1) * 128], B["v"][:, h * 128:(h + 1) * 128], True, True,
                 [B["kb"], B["v"]], [dps])
        C.tt(B["tmp"][:], dps[:].rearrange("p (h v) -> p h v", h=4), bc_last(B["ex"][:, :, 2], 128), ALU.mult, [dps, B["ex"]], [B["tmp"]])
        C.tt(Sd[:], Sd[:], bc_last(B["ex"][:, :, 1], 128), ALU.mult, [Sd, B["ex"]], [Sd], eng="pool")
        C.tt(Sd[:], Sd[:], B["tmp"][:], ALU.add, [Sd, B["tmp"]], [Sd], eng="pool")

    for d in range(2):
        load(d, 0)
    for step in range(NCH):
        for d in range(2):
            if step + 1 < NCH:
                load(d, step + 1)
        interleave(compute(0, step), compute(1, step))


def residual_norm2_router(C, K, W, i, l, r, ybanks, xt, mods2, bufs):
    S = C.dscr
    g1b, gs2, sh2 = mods2
    x1, h2, h2T = bufs
    rows = slice(i * 128, (i + 1) * 128)
    if x1 is None:
        tmp = K["ntmp"]
        for n in range(2):
            C.tt(tmp[:, n * 512:(n + 1) * 512], ybanks[n][:], g1b[:, n * 512:(n + 1) * 512], ALU.mult, [ybanks[n], g1b], [tmp])
        C.tt(xt[:], tmp[:], xt[:], ALU.add, [tmp, xt], [xt])
        x1 = xt
    else:
        for n in range(2):
            C.tt(x1[:, n * 512:(n + 1) * 512], ybanks[n][:], g1b[:, n * 512:(n + 1) * 512], ALU.mult, [ybanks[n], g1b], [x1])
        C.tt(x1[:], x1[:], xt[:], ALU.add, [x1, xt], [x1])
    C.dma(S["xres"][rows, :], x1[:], [x1], [S["xres"].sub(i)], eng="pool")
    norm_mod_transpose(C, K, x1, gs2, sh2, h2, h2T)
    C.dma(S["hA"][rows, :], h2[:], [h2], [S["hA"].sub(i)], eng="pool")
    bk = C.bank()
    for c in range(8):
        C.mm(bk[:, 0:NE], h2T[:, c, :], W["wr"][:, c, :], c == 0, c == 7, [h2T, W["wr"]], [bk])
    C.copy(K["logits"][:, i, :], bk[:, 0:NE], [bk], [K["logits"].sub(i)])


def load_router_w(C, l):
    wr = C.tile([128, 8, NE], BF16)
    C.dma(wr[:], C.din["router_w"][l, :, :].rearrange("(c p) n -> p c n", p=128), (), [wr], eng="pool")
    return wr


def phase_mixer_out(C, K):
    C.new_phase()
    I, S = C.din, C.dscr
    Kp = []
    for _ in range(2):
        kk_ = dict(K)
        alloc_norm_scratch(C, kk_)
        Kp.append(kk_)
    w_out = C.tile([128, 8, D], BF16)
    load_w_bf16(C, w_out, I["ab_w_out"][:], D, 8)
    W = dict(wr=load_router_w(C, 0))
    ngb = C.tile([128, 512], F32)
    C.dma(ngb[:], I["hgrn_norm_g"][:].rearrange("h v -> (h v)").partition_broadcast(128), (), [ngb])
    mods2 = {}
    for r in range(2):
        g1b, gs2, sh2 = C.tile([128, D], F32), C.tile([128, D], F32), C.tile([128, D], F32)
        load_mod(C, g1b, 0, r, 2)
        load_mod(C, gs2, 0, r, 4)
        load_mod(C, sh2, 0, r, 3)
        mods2[r] = (g1b, gs2, sh2)
    NB = 4
    of = [C.tile([128, 4, 128], F32) for _ in range(NB)]
    ob = [C.tile([128, 4, 128], F32) for _ in range(NB)]
    ogt = [C.tile([128, 512], BF16) for _ in range(NB)]
    ycat = [C.tile([128, D], BF16) for _ in range(NB)]
    xt = [C.tile([128, D], F32) for _ in range(NB)]
    sq2 = [C.tile([128, 4, 128], F32) for _ in range(2)]
    st42 = [C.tile([128, 4], F32) for _ in range(2)]
    ycT2 = [C.tile([128, 8, 128], BF16) for _ in range(2)]
    h22 = [C.tile([128, D], BF16) for _ in range(2)]
    h2T2 = [C.tile([128, 8, 128], BF16) for _ in range(2)]

    def load(i):
        b = i % NB
        rows = slice(i * 128, (i + 1) * 128)
        C.dma(of[b][:].rearrange("p h v -> p (h v)"), S["o"][0, rows, :], [S["o"]], [of[b]])
        C.dma(ob[b][:].rearrange("p h v -> p (h v)"), S["o"][1, rows, :], [S["o"]], [ob[b]])
        C.dma(ogt[b][:], S["og"][rows, :], [S["og"]], [ogt[b]])
        C.dma(ycat[b][:, 0:512], S["ya"][rows, :], [S["ya"]], [ycat[b]])
        src = I["ctx"][i * 128:(i + 1) * 128, :] if i < 2 else I["x"][(i - 2) * 128:(i - 1) * 128, :]
        C.dma(xt[b][:], src, (), [xt[b]])

    def tile_body(i):
        b = i % NB
        pb_ = i % 2
        sq, st4, ycT, h2, h2T = sq2[pb_], st42[pb_], ycT2[pb_], h22[pb_], h2T2[pb_]
        r = 1 if i < 2 else 0
        o = of[b]
        C.tt(o[:], o[:], ob[b][:], ALU.add, [o, ob[b]], [o])
        C.act(sq[:], o[:], AF.Square, [o], [sq])
        C.reduce(st4[:], sq[:], ALU.add, [sq], [st4])
        C.ts(st4[:], st4[:], 1.0 / 128, EPS, ALU.mult, ALU.add, [st4], [st4])
        C.act(st4[:], st4[:], AF.Sqrt, [st4], [st4])
        C.recip(st4[:], st4[:], [st4], [st4])
        C.tt(o[:], o[:], bc_last(st4[:], 128), ALU.mult, [o, st4], [o])
        of2 = o[:].rearrange("p h v -> p (h v)")
        C.tt(of2, of2, ngb[:], ALU.mult, [o, ngb], [o])
        C.tt(ycat[b][:, 512:1024], of2, ogt[b][:], ALU.mult, [o, ogt[b], ycat[b]], [ycat[b]])
        yield
        transpose_1024(C, K, ycat[b], ycT)
        yield
        yb = []
        for n in range(2):
            bk = C.bank()
            for c in range(8):
                C.mm(bk[:], ycT[:, c, :], w_out[:, c, n * 512:(n + 1) * 512], c == 0, c == 7, [ycT, w_out], [bk])
            yb.append(bk)
        residual_norm2_router(C, Kp[pb_], W, i, 0, r, yb, xt[b], mods2[r], (None, h2, h2T))

    load(0)
    load(1)
    for i in range(0, NT_ALL, 2):
        for j in (i + 2, i + 3):
            if j < NT_ALL:
                load(j)
        interleave(tile_body(i), tile_body(i + 1))


def phase_route(C, K, tile0, ntl, cap, rt):
    C.new_phase()
    I = C.din
    lg = K["logits"][:, tile0:tile0 + ntl, :]
    LR = [K["logits"]]
    NF = ntl * NE
    aff = C.tile([128, ntl, NE], F32)
    t2 = C.tile([128, ntl], F32)
    C.reduce(t2[:], lg, ALU.max, LR, [t2])
    C.tt(aff[:], lg, bc_last(t2[:], NE), ALU.subtract, LR + [t2], [aff])
    C.act(aff[:], aff[:], AF.Exp, [aff], [aff])
    C.reduce(t2[:], aff[:], ALU.add, [aff], [t2])
    C.recip(t2[:], t2[:], [t2], [t2])
    C.tt(aff[:], aff[:], bc_last(t2[:], NE), ALU.mult, [aff, t2], [aff])
    lo, hi, mid = C.tile([128, NE], F32), C.tile([128, NE], F32), C.tile([128, NE], F32)
    cnt, ge, dlt = C.tile([128, NE], F32), C.tile([128, NE], F32), C.tile([128, NE], F32)
    cmpb = C.tile([128, ntl, NE], BF16)
    C.memset(lo[:], 0.0, [lo])
    C.memset(hi[:], 1.0, [hi])
    C.memset(mid[:], 0.5, [mid])
    onesb = K["onesb"]
    for it in range(30):
        C.tt(cmpb[:], aff[:], bc_mid(mid[:], ntl), ALU.is_gt, [aff, mid], [cmpb])
        bk = C.bank()
        C.mm(bk[:, 0:NF], onesb[:], cmpb[:].rearrange("p i e -> p (i e)"), True, True, [onesb, cmpb], [bk])
        C.reduce(cnt[:], bk[:, 0:NF].rearrange("p (i e) -> p e i", e=NE), ALU.add, [bk], [cnt])
        C.ts(ge[:], cnt[:], float(cap), None, ALU.is_ge, None, [cnt], [ge])
        C.tt(dlt[:], mid[:], lo[:], ALU.subtract, [mid, lo], [dlt])
        C.tt(dlt[:], dlt[:], ge[:], ALU.mult, [dlt, ge], [dlt])
        C.tt(lo[:], lo[:], dlt[:], ALU.add, [lo, dlt], [lo])
        C.tt(dlt[:], hi[:], mid[:], ALU.subtract, [hi, mid], [dlt])
        C.tt(dlt[:], dlt[:], ge[:], ALU.mult, [dlt, ge], [dlt])
        C.tt(hi[:], mid[:], dlt[:], ALU.add, [mid, dlt], [hi])
        C.tt(mid[:], lo[:], hi[:], ALU.add, [lo, hi], [mid])
        C.ts(mid[:], mid[:], 0.5, None, ALU.mult, None, [mid], [mid])
    Ae = C.tile([128, NE, ntl], F32)
    inc = C.tile([128, NE, ntl], F32)
    rmask = C.tile([128, NE, ntl], F32)
    Aeb = C.tile([128, NE, ntl], BF16)
    excb = C.tile([128, NE, ntl], BF16)
    posm = C.tile([128, NE, ntl], F32)
    C.tt(Ae[:], aff[:].rearrange("p i e -> p e i"), bc_last(lo[:], ntl), ALU.is_gt, [aff, lo], [Ae])
    C.memset(rmask[:], 1.0, [rmask])
    C.memset(rmask[:, :, 0:1], 0.0, [rmask])
    flat = lambda t: t[:].rearrange("p e i -> p (e i)")
    C.p.op("dve", lambda e: e.tensor_tensor_scan(out=flat(inc), data0=flat(rmask), data1=flat(Ae), initial=0.0,
                                                 op0=ALU.mult, op1=ALU.add), [rmask, Ae], [inc])
    C.tt(inc[:], inc[:], Ae[:], ALU.subtract, [inc, Ae], [inc])
    C.copy(Aeb[:], Ae[:], [Ae], [Aeb])
    C.copy(excb[:], inc[:], [inc], [excb])
    bk = C.bank()
    C.mm(bk[:, 0:NF], K["ustrict"][:], flat(Aeb), True, False, [K["ustrict"], Aeb], [bk])
    C.mm(bk[:, 0:NF], onesb[:], flat(excb), False, True, [onesb, excb], [bk])
    C.stt(flat(posm), bk[:, 0:NF], 1.0, flat(Ae), ALU.add, ALU.mult, [bk, Ae], [posm])
    C.ts(posm[:], posm[:], -1.0, None, ALU.add, None, [posm], [posm])
    vals = C.tile([128, ntl, NE, 4], BF16)
    tv = C.tile([128, NT_ALL, 2], BF16)
    C.dma(tv[:], I["tvals"][:], (), [tv])
    affb = C.tile([128, ntl, NE], BF16)
    afr = C.tile([128, ntl, NE], F32)
    C.copy(affb[:], aff[:], [aff], [affb])
    C.tt(afr[:], aff[:], affb[:], ALU.subtract, [aff, affb], [afr])
    for j in range(2):
        C.copy(vals[:, :, :, j], bc_last(tv[:, tile0:tile0 + ntl, j], NE), [tv], [vals])
    C.copy(vals[:, :, :, 2], affb[:], [affb], [vals])
    C.copy(vals[:, :, :, 3], afr[:], [afr], [vals])
    iota = C.tile([128, 512], F32)
    C.dma(iota[:], I["iota"][:], (), [iota])
    rows = min(cap, 128)
    njt = cap // rows
    Pm = [C.tile([128, cap], BF16) for _ in range(4)]
    Rsb = [C.tile([4, cap], F32) for _ in range(2)]
    tq = C.tile([128, 4], F32)
    idf = C.tile([128, 1], F32)
    k = 0
    for e in range(NE):
        rb = C.bank()
        for i in range(ntl):
            Pt = Pm[k % 4]
            k += 1
            C.ts(Pt[:], iota[:, 0:cap], posm[:, e, i:i + 1], None, ALU.is_equal, None, [iota, posm], [Pt])
            C.mm(rb[0:4, 0:cap], vals[:, i, e, :], Pt[:], i == 0, i == ntl - 1, [vals, Pt], [rb])
        R = Rsb[e % 2]
        C.copy(R[:], rb[0:4, 0:cap], [rb], [R], eng="act")
        for jt in range(njt):
            tb = C.bank()
            C.tr(tb[0:rows, 0:4], R[:, jt * rows:(jt + 1) * rows], K["identf"][0:4, 0:4], [R, K["identf"]], [tb])
            C.copy(tq[0:rows, :], tb[0:rows, 0:4], [tb], [tq])
            C.stt(idf[0:rows, :], tq[0:rows, 0:1], 64.0, tq[0:rows, 1:2], ALU.mult, ALU.add, [tq], [idf])
            C.copy(rt["idx"][0:rows, e, jt:jt + 1], idf[0:rows, :], [idf], [rt["idx"]])
            C.tt(rt["gate"][0:rows, e, jt:jt + 1], tq[0:rows, 2:3], tq[0:rows, 3:4], ALU.add, [tq], [rt["gate"]])


def phase_experts(C, K, l, groups):
    C.new_phase()
    I, S = C.din, C.dscr
    ttiles, segs = [], []
    col = 0
    for r, cap, rt in groups:
        g2b = C.tile([128, D], F32)
        load_mod(C, g2b, l, r, 5)
        rows = min(cap, 128)
        for jt in range(cap // rows):
            ttiles.append((rt, jt, rows, col + jt * rows, g2b))
        segs.append((col, cap))
        col += cap
    NTOK = col
    ntt = len(ttiles)
    NBA, NBY = 3, 2
    W1 = [C.tile([128, 8, 512], BF16) for _ in range(NBA)]
    W3 = [C.tile([128, 8, 512], BF16) for _ in range(NBA)]
    W2 = [C.tile([128, 16, 512], BF16) for _ in range(NBY)]
    xe = [C.tile([128, D], BF16) for _ in range(2)]
    xeT = [C.tile([128, 8, NTOK], BF16) for _ in range(2)]
    gT = [C.tile([128, 16, NTOK], BF16) for _ in range(2)]
    sa = [C.tile([128, NTOK], BF16) for _ in range(2)]
    yo = [C.tile([128, ntt, D], F32) for _ in range(2)]
    pieces = []
    for e in range(NE):
        pieces += [(e, "a", fq) for fq in range(4)] + [(e, "y", dh) for dh in range(2)]
    cnt = {"a": 0, "y": 0}
    slot = {}

    def load_piece(pi):
        e, kind, j = pieces[pi]
        if kind == "a":
            s_ = cnt["a"] % NBA
            cnt["a"] += 1
            slot[pi] = s_
            C.dma(W1[s_][:], I["expert_w1"][l, e, :, j * 512:(j + 1) * 512].rearrange("(c p) n -> p c n", p=128), (), [W1[s_]], eng="pool")
            C.dma(W3[s_][:], I["expert_w3"][l, e, :, j * 512:(j + 1) * 512].rearrange("(c p) n -> p c n", p=128), (), [W3[s_]], eng="pool")
        else:
            s_ = cnt["y"] % NBY
            cnt["y"] += 1
            slot[pi] = s_
            C.dma(W2[s_][:], I["expert_w2"][l, e, :, j * 512:(j + 1) * 512].rearrange("(c p) n -> p c n", p=128), (), [W2[s_]], eng="pool")

    def gather(e):
        xT = xeT[e % 2]
        for ti, (rt, jt, rows, col0, _) in enumerate(ttiles):
            xg = xe[ti % 2]
            C.p.op("pool", lambda en, xg=xg, jt=jt, rt=rt, rows=rows: en.indirect_dma_start(
                out=xg[0:rows, :], out_offset=None, in_=S["hA"][:, :],
                in_offset=bass.IndirectOffsetOnAxis(ap=rt["idx"][0:rows, e, jt:jt + 1], axis=0)),
                [rt["idx"], S["hA"]], [xg], dma=True)
            transpose_1024(C, K, xg, xT, npart=rows, col0=col0)

    load_piece(0)
    load_piece(1)
    gather(0)
    for pi in range(len(pieces)):
        if pi + 2 < len(pieces):
            load_piece(pi + 2)
        e, kind, j = pieces[pi]
        s_ = slot[pi]
        xT, g_, y_ = xeT[e % 2], gT[e % 2], yo[e % 2]
        if kind == "a":
            for ft in range(4):
                f = j * 4 + ft
                sb_ = sa[f % 2]
                for c0, nc_ in segs:
                    ab, ub = C.bank(), C.bank()
                    for c in range(8):
                        C.mm(ab[:, 0:nc_], W1[s_][:, c, ft * 128:(ft + 1) * 128], xT[:, c, c0:c0 + nc_], c == 0, c == 7, [W1[s_], xT], [ab])
                    for c in range(8):
                        C.mm(ub[:, 0:nc_], W3[s_][:, c, ft * 128:(ft + 1) * 128], xT[:, c, c0:c0 + nc_], c == 0, c == 7, [W3[s_], xT], [ub])
                    C.act(sb_[:, c0:c0 + nc_], ab[:, 0:nc_], AF.Silu, [ab], [sb_])
                    C.tt(g_[:, f, c0:c0 + nc_], sb_[:, c0:c0 + nc_], ub[:, 0:nc_], ALU.mult, [sb_, ub], [g_])
        else:
            if j == 0 and e + 1 < NE:
                gather(e + 1)
            for ti, (rt, jt, rows, col0, g2b) in enumerate(ttiles):
                yb = C.bank()
                for fc in range(16):
                    C.mm(yb[0:rows, :], g_[:, fc, col0:col0 + rows], W2[s_][:, fc, :], fc == 0, fc == 15, [g_, W2[s_]], [yb])
                C.stt(y_[0:rows, ti, j * 512:(j + 1) * 512], yb[0:rows, :], rt["gate"][0:rows, e, jt:jt + 1], g2b[0:rows, j * 512:(j + 1) * 512],
                      ALU.mult, ALU.mult, [yb, rt["gate"], g2b], [y_])
            if j == 1:
                for ti, (rt, jt, rows, col0, g2b) in enumerate(ttiles):
                    C.p.op("pool", lambda en, y_=y_, jt=jt, e=e, rt=rt, rows=rows, ti=ti: en.indirect_dma_start(
                        out=S["xres"][:, :], out_offset=bass.IndirectOffsetOnAxis(ap=rt["idx"][0:rows, e, jt:jt + 1], axis=0),
                        in_=y_[0:rows, ti, :], in_offset=None, compute_op=ALU.add),
                        [rt["idx"], y_], [S["xres"]], dma=True)


def alloc_route(C, cap):
    rows = min(cap, 128)
    njt = cap // rows
    return dict(idx=C.tile([128, NE, njt], I32, persist=True), gate=C.tile([128, NE, njt], F32, persist=True))


import os
ATT_LEVEL = int(os.environ.get('ATT_LEVEL', '9'))
ATT_TILES = int(os.environ.get('ATT_TILES', str(NT_ALL)))


def phase_attention(C, K):
    C.new_phase()
    I, S = C.din, C.dscr
    alloc_norm_scratch(C, K)
    wqkv = C.tile([128, 8, 1536], BF16)
    load_w_bf16(C, wqkv, I["attn_w_qkv"][:], 1536, 8)
    w_out = C.tile([128, 8, D], BF16)
    load_w_bf16(C, w_out, I["attn_w_out"][:], D, 8)
    W = dict(wr=load_router_w(C, 1))
    idb = K["identb"]
    kT = C.tile([64, 4, NALL], BF16)
    vaug = C.tile([128, NT_ALL, 4, 80], BF16)
    C.memset(vaug[:], 1.0, [vaug])
    masks = C.tile([128, 2, 128], BF16)
    C.dma(masks[:], I["amask"][:], (), [masks])
    esink = C.tile([128, 16], F32)
    C.dma(esink[:], I["attn_sink"][:].partition_broadcast(128), (), [esink])
    C.act(esink[:], esink[:], AF.Exp, [esink], [esink])
    gs1, sh1 = C.tile([128, D], F32), C.tile([128, D], F32)
    load_mod(C, gs1, 1, 1, 1)
    load_mod(C, sh1, 1, 1, 0)
    g1b, gs2, sh2 = C.tile([128, D], F32), C.tile([128, D], F32), C.tile([128, D], F32)
    load_mod(C, g1b, 1, 0, 2)
    load_mod(C, gs2, 1, 0, 4)
    load_mod(C, sh2, 1, 0, 3)
    xt = [C.tile([128, D], F32) for _ in range(2)]
    xt2 = [C.tile([128, D], F32) for _ in range(1)]
    rc = [C.tile([128, 512], F32) for _ in range(2)]
    rs = [C.tile([128, 512], F32) for _ in range(2)]
    h_bf = C.tile([128, D], BF16)
    hT = C.tile([128, 8, 128], BF16)
    zs = C.tile([128, 1280], F32)
    t1 = C.tile([128, 20, 64], F32)
    t2 = C.tile([128, 20, 64], F32)
    qk = C.tile([128, 20, 64], BF16)
    qT = [C.tile([64, 16, 128], BF16) for _ in range(3)]
    pT = [C.tile([128, 512], BF16) for _ in range(5)]
    oat2 = [C.tile([128, 16, 64], BF16) for _ in range(2)]
    den = C.tile([128, 4], F32)
    oT = C.tile([128, 8, 128], BF16)
    h2 = C.tile([128, D], BF16)
    h2T = oT
    K2 = dict(K)
    K2["ntmp"] = C.tile([128, D], F32)
    K2["nss"] = C.tile([128, 1], F32)

    def load(i):
        b = i % 2
        C.dma(xt[b][:], S["xres"][i * 128:(i + 1) * 128, :], [S["xres"].sub(i)], [xt[b]])
        if i >= 2:
            C.dma(rc[b][:], I["rope_c"][(i - 2) * 128:(i - 1) * 128, :], (), [rc[b]])
            C.dma(rs[b][:], I["rope_s"][(i - 2) * 128:(i - 1) * 128, :], (), [rs[b]])

    def qkv(i):
        b = i % 2
        if i == 2:
            load_mod(C, gs1, 1, 0, 1)
            load_mod(C, sh1, 1, 0, 0)
        norm_mod_transpose(C, K, xt[b], gs1, sh1, h_bf, hT)
        yield
        zb = []
        for n in range(3):
            if i < 2 and n < 2:
                zb.append(None)
                continue
            bk = C.bank()
            for c in range(8):
                C.mm(bk[:], hT[:, c, :], wqkv[:, c, n * 512:(n + 1) * 512], c == 0, c == 7, [hT, wqkv], [bk])
            zb.append(bk)
        kvb = zb[2]
        if ATT_LEVEL < 1:
            return
        C.copy(vaug[:, i, :, 0:64], kvb[:, 256:512].rearrange("p (k d) -> p k d", k=4), [kvb], [vaug.sub(i)], eng="act")
        if ATT_LEVEL < 2:
            return
        if i < 2:
            C.copy(qk[:, 16:20, :], kvb[:, 0:256].rearrange("p (k d) -> p k d", k=4), [kvb], [qk], eng="act")
        else:
            srcs = [(zb[0], 512, 0), (zb[1], 512, 8), (kvb, 256, 16)]
            for bkk, w, h0 in srcs:
                nh = w // 64
                zsv = zs[:, h0 * 64:h0 * 64 + w]
                C.copy(zsv, bkk[:, 0:w], [bkk], [zs], eng="act")
                t1v = t1[:, h0:h0 + nh, :].rearrange("p h d -> p (h d)")
                C.tt(t1v, zsv, rc[b][:, 0:w], ALU.mult, [zs, rc[b]], [t1])
                s5 = zsv.rearrange("p (h u a w) -> p h u a w", u=2, a=2, w=16)
                d5 = t2[:, h0:h0 + nh, :].rearrange("p h (u a w) -> p h u a w", u=2, a=2)
                sn = rs[b][:, 0:w].rearrange("p (h u a w) -> p h u a w", u=2, a=2, w=16)
                for a in range(2):
                    C.tt(d5[:, :, :, a, :], s5[:, :, :, 1 - a, :], sn[:, :, :, a, :], ALU.mult, [zs, rs[b]], [t2])
            C.tt(qk[:], t1[:], t2[:], ALU.add, [t1, t2], [qk])
        yield
        if ATT_LEVEL < 3:
            return
        tb = C.bank()
        pb = tb[:].bitcast(BF16)
        for kv in range(4):
            C.tr(pb[0:64, kv * 128:(kv + 1) * 128], qk[:, 16 + kv, :], idb[:], [qk, idb], [tb])
        C.copy(kT[:, :, i * 128:(i + 1) * 128], pb[0:64, 0:512].rearrange("p (k t) -> p k t", k=4), [tb], [kT.sub(i)], eng="act")
        if i >= 2:
            q_ = qT[i % 3]
            for half in range(2):
                tb = C.bank()
                pb = tb[:].bitcast(BF16)
                for hh in range(8):
                    C.tr(pb[0:64, hh * 128:(hh + 1) * 128], qk[:, half * 8 + hh, :], idb[:], [qk, idb], [tb])
                C.copy(q_[:, half * 8:(half + 1) * 8, :], pb[0:64, :].rearrange("p (h t) -> p h t", h=8), [tb], [q_], eng="act")

    def attend(i):
        if ATT_LEVEL < 4:
            return
        b = i % 2
        q_ = qT[i % 3]
        oat = oat2[i % 2]
        oat_flat = oat.view(oat[:].rearrange("p h d -> p (h d)"))
        C.dma(xt2[0][:], S["xres"][i * 128:(i + 1) * 128, :], [S["xres"].sub(i)], [xt2[0]])
        kbl = [(0, None), (1, None)]
        if i - 1 >= 2:
            kbl.append((i - 1, 0))
        kbl.append((i, None))
        if i + 1 < NT_ALL:
            kbl.append((i + 1, 1))
        for kv in range(4):
            pts = []
            for j, (kb, mk) in enumerate(kbl):
                sb_ = C.bank()
                C.mm(sb_[:], kT[:, kv, kb * 128:(kb + 1) * 128], q_[:, 4 * kv:4 * kv + 4, :].rearrange("p h t -> p (h t)"), True, True,
                     [kT.sub(kb), q_], [sb_])
                pt = pT[j]
                C.act(pt[:], sb_[:], AF.Exp, [sb_], [pt], scale=0.125)
                if mk is not None:
                    C.tt(pt[:].rearrange("p (h t) -> p h t", h=4), pt[:].rearrange("p (h t) -> p h t", h=4),
                         bc_mid(masks[:, mk, :], 4), ALU.mult, [pt, masks], [pt])
                pts.append(pt)
            yield
            if ATT_LEVEL < 5:
                continue
            ob_ = C.bank()
            for g in range(4):
                for j, (kb, mk) in enumerate(kbl):
                    C.mm(ob_[:, g * 128:g * 128 + 65], pts[j][:, g * 128:(g + 1) * 128], vaug[:, kb, kv, 0:65], j == 0, j == len(kbl) - 1,
                         [pts[j], vaug.sub(kb)], [ob_])
            ov = ob_[:, :].rearrange("p (g d) -> p g d", g=4)
            C.tt(den[:], ov[:, :, 64], esink[:, 4 * kv:4 * kv + 4], ALU.add, [ob_, esink], [den])
            C.recip(den[:], den[:], [den], [den])
            C.tt(oat[:, 4 * kv:4 * kv + 4, :], ov[:, :, 0:64], bc_last(den[:], 64), ALU.mult, [ob_, den], [oat])
            yield
        if ATT_LEVEL < 6:
            return
        transpose_1024(C, K, oat_flat, oT)
        yield
        yb = []
        for n in range(2):
            bk = C.bank()
            for c in range(8):
                C.mm(bk[:], oT[:, c, :], w_out[:, c, n * 512:(n + 1) * 512], c == 0, c == 7, [oT, w_out], [bk])
            yb.append(bk)
        residual_norm2_router(C, K2, W, i, 1, 0, yb, xt2[0], (g1b, gs2, sh2), (None, h2, h2T))

    load(0)
    for i in range(ATT_TILES):
        if i + 1 < ATT_TILES:
            load(i + 1)
        interleave(qkv(i), attend(i - 2) if i - 2 >= 2 else None)
    if ATT_TILES == NT_ALL:
        interleave(attend(NT_ALL - 2))
        interleave(attend(NT_ALL - 1))


def phase_final(C, K):
    C.new_phase()
    I, S = C.din, C.dscr
    alloc_norm_scratch(C, K)
    gb = C.tile([128, D], F32)
    C.dma(gb[:], I["final_g"][:].partition_broadcast(128), (), [gb])
    xt = [C.tile([128, D], F32) for _ in range(2)]
    yt = [C.tile([128, D], F32) for _ in range(2)]
    out = C.dout

    def load(i):
        C.dma(xt[i % 2][:], S["xres"][(i + 2) * 128:(i + 3) * 128, :], [S["xres"]], [xt[i % 2]])

    load(0)
    for i in range(NLAT // 128):
        if i + 1 < NLAT // 128:
            load(i + 1)
        b = i % 2
        rms_rstd(C, xt[b][:], 128, D, K["ntmp"], K["nss"], [xt[b]])
        C.stt(yt[b][:], xt[b][:], K["nss"][:], gb[:], ALU.mult, ALU.mult, [xt[b], K["nss"], gb], [yt[b]])
        C.dma(out[i * 128:(i + 1) * 128, :], yt[b][:], [yt[b]], [C.dout_buf], eng="pool")


INPUT_SPECS = [
    ("x", [NLAT, D], F32), ("ctx", [NCTX, D], F32), ("c2", [2, D], F32),
    ("mod_w", [2, D, 6 * D], F32), ("mod_b", [2, 6 * D], F32), ("norm1_g", [2, D], F32), ("norm2_g", [2, D], F32),
    ("ab_w_in", [D, 3584], F32), ("ab_w_out", [D, D], F32), ("sgu_w", [4, 128, 128], F32), ("sgu_b", [4, 128], F32),
    ("hgrn_lb_logits", [2, 2, 512], F32), ("hgrn_norm_g", [4, 128], F32),
    ("attn_w_qkv", [D, 1536], F32), ("attn_w_out", [D, D], F32), ("attn_sink", [16], F32),
    ("router_w", [2, D, NE], F32), ("expert_w1", [2, NE, D, FF], F32), ("expert_w3", [2, NE, D, FF], F32),
    ("expert_w2", [2, NE, FF, D], F32), ("final_g", [D], F32),
    ("identb", [128, 128], BF16), ("identf", [128, 128], F32), ("onesb", [128, 128], BF16), ("ustrict", [128, 128], BF16),
    ("gla_m", [64, 2, 64], F32), ("gla_sel", [64, 2, 2], F32), ("gla_tri", [64, 2, 64], F32),
    ("amask", [128, 2, 128], BF16), ("rope_c", [NLAT, 512], F32), ("rope_s", [NLAT, 512], F32),
    ("tvals", [128, NT_ALL, 2], BF16), ("iota", [128, 512], F32),
]


def host_constants():
    bf = ml_dtypes.bfloat16
    c = {}
    c["identb"] = np.eye(128, dtype=np.float32).astype(bf)
    c["identf"] = np.eye(128, dtype=np.float32)
    c["onesb"] = np.ones((128, 128), np.float32).astype(bf)
    pp = np.arange(128)
    c["ustrict"] = (pp[:, None] < pp[None, :]).astype(np.float32).astype(bf)
    s = np.arange(64)[:, None]
    t = np.arange(64)[None, :]
    m = np.zeros((64, 2, 64), np.float32)
    m[:, 0, :] = (s <= t).astype(np.float32) - (s <= 31).astype(np.float32)
    m[:, 1, :] = (s >= t).astype(np.float32) - (s >= 32).astype(np.float32)
    c["gla_m"] = m
    sel = np.zeros((64, 2, 2), np.float32)
    sel[:, 0, 0] = (np.arange(64) <= 31)
    sel[:, 1, 0] = (np.arange(64) >= 32)
    sel[:, :, 1] = 1.0
    c["gla_sel"] = sel
    tri = np.zeros((64, 2, 64), np.float32)
    tri[:, 0, :] = (s <= t)
    tri[:, 1, :] = (s >= t)
    c["gla_tri"] = tri
    j = np.arange(128)[:, None]
    i = np.arange(128)[None, :]
    am = np.zeros((128, 2, 128), np.float32)
    am[:, 0, :] = (i <= j)
    am[:, 1, :] = (j <= i)
    c["amask"] = am.astype(bf)
    tt = np.arange(NLAT)
    inv = (10000.0 ** (-np.arange(16, dtype=np.float32) / 16)).astype(np.float32)
    ar = (tt // 64).astype(np.float32)[:, None] * inv[None, :]
    ac = (tt % 64).astype(np.float32)[:, None] * inv[None, :]
    c["rope_c"] = np.tile(np.concatenate([np.cos(ar), np.cos(ar), np.cos(ac), np.cos(ac)], axis=1).astype(np.float32), (1, 8))
    c["rope_s"] = np.tile(np.concatenate([-np.sin(ar), np.sin(ar), -np.sin(ac), np.sin(ac)], axis=1).astype(np.float32), (1, 8))
    rowid = np.arange(NT_ALL)[None, :] * 128 + np.arange(128)[:, None]
    c["tvals"] = np.stack([rowid // 64, rowid % 64], axis=-1).astype(np.float32).astype(bf)
    c["iota"] = np.broadcast_to(np.arange(512, dtype=np.float32), (128, 512)).copy()
    return c


SCRATCH_SPECS = [
    ("modv", [2, 2, 6 * D], F32), ("xres", [NALL, D], F32), ("hA", [NALL, D], BF16),
    ("ya", [NALL, 512], BF16), ("q", [NALL, 512], BF16), ("kk", [2, NALL, 512], BF16), ("gl", [2, NALL, 512], F32),
    ("v", [NALL, 512], BF16), ("og", [NALL, 512], BF16), ("o", [2, NALL, 512], F32),
]


def build_program(debug=(), stop_after=None, only=None):
    nc = bass.Bass("TRN2", target_bir_lowering=False)
    C = Ctx(nc, debug)
    K = {}
    for name, shape, dt in INPUT_SPECS:
        C.inp(name, shape, dt)
    for name, shape, dt in SCRATCH_SPECS:
        C.scr(name, shape, dt)
    C.dout_buf = Buf(nc.dram_tensor("out", [NLAT, D], F32, kind="ExternalOutput").ap(), "out")
    C.dout = C.dout_buf.t
    load_consts(C, K)
    rt_ctx = alloc_route(C, 32)
    rt_lat = alloc_route(C, 512)
    phases = [
        ("prep", lambda: phase_prep(C, K)),
        ("mixer_in", lambda: phase_mixer_in(C, K)),
        ("gla", lambda: phase_gla(C, K)),
        ("mixer_out", lambda: phase_mixer_out(C, K)),
        ("route_ctx0", lambda: phase_route(C, K, 0, 2, 32, rt_ctx)),
        ("route_lat0", lambda: phase_route(C, K, 2, 32, 512, rt_lat)),
        ("experts_lat0", lambda: phase_experts(C, K, 0, [(0, 512, rt_lat), (1, 32, rt_ctx)])),
        ("attention", lambda: phase_attention(C, K)),
        ("route_lat1", lambda: phase_route(C, K, 2, 32, 512, rt_lat)),
        ("experts_lat1", lambda: phase_experts(C, K, 1, [(0, 512, rt_lat)])),
        ("final", lambda: phase_final(C, K)),
    ]
    for name, fn in phases:
        if only is not None and name not in only:
            continue
        fn()
        if stop_after == name:
            break
    C.p.emit()
    return nc


def make_in_maps(inputs, cores):
    consts = host_constants()
    f = lambda a: np.ascontiguousarray(np.asarray(a, dtype=np.float32))
    shared = {
        "mod_w": f(inputs["mod_w"]), "mod_b": f(inputs["mod_b"]), "norm1_g": f(inputs["norm1_g"]), "norm2_g": f(inputs["norm2_g"]),
        "ab_w_in": f(inputs["ab_w_in"][0]), "ab_w_out": f(inputs["ab_w_out"][0]), "sgu_w": f(inputs["sgu_w"][0]), "sgu_b": f(inputs["sgu_b"][0]),
        "hgrn_lb_logits": f(inputs["hgrn_lb_logits"]), "hgrn_norm_g": f(inputs["hgrn_norm_g"][0]),
        "attn_w_qkv": f(inputs["attn_w_qkv"][0]), "attn_w_out": f(inputs["attn_w_out"][0]), "attn_sink": f(inputs["attn_sink"][0]),
        "router_w": f(inputs["router_w"]), "expert_w1": f(inputs["expert_w1"]), "expert_w3": f(inputs["expert_w3"]),
        "expert_w2": f(inputs["expert_w2"]), "final_g": f(inputs["final_g"]),
    }
    shared.update(consts)
    maps = []
    for b in cores:
        m = dict(shared)
        m["x"] = f(inputs["x"][b])
        m["ctx"] = f(inputs["ctx"][b])
        m["c2"] = np.ascontiguousarray(np.stack([f(inputs["c"][b]), f(inputs["c_ctx"])], axis=0))
        maps.append(m)
    return maps


def kernel(**inputs):
    nc = build_program()
    maps = make_in_maps(inputs, list(range(8)))
    res = run_bass_kernel_spmd(nc, maps, core_ids=list(range(8)))
    return np.stack([np.asarray(r["out"], dtype=np.float32) for r in res.results], axis=0)
```

```python
import contextlib
import numpy as np
import ml_dtypes
import concourse.bass as bass
import concourse.mybir as mybir
from concourse.bass_utils import run_bass_kernel_spmd

F32 = mybir.dt.float32
BF16 = mybir.dt.bfloat16
I32 = mybir.dt.int32
U8 = mybir.dt.uint8
ALU = mybir.AluOpType
AF = mybir.ActivationFunctionType
AX = mybir.AxisListType
ISZ = {F32: 4, BF16: 2, I32: 4}

D = 1024
NLAT = 4096
NCTX = 256
NALL = NLAT + NCTX
NT_ALL = NALL // 128
EPS = 1e-6
NE = 16
FF = 2048

ENGS = ("pe", "act", "dve", "pool", "sp")
N_DMA_SEMS = 32
import os as _os
STRICT_SAME_ENGINE = _os.environ.get('STRICT_SAME_ENGINE', '0') == '1'


class Buf:
    def __init__(self, t, name, root=None):
        self.t = t
        self.name = name
        self.root = root if root is not None else self
        if root is None:
            self.whole = [None, []]
            self.subs = {}

    def __getitem__(self, idx):
        return self.t[idx]

    def sub(self, key):
        return (self.root, key)

    def view(self, ap):
        return Buf(ap, self.name + "_v", root=self.root)


def _nk(k):
    return (k.root, None) if isinstance(k, Buf) else (k[0].root, k[1])


class Prog:
    def __init__(self, nc):
        self.nc = nc
        self.ops = {e: [] for e in ENGS}
        self.dma_count = [0] * N_DMA_SEMS
        self.dma_last = [None] * N_DMA_SEMS
        self.dma_rr = 0
        self.dma_rr_pool = 0
        self.bar = {}

    def _states(self, key):
        buf, sk = _nk(key)
        if sk is None:
            return buf, sk, [buf.whole] + list(buf.subs.values())
        if sk not in buf.subs:
            buf.subs[sk] = [None, []]
        return buf, sk, [buf.whole, buf.subs[sk]]

    def barrier(self):
        toks = set()
        for e in ENGS:
            for i in range(len(self.ops[e]) - 1, -1, -1):
                if self.ops[e][i]["dma"] is None:
                    toks.add(("e", e, i))
                    break
        for t in self.dma_last:
            if t is not None:
                toks.add(t)
        self.bar = {e: set(toks) for e in ENGS}

    def op(self, eng, fn, reads=(), writes=(), dma=False):
        deps = set()
        if self.bar.get(eng):
            deps |= self.bar.pop(eng)
        for k in reads:
            _, _, sts = self._states(k)
            for st in sts:
                if st[0] is not None:
                    deps.add(st[0])
        for k in writes:
            _, _, sts = self._states(k)
            for st in sts:
                if st[0] is not None:
                    deps.add(("W",) + st[0])
                for r in st[1]:
                    deps.add(("W",) + r)
        idx = len(self.ops[eng])
        rec = dict(fn=fn, deps=[], sig=False, dma=None)
        if dma:
            half = N_DMA_SEMS // 2
            if eng == "pool":
                s = half + self.dma_rr_pool % half
                self.dma_rr_pool += 1
            else:
                s = self.dma_rr % half
                self.dma_rr += 1
            prev = self.dma_last[s]
            self.dma_count[s] += 16
            tok = ("d", s, self.dma_count[s])
            rec["dma"] = (s, self.dma_count[s])
            if prev is not None:
                deps.add(prev)
            self.dma_last[s] = tok
        else:
            tok = ("e", eng, idx)
        final = set()
        for d in deps:
            war = d[0] == "W"
            if war:
                d = d[1:]
            if d[0] == "e" and d[1] == eng and not dma and war and (eng == "pe" or not STRICT_SAME_ENGINE):
                continue
            if d != tok:
                final.add(d)
        for d in final:
            if d[0] == "e":
                self.ops[d[1]][d[2]]["sig"] = True
        rec["deps"] = sorted(final, key=str)
        self.ops[eng].append(rec)
        for k in reads:
            buf, sk, _ = self._states(k)
            rl = (buf.whole if sk is None else buf.subs[sk])[1]
            if tok[0] == "e":
                rl[:] = [t for t in rl if not (t[0] == "e" and t[1] == tok[1])]
            else:
                rl[:] = [t for t in rl if not (t[0] == "d" and t[1] == tok[1])]
            rl.append(tok)
        for k in writes:
            buf, sk, _ = self._states(k)
            if sk is None:
                buf.whole = [tok, []]
                buf.subs = {}
            else:
                buf.subs[sk] = [tok, []]
        return tok

    def dma(self, out, in_, reads=(), writes=(), eng="sp", **kw):
        return self.op(eng, lambda e: e.dma_start(out=out, in_=in_, **kw), reads, writes, dma=True)

    def emit(self):
        nc = self.nc
        with contextlib.ExitStack() as es:
            esem = {e: es.enter_context(nc.semaphore(f"s_{e}")) for e in ENGS}
            dsem = [es.enter_context(nc.semaphore(f"s_dma{i}")) for i in range(N_DMA_SEMS)]
            sigcnt = {}
            for e in ENGS:
                c = 0
                for i, r in enumerate(self.ops[e]):
                    if r["sig"] and r["dma"] is None:
                        c += 1
                        sigcnt[(e, i)] = c
            es.enter_context(nc.allow_non_contiguous_dma(reason="small strided loads of parameters"))
            block = es.enter_context(nc.Block())
            final_dma = [(s, self.dma_count[s]) for s in range(N_DMA_SEMS) if self.dma_count[s] > 0]

            def run(engname, eobj):
                waited = {}
                for r in self.ops[engname]:
                    for d in r["deps"]:
                        if d[0] == "e":
                            sem, val, key = esem[d[1]], sigcnt[(d[1], d[2])], ("e", d[1])
                        else:
                            sem, val, key = dsem[d[1]], d[2], ("d", d[1])
                        if waited.get(key, 0) >= val:
                            continue
                        waited[key] = val
                        eobj.wait_ge(sem, val)
                    ins = r["fn"](eobj)
                    if r["dma"] is not None:
                        ins.then_inc(dsem[r["dma"][0]], 16)
                    elif r["sig"]:
                        ins.then_inc(esem[engname], 1)
                if engname == "sp":
                    for s, v in final_dma:
                        eobj.wait_ge(dsem[s], v)

            block.sync(lambda e: run("sp", e))
            block.tensor(lambda e: run("pe", e))
            block.scalar(lambda e: run("act", e))
            block.vector(lambda e: run("dve", e))
            block.gpsimd(lambda e: run("pool", e))


ARENA_BYTES = 200 * 1024


class Ctx:
    def __init__(self, nc, debug=()):
        self.nc = nc
        self.p = Prog(nc)
        self.debug = set(debug)
        self.arena = nc.alloc_sbuf_tensor("arena", [128, ARENA_BYTES], U8)
        self.persist_top = 0
        self.off = 0
        self.cnt = 0
        self.banks = [Buf(nc.alloc_psum_tensor(f"psb{i}", [128, 512], F32)[:], f"psb{i}") for i in range(8)]
        self.bank_rr = 0
        self.din = {}
        self.dscr = {}

    def _carve(self, shape, dt, off):
        nb = int(np.prod(shape[1:])) * ISZ[dt]
        t = self.arena[0:shape[0], off:off + nb].bitcast(dt)
        if len(shape) == 3:
            t = t.rearrange("p (a b) -> p a b", a=shape[1])
        elif len(shape) == 4:
            t = t.rearrange("p (a b c) -> p a b c", a=shape[1], b=shape[2])
        return t, (nb + 63) // 64 * 64

    def tile(self, shape, dt, name=None, persist=False):
        self.cnt += 1
        name = name or f"t{self.cnt}"
        if persist:
            assert self.off == self.persist_top, "persistent tiles must be allocated at phase start"
        t, nb = self._carve(shape, dt, self.off)
        self.off += nb
        assert self.off <= ARENA_BYTES, f"SBUF arena overflow {self.off}"
        if persist:
            self.persist_top = self.off
        return Buf(t, name)

    def new_phase(self):
        self.p.barrier()
        self.off = self.persist_top

    def bank(self):
        b = self.banks[self.bank_rr % 8]
        self.bank_rr += 1
        return b

    def inp(self, name, shape, dt):
        self.din[name] = Buf(self.nc.dram_tensor(name, list(shape), dt, kind="ExternalInput").ap(), name)
        return self.din[name]

    def scr(self, name, shape, dt):
        kind = "ExternalOutput" if name in self.debug else "Internal"
        self.dscr[name] = Buf(self.nc.dram_tensor(name, list(shape), dt, kind=kind).ap(), name)
        return self.dscr[name]

    def act(self, out, in_, func, reads, writes, **kw):
        return self.p.op("act", lambda e: e.activation(out=out, in_=in_, func=func, **kw), reads, writes)

    def tt(self, out, in0, in1, op, reads, writes, eng="dve"):
        return self.p.op(eng, lambda e: e.tensor_tensor(out=out, in0=in0, in1=in1, op=op), reads, writes)

    def ts(self, out, in0, s1, s2, op0, op1, reads, writes, eng="dve"):
        if op1 is None:
            return self.p.op(eng, lambda e: e.tensor_scalar(out=out, in0=in0, scalar1=s1, scalar2=None, op0=op0), reads, writes)
        return self.p.op(eng, lambda e: e.tensor_scalar(out=out, in0=in0, scalar1=s1, scalar2=s2, op0=op0, op1=op1), reads, writes)

    def stt(self, out, in0, scalar, in1, op0, op1, reads, writes, eng="dve"):
        return self.p.op(eng, lambda e: e.scalar_tensor_tensor(out=out, in0=in0, scalar=scalar, in1=in1, op0=op0, op1=op1), reads, writes)

    def copy(self, out, in_, reads, writes, eng="dve"):
        if eng == "act":
            return self.p.op("act", lambda e: e.copy(out=out, in_=in_), reads, writes)
        return self.p.op(eng, lambda e: e.tensor_copy(out=out, in_=in_), reads, writes)

    def mm(self, out, lhsT, rhs, start, stop, reads, writes):
        return self.p.op("pe", lambda e: e.matmul(out, lhsT=lhsT, rhs=rhs, start=start, stop=stop), reads, writes)

    def tr(self, out, in_, ident, reads, writes):
        return self.p.op("pe", lambda e: e.transpose(out=out, in_=in_, identity=ident), reads, writes)

    def reduce(self, out, in_, op, reads, writes):
        return self.p.op("dve", lambda e: e.tensor_reduce(out=out, in_=in_, axis=AX.X, op=op), reads, writes)

    def recip(self, out, in_, reads, writes):
        return self.p.op("dve", lambda e: e.reciprocal(out=out, in_=in_), reads, writes)

    def memset(self, ap, val, writes, eng="pool"):
        return self.p.op(eng, lambda e: e.memset(ap, val), (), writes)

    def dma(self, out, in_, reads=(), writes=(), eng="sp", **kw):
        return self.p.dma(out, in_, reads, writes, eng, **kw)


def interleave(*gens):
    gens = [g for g in gens if g is not None]
    while gens:
        for g in list(gens):
            try:
                next(g)
            except StopIteration:
                gens.remove(g)


def bc_mid(ap2d, n):
    P, Fd = ap2d.shape
    return ap2d.unsqueeze(1).to_broadcast([P, n, Fd])


def bc_last(ap2d, n):
    P, A = ap2d.shape
    return ap2d.unsqueeze(2).to_broadcast([P, A, n])


def rms_rstd(C, x, npart, width, tmp, ss, reads):
    C.act(tmp[0:npart, 0:width], x, AF.Square, reads, [tmp, ss], accum_out=ss[0:npart, :])
    C.ts(ss[0:npart, :], ss[0:npart, :], 1.0 / width, EPS, ALU.mult, ALU.add, [ss], [ss])
    C.act(ss[0:npart, :], ss[0:npart, :], AF.Sqrt, [ss], [ss])
    C.recip(ss[0:npart, :], ss[0:npart, :], [ss], [ss])


def norm_mod_transpose(C, K, xt, gs, sh, h_bf, hT, want_T=True):
    tmp, ss = K["ntmp"], K["nss"]
    rms_rstd(C, xt[:], 128, D, tmp, ss, [xt])
    C.stt(tmp[:], xt[:], ss[:], gs[:], ALU.mult, ALU.mult, [xt, ss, gs, tmp], [tmp])
    C.tt(h_bf[:], tmp[:], sh[:], ALU.add, [tmp, sh], [h_bf])
    if want_T:
        transpose_1024(C, K, h_bf, hT)


def transpose_1024(C, K, src_bf, dstT, npart=128, col0=0):
    bk = C.bank()
    pb = bk[:].bitcast(BF16)
    for c in range(8):
        C.tr(pb[:, c * 128:c * 128 + npart], src_bf[0:npart, c * 128:(c + 1) * 128], K["identb"][0:npart, 0:npart],
             [src_bf, K["identb"]], [bk])
    src = pb.rearrange("p (c t) -> p c t", c=8)[:, :, 0:npart]
    C.copy(dstT[:, :, col0:col0 + npart], src, [bk], [dstT], eng="act")


def load_w_bf16(C, dst, w_ap, ncols, nchunks, col0=0, step=512):
    for n0 in range(0, ncols, step):
        n1 = min(ncols, n0 + step)
        C.dma(dst[:, :, col0 + n0:col0 + n1], w_ap[:, n0:n1].rearrange("(c p) n -> p c n", p=128), (), [dst], eng="pool")


def phase_prep(C, K):
    C.new_phase()
    I = C.din
    modv = C.dscr["modv"]
    cs = C.tile([128, 8, 2], F32)
    c2s = C.tile([2, D], F32)
    C.dma(c2s[:], I["c2"][:], (), [c2s])
    C.act(c2s[:], c2s[:], AF.Silu, [c2s], [c2s])
    bk = C.bank()
    for c in range(8):
        C.tr(bk[:, 2 * c:2 * c + 2], c2s[:, c * 128:(c + 1) * 128], K["identf"][0:2, 0:2], [c2s, K["identf"]], [bk])
    C.copy(cs[:].rearrange("p c r -> p (c r)"), bk[:, 0:16], [bk], [cs])
    wbuf = [C.tile([128, 8, 512], F32) for _ in range(2)]
    mv = C.tile([2, 6 * D], F32)
    mb = C.tile([2, 6 * D], F32)
    ng = C.tile([2, D], F32)
    k = 0
    for l in range(2):
        C.dma(mb[:], I["mod_b"][l, :].partition_broadcast(2), (), [mb])
        for n in range(12):
            w = wbuf[k % 2]
            k += 1
            C.dma(w[:], I["mod_w"][l, :, n * 512:(n + 1) * 512].rearrange("(c p) n -> p c n", p=128), (), [w])
            bk = C.bank()
            for c in range(8):
                C.mm(bk[0:2, :], cs[:, c, :], w[:, c, :], c == 0, c == 7, [cs, w], [bk])
            C.tt(mv[:, n * 512:(n + 1) * 512], bk[0:2, :], mb[:, n * 512:(n + 1) * 512], ALU.add, [bk, mb], [mv])
        for slot, gname in ((1, "norm1_g"), (4, "norm2_g")):
            C.dma(ng[:], I[gname][l, :].partition_broadcast(2), (), [ng])
            C.stt(mv[:, slot * D:(slot + 1) * D], mv[:, slot * D:(slot + 1) * D], 1.0, ng[:], ALU.add, ALU.mult, [mv, ng], [mv])
        C.dma(modv[l, :, :], mv[:], [mv], [modv])


def load_mod(C, dst, l, r, slot):
    C.dma(dst[:], C.dscr["modv"][l, r, slot * D:(slot + 1) * D].partition_broadcast(128), [C.dscr["modv"]], [dst])


def alloc_norm_scratch(C, K):
    K["ntmp"] = C.tile([128, D], F32)
    K["nss"] = C.tile([128, 1], F32)


def load_consts(C, K):
    I = C.din
    for name, shape, dt in (("identb", [128, 128], BF16), ("identf", [128, 128], F32), ("onesb", [128, 128], BF16),
                            ("ustrict", [128, 128], BF16)):
        K[name] = C.tile(shape, dt, name, persist=True)
        C.dma(K[name][:], I[name][:], (), [K[name]])
    K["logits"] = C.tile([128, NT_ALL, NE], F32, "logits", persist=True)


def phase_mixer_in(C, K):
    C.new_phase()
    I, S = C.din, C.dscr
    Kp = []
    for _ in range(2):
        kk_ = dict(K)
        alloc_norm_scratch(C, kk_)
        Kp.append(kk_)
    w_in = C.tile([128, 8, 3584], BF16)
    load_w_bf16(C, w_in, I["ab_w_in"][:], 3584, 8)
    ws = C.tile([128, 4, 128], F32)
    wsT = C.tile([128, 4, 128], BF16)
    C.dma(ws[:], I["sgu_w"][:].rearrange("g t s -> t g s"), (), [ws])
    bk = C.bank()
    for g in range(4):
        C.tr(bk[:, g * 128:(g + 1) * 128], ws[:, g, :], K["identf"][:], [ws, K["identf"]], [bk])
    C.copy(wsT[:].rearrange("p g t -> p (g t)"), bk[:], [bk], [wsT])
    bsT = C.tile([128, 4], F32)
    C.dma(bsT[:], I["sgu_b"][:].rearrange("g t -> t g"), (), [bsT])
    lbl = C.tile([128, 2, 2, 512], F32)
    C.dma(lbl[:].rearrange("p a b c -> p (a b c)"), I["hgrn_lb_logits"][:].rearrange("a b c -> (a b c)").partition_broadcast(128), (), [lbl])
    lb = C.tile([128, 2, 512], F32)
    oml = C.tile([128, 2, 512], F32)
    C.tt(lb[:], lbl[:, 0, :, :], lbl[:, 1, :, :], ALU.subtract, [lbl], [lb])
    C.act(lb[:], lb[:], AF.Sigmoid, [lb], [lb])
    C.ts(oml[:], lb[:], -1.0, 1.0, ALU.mult, ALU.add, [lb], [oml])
    mods = {}
    for r in range(2):
        gs = C.tile([128, D], F32)
        sh = C.tile([128, D], F32)
        load_mod(C, gs, 0, r, 1)
        load_mod(C, sh, 0, r, 0)
        mods[r] = (gs, sh)
    xt = [C.tile([128, D], F32) for _ in range(4)]
    h_bf2 = [C.tile([128, D], BF16) for _ in range(2)]
    hT2 = [C.tile([128, 8, 128], BF16) for _ in range(2)]
    u2 = [C.tile([128, 4, 128], F32) for _ in range(2)]
    v2 = [C.tile([128, 4, 128], F32) for _ in range(2)]
    sq2 = [C.tile([128, 4, 128], F32) for _ in range(2)]
    st42 = [C.tile([128, 4], F32) for _ in range(2)]
    st4b2 = [C.tile([128, 4], F32) for _ in range(2)]
    vn2 = [C.tile([128, 4, 128], BF16) for _ in range(2)]
    f32t4 = [C.tile([128, 512], F32) for _ in range(4)]
    ya = [C.tile([128, 512], BF16) for _ in range(2)]
    qo = [C.tile([128, 512], BF16) for _ in range(2)]
    kk = [C.tile([128, 512], BF16) for _ in range(4)]
    gl = [C.tile([128, 512], F32) for _ in range(4)]
    vo = [C.tile([128, 512], BF16) for _ in range(2)]
    og = [C.tile([128, 512], BF16) for _ in range(2)]

    def src_rows(i):
        if i < 2:
            return I["ctx"][i * 128:(i + 1) * 128, :]
        return I["x"][(i - 2) * 128:(i - 1) * 128, :]

    def load(i):
        C.dma(xt[i % 4][:], src_rows(i), (), [xt[i % 4]])

    def tile_body(i):
        b = i % 2
        r = 1 if i < 2 else 0
        rows = slice(i * 128, (i + 1) * 128)
        h_bf, hT, u, v, sq, st4, st4b, vn = h_bf2[b], hT2[b], u2[b], v2[b], sq2[b], st42[b], st4b2[b], vn2[b]
        norm_mod_transpose(C, Kp[b], xt[i % 4], mods[r][0], mods[r][1], h_bf, hT)
        yield
        zb = []
        for n in range(7):
            bk = C.bank()
            for c in range(8):
                C.mm(bk[:], hT[:, c, :], w_in[:, c, n * 512:(n + 1) * 512], c == 0, c == 7, [hT, w_in], [bk])
            zb.append(bk)
        C.act(u[:].rearrange("p g c -> p (g c)"), zb[0][:], AF.Gelu, [zb[0]], [u])
        C.act(v[:].rearrange("p g c -> p (g c)"), zb[1][:], AF.Gelu, [zb[1]], [v])
        C.reduce(st4[:], v[:], ALU.add, [v], [st4])
        C.ts(st4[:], st4[:], 1.0 / 128, None, ALU.mult, None, [st4], [st4])
        C.tt(v[:], v[:], bc_last(st4[:], 128), ALU.subtract, [v, st4], [v])
        C.act(sq[:], v[:], AF.Square, [v], [sq])
        C.reduce(st4b[:], sq[:], ALU.add, [sq], [st4b])
        C.ts(st4b[:], st4b[:], 1.0 / 128, EPS, ALU.mult, ALU.add, [st4b], [st4b])
        C.act(st4b[:], st4b[:], AF.Sqrt, [st4b], [st4b])
        C.recip(st4b[:], st4b[:], [st4b], [st4b])
        C.tt(vn[:], v[:], bc_last(st4b[:], 128), ALU.mult, [v, st4b], [vn])
        bk = C.bank()
        for g in range(4):
            C.mm(bk[:, g * 128:(g + 1) * 128], wsT[:, g, :], vn[:, g, :], True, True, [wsT, vn], [bk])
        for g in range(4):
            C.stt(ya[b][:, g * 128:(g + 1) * 128], bk[:, g * 128:(g + 1) * 128], bsT[:, g:g + 1], u[:, g, :],
                  ALU.add, ALU.mult, [bk, bsT, u], [ya[b]])
        C.dma(S["ya"][rows, :], ya[b][:], [ya[b]], [S["ya"].sub(i)], eng="pool")
        C.act(qo[b][:], zb[2][:], AF.Silu, [zb[2]], [qo[b]])
        C.dma(S["q"][rows, :], qo[b][:], [qo[b]], [S["q"].sub(i)], eng="pool")
        for d in range(2):
            kb_, gb_ = kk[2 * b + d], gl[2 * b + d]
            f32t = f32t4[2 * b + d]
            C.act(f32t[:], zb[3 + d][:], AF.Sigmoid, [zb[3 + d]], [f32t])
            C.tt(f32t[:], f32t[:], oml[:, d, :], ALU.mult, [f32t, oml], [f32t])
            C.tt(f32t[:], f32t[:], lb[:, d, :], ALU.add, [f32t, lb], [f32t])
            C.ts(kb_[:], f32t[:], -1.0, 1.0, ALU.mult, ALU.add, [f32t], [kb_])
            C.act(gb_[:], f32t[:], AF.Ln, [f32t], [gb_])
            C.dma(S["kk"][d, rows, :], kb_[:], [kb_], [S["kk"].sub((d, i))], eng="pool")
            C.dma(S["gl"][d, rows, :], gb_[:], [gb_], [S["gl"].sub((d, i))], eng="pool")
        C.copy(vo[b][:], zb[5][:], [zb[5]], [vo[b]], eng="act")
        C.dma(S["v"][rows, :], vo[b][:], [vo[b]], [S["v"].sub(i)], eng="pool")
        C.act(og[b][:], zb[6][:], AF.Silu, [zb[6]], [og[b]])
        C.dma(S["og"][rows, :], og[b][:], [og[b]], [S["og"].sub(i)], eng="pool")

    load(0)
    load(1)
    for i in range(0, NT_ALL, 2):
        for j in (i + 2, i + 3):
            if j < NT_ALL:
                load(j)
        interleave(tile_body(i), tile_body(i + 1))


def phase_gla(C, K):
    C.new_phase()
    I, S = C.din, C.dscr
    NCH = NALL // 64
    cst = {}
    for name, shape, dt in (("gla_m", [64, 2, 64], F32), ("gla_sel", [64, 2, 2], F32), ("gla_tri", [64, 2, 64], F32)):
        cst[name] = C.tile(shape, dt)
        C.dma(cst[name][:], I[name][:], (), [cst[name]])
    St = [C.tile([128, 4, 128], F32) for _ in range(2)]
    for d in range(2):
        C.memset(St[d][:], 0.0, [St[d]])
    NB = 2
    bufs = {}
    for d in range(2):
        for j in range(NB):
            bufs[(d, j)] = dict(
                q=C.tile([64, 512], BF16), kk=C.tile([64, 512], BF16), g=C.tile([64, 512], F32), v=C.tile([64, 512], BF16),
                ep=C.tile([64, 512], F32), em=C.tile([64, 512], F32), qb=C.tile([64, 512], BF16), kb=C.tile([64, 512], BF16),
                qkT=C.tile([128, 512], BF16), e3=C.tile([128, 4, 3], F32), ex=C.tile([128, 4, 3], F32),
                at=C.tile([64, 4, 64], BF16), ssc=C.tile([128, 4, 128], BF16), osb=C.tile([64, 512], F32),
                tmp=C.tile([128, 4, 128], F32))
    order = {0: list(range(NCH)), 1: [3, 2, 1, 0] + list(range(NCH - 1, 3, -1))}

    def load(d, step):
        ci = order[d][step]
        B = bufs[(d, step % NB)]
        rows = slice(ci * 64, (ci + 1) * 64)
        t = ci // 2
        C.dma(B["q"][:], S["q"][rows, :], [S["q"].sub(t)], [B["q"]])
        C.dma(B["kk"][:], S["kk"][d, rows, :], [S["kk"].sub((d, t))], [B["kk"]])
        C.dma(B["g"][:], S["gl"][d, rows, :], [S["gl"].sub((d, t))], [B["g"]])
        C.dma(B["v"][:], S["v"][rows, :], [S["v"].sub(t)], [B["v"]])

    def compute(d, step):
        ci = order[d][step]
        B = bufs[(d, step % NB)]
        rows = slice(ci * 64, (ci + 1) * 64)
        Sd = St[d]
        idb = K["identb"]
        bps = C.bank()
        C.mm(bps[0:64, :], cst["gla_m"][:, d, :], B["g"][:], True, True, [cst["gla_m"], B["g"]], [bps])
        C.act(B["ep"][:], bps[0:64, :], AF.Exp, [bps], [B["ep"]])
        C.act(B["em"][:], bps[0:64, :], AF.Exp, [bps], [B["em"]], scale=-1.0)
        C.tt(B["qb"][:], B["q"][:], B["ep"][:], ALU.mult, [B["q"], B["ep"]], [B["qb"]])
        C.tt(B["kb"][:], B["kk"][:], B["em"][:], ALU.mult, [B["kk"], B["em"]], [B["kb"]])
        yield
        tb = C.bank()
        pb = tb[:].bitcast(BF16)
        for h in range(4):
            C.tr(pb[:, h * 64:(h + 1) * 64], B["qb"][:, h * 128:(h + 1) * 128], idb[0:64, 0:64], [B["qb"], idb], [tb])
        for h in range(4):
            C.tr(pb[:, 256 + h * 64:256 + (h + 1) * 64], B["kb"][:, h * 128:(h + 1) * 128], idb[0:64, 0:64], [B["kb"], idb], [tb])
        C.copy(B["qkT"][:], pb[:, 0:512], [tb], [B["qkT"]], eng="act")
        yield
        sps = C.bank()
        for h in range(4):
            C.mm(sps[:, h * 2:h * 2 + 2], B["g"][:, h * 128:(h + 1) * 128], cst["gla_sel"][:, d, :], True, True,
                 [B["g"], cst["gla_sel"]], [sps])
        spv = sps[:, 0:8].rearrange("p (h c) -> p h c", c=2)
        C.copy(B["e3"][:, :, 0:2], spv, [sps], [B["e3"]])
        C.tt(B["e3"][:, :, 2], B["e3"][:, :, 1], B["e3"][:, :, 0], ALU.subtract, [B["e3"]], [B["e3"]])
        C.act(B["ex"][:], B["e3"][:], AF.Exp, [B["e3"]], [B["ex"]])
        yield
        aps = C.bank()
        for h in range(4):
            C.mm(aps[0:64, h * 64:(h + 1) * 64], B["qkT"][:, 256 + h * 64:256 + (h + 1) * 64], B["qkT"][:, h * 64:(h + 1) * 64],
                 True, True, [B["qkT"]], [aps])
        C.tt(B["at"][:], aps[0:64, 0:256].rearrange("p (h t) -> p h t", h=4), bc_mid(cst["gla_tri"][:, d, :], 4), ALU.mult,
             [aps, cst["gla_tri"]], [B["at"]])
        yield
        C.tt(B["ssc"][:], Sd[:], bc_last(B["ex"][:, :, 0], 128), ALU.mult, [Sd, B["ex"]], [B["ssc"]], eng="pool")
        ops_ = C.bank()
        for h in range(4):
            C.mm(ops_[0:64, h * 128:(h + 1) * 128], B["at"][:, h, :], B["v"][:, h * 128:(h + 1) * 128], True, False,
                 [B["at"], B["v"]], [ops_])
            C.mm(ops_[0:64, h * 128:(h + 1) * 128], B["qkT"][:, h * 64:(h + 1) * 64], B["ssc"][:, h, :], False, True,
                 [B["qkT"], B["ssc"]], [ops_])
        C.copy(B["osb"][:], ops_[0:64, :], [ops_], [B["osb"]], eng="act")
        C.dma(S["o"][d, rows, :], B["osb"][:], [B["osb"]], [S["o"].sub((d, ci))], eng="sp")
        yield
        dps = C.bank()
        for h in range(4):
            C.mm(dps[:, h * 128:(h + 1) * 128], B["kb"][:, h * 128:(h + 1) * 128], B["v"][:, h * 128:(h + 1) * 128], True, True,
                 [B["kb"], B["v"]], [dps])
        C.tt(B["tmp"][:], dps[:].rearrange("p (h v) -> p h v", h=4), bc_last(B["ex"][:, :, 2], 128), ALU.mult, [dps, B["ex"]], [B["tmp"]])
        C.tt(Sd[:], Sd[:], bc_last(B["ex"][:, :, 1], 128), ALU.mult, [Sd, B["ex"]], [Sd], eng="pool")
        C.tt(Sd[:], Sd[:], B["tmp"][:], ALU.add, [Sd, B["tmp"]], [Sd], eng="pool")

    for d in range(2):
        load(d, 0)
    for step in range(NCH):
        for d in range(2):
            if step + 1 < NCH:
                load(d, step + 1)
        interleave(compute(0, step), compute(1, step))


def residual_norm2_router(C, K, W, i, l, r, ybanks, xt, mods2, bufs):
    S = C.dscr
    g1b, gs2, sh2 = mods2
    x1, h2, h2T = bufs
    rows = slice(i * 128, (i + 1) * 128)
    if x1 is None:
        tmp = K["ntmp"]
        for n in range(2):
            C.tt(tmp[:, n * 512:(n + 1) * 512], ybanks[n][:], g1b[:, n * 512:(n + 1) * 512], ALU.mult, [ybanks[n], g1b], [tmp])
        C.tt(xt[:], tmp[:], xt[:], ALU.add, [tmp, xt], [xt])
        x1 = xt
    else:
        for n in range(2):
            C.tt(x1[:, n * 512:(n + 1) * 512], ybanks[n][:], g1b[:, n * 512:(n + 1) * 512], ALU.mult, [ybanks[n], g1b], [x1])
        C.tt(x1[:], x1[:], xt[:], ALU.add, [x1, xt], [x1])
    C.dma(S["xres"][rows, :], x1[:], [x1], [S["xres"].sub(i)], eng="pool")
    norm_mod_transpose(C, K, x1, gs2, sh2, h2, h2T)
    C.dma(S["hA"][rows, :], h2[:], [h2], [S["hA"].sub(i)], eng="pool")
    bk = C.bank()
    for c in range(8):
        C.mm(bk[:, 0:NE], h2T[:, c, :], W["wr"][:, c, :], c == 0, c == 7, [h2T, W["wr"]], [bk])
    C.copy(K["logits"][:, i, :], bk[:, 0:NE], [bk], [K["logits"].sub(i)])


def load_router_w(C, l):
    wr = C.tile([128, 8, NE], BF16)
    C.dma(wr[:], C.din["router_w"][l, :, :].rearrange("(c p) n -> p c n", p=128), (), [wr], eng="pool")
    return wr


def phase_mixer_out(C, K):
    C.new_phase()
    I, S = C.din, C.dscr
    Kp = []
    for _ in range(2):
        kk_ = dict(K)
        alloc_norm_scratch(C, kk_)
        Kp.append(kk_)
    w_out = C.tile([128, 8, D], BF16)
    load_w_bf16(C, w_out, I["ab_w_out"][:], D, 8)
    W = dict(wr=load_router_w(C, 0))
    ngb = C.tile([128, 512], F32)
    C.dma(ngb[:], I["hgrn_norm_g"][:].rearrange("h v -> (h v)").partition_broadcast(128), (), [ngb])
    mods2 = {}
    for r in range(2):
        g1b, gs2, sh2 = C.tile([128, D], F32), C.tile([128, D], F32), C.tile([128, D], F32)
        load_mod(C, g1b, 0, r, 2)
        load_mod(C, gs2, 0, r, 4)
        load_mod(C, sh2, 0, r, 3)
        mods2[r] = (g1b, gs2, sh2)
    NB = 4
    of = [C.tile([128, 4, 128], F32) for _ in range(NB)]
    ob = [C.tile([128, 4, 128], F32) for _ in range(NB)]
    ogt = [C.tile([128, 512], BF16) for _ in range(NB)]
    ycat = [C.tile([128, D], BF16) for _ in range(NB)]
    xt = [C.tile([128, D], F32) for _ in range(NB)]
    sq2 = [C.tile([128, 4, 128], F32) for _ in range(2)]
    st42 = [C.tile([128, 4], F32) for _ in range(2)]
    ycT2 = [C.tile([128, 8, 128], BF16) for _ in range(2)]
    h22 = [C.tile([128, D], BF16) for _ in range(2)]
    h2T2 = [C.tile([128, 8, 128], BF16) for _ in range(2)]

    def load(i):
        b = i % NB
        rows = slice(i * 128, (i + 1) * 128)
        C.dma(of[b][:].rearrange("p h v -> p (h v)"), S["o"][0, rows, :], [S["o"]], [of[b]])
        C.dma(ob[b][:].rearrange("p h v -> p (h v)"), S["o"][1, rows, :], [S["o"]], [ob[b]])
        C.dma(ogt[b][:], S["og"][rows, :], [S["og"]], [ogt[b]])
        C.dma(ycat[b][:, 0:512], S["ya"][rows, :], [S["ya"]], [ycat[b]])
        src = I["ctx"][i * 128:(i + 1) * 128, :] if i < 2 else I["x"][(i - 2) * 128:(i - 1) * 128, :]
        C.dma(xt[b][:], src, (), [xt[b]])

    def tile_body(i):
        b = i % NB
        pb_ = i % 2
        sq, st4, ycT, h2, h2T = sq2[pb_], st42[pb_], ycT2[pb_], h22[pb_], h2T2[pb_]
        r = 1 if i < 2 else 0
        o = of[b]
        C.tt(o[:], o[:], ob[b][:], ALU.add, [o, ob[b]], [o])
        C.act(sq[:], o[:], AF.Square, [o], [sq])
        C.reduce(st4[:], sq[:], ALU.add, [sq], [st4])
        C.ts(st4[:], st4[:], 1.0 / 128, EPS, ALU.mult, ALU.add, [st4], [st4])
        C.act(st4[:], st4[:], AF.Sqrt, [st4], [st4])
        C.recip(st4[:], st4[:], [st4], [st4])
        C.tt(o[:], o[:], bc_last(st4[:], 128), ALU.mult, [o, st4], [o])
        of2 = o[:].rearrange("p h v -> p (h v)")
        C.tt(of2, of2, ngb[:], ALU.mult, [o, ngb], [o])
        C.tt(ycat[b][:, 512:1024], of2, ogt[b][:], ALU.mult, [o, ogt[b], ycat[b]], [ycat[b]])
        yield
        transpose_1024(C, K, ycat[b], ycT)
        yield
        yb = []
        for n in range(2):
            bk = C.bank()
            for c in range(8):
                C.mm(bk[:], ycT[:, c, :], w_out[:, c, n * 512:(n + 1) * 512], c == 0, c == 7, [ycT, w_out], [bk])
            yb.append(bk)
        residual_norm2_router(C, Kp[pb_], W, i, 0, r, yb, xt[b], mods2[r], (None, h2, h2T))

    load(0)
    load(1)
    for i in range(0, NT_ALL, 2):
        for j in (i + 2, i + 3):
            if j < NT_ALL:
                load(j)
        interleave(tile_body(i), tile_body(i + 1))


def phase_route(C, K, tile0, ntl, cap, rt):
    C.new_phase()
    I = C.din
    lg = K["logits"][:, tile0:tile0 + ntl, :]
    LR = [K["logits"]]
    NF = ntl * NE
    aff = C.tile([128, ntl, NE], F32)
    t2 = C.tile([128, ntl], F32)
    C.reduce(t2[:], lg, ALU.max, LR, [t2])
    C.tt(aff[:], lg, bc_last(t2[:], NE), ALU.subtract, LR + [t2], [aff])
    C.act(aff[:], aff[:], AF.Exp, [aff], [aff])
    C.reduce(t2[:], aff[:], ALU.add, [aff], [t2])
    C.recip(t2[:], t2[:], [t2], [t2])
    C.tt(aff[:], aff[:], bc_last(t2[:], NE), ALU.mult, [aff, t2], [aff])
    lo, hi, mid = C.tile([128, NE], F32), C.tile([128, NE], F32), C.tile([128, NE], F32)
    cnt, ge, dlt = C.tile([128, NE], F32), C.tile([128, NE], F32), C.tile([128, NE], F32)
    cmpb = C.tile([128, ntl, NE], BF16)
    C.memset(lo[:], 0.0, [lo])
    C.memset(hi[:], 1.0, [hi])
    C.memset(mid[:], 0.5, [mid])
    onesb = K["onesb"]
    for it in range(30):
        C.tt(cmpb[:], aff[:], bc_mid(mid[:], ntl), ALU.is_gt, [aff, mid], [cmpb])
        bk = C.bank()
        C.mm(bk[:, 0:NF], onesb[:], cmpb[:].rearrange("p i e -> p (i e)"), True, True, [onesb, cmpb], [bk])
        C.reduce(cnt[:], bk[:, 0:NF].rearrange("p (i e) -> p e i", e=NE), ALU.add, [bk], [cnt])
        C.ts(ge[:], cnt[:], float(cap), None, ALU.is_ge, None, [cnt], [ge])
        C.tt(dlt[:], mid[:], lo[:], ALU.subtract, [mid, lo], [dlt])
        C.tt(dlt[:], dlt[:], ge[:], ALU.mult, [dlt, ge], [dlt])
        C.tt(lo[:], lo[:], dlt[:], ALU.add, [lo, dlt], [lo])
        C.tt(dlt[:], hi[:], mid[:], ALU.subtract, [hi, mid], [dlt])
        C.tt(dlt[:], dlt[:], ge[:], ALU.mult, [dlt, ge], [dlt])
        C.tt(hi[:], mid[:], dlt[:], ALU.add, [mid, dlt], [hi])
        C.tt(mid[:], lo[:], hi[:], ALU.add, [lo, hi], [mid])
        C.ts(mid[:], mid[:], 0.5, None, ALU.mult, None, [mid], [mid])
    Ae = C.tile([128, NE, ntl], F32)
    inc = C.tile([128, NE, ntl], F32)
    rmask = C.tile([128, NE, ntl], F32)
    Aeb = C.tile([128, NE, ntl], BF16)
    excb = C.tile([128, NE, ntl], BF16)
    posm = C.tile([128, NE, ntl], F32)
    C.tt(Ae[:], aff[:].rearrange("p i e -> p e i"), bc_last(lo[:], ntl), ALU.is_gt, [aff, lo], [Ae])
    C.memset(rmask[:], 1.0, [rmask])
    C.memset(rmask[:, :, 0:1], 0.0, [rmask])
    flat = lambda t: t[:].rearrange("p e i -> p (e i)")
    C.p.op("dve", lambda e: e.tensor_tensor_scan(out=flat(inc), data0=flat(rmask), data1=flat(Ae), initial=0.0,
                                                 op0=ALU.mult, op1=ALU.add), [rmask, Ae], [inc])
    C.tt(inc[:], inc[:], Ae[:], ALU.subtract, [inc, Ae], [inc])
    C.copy(Aeb[:], Ae[:], [Ae], [Aeb])
    C.copy(excb[:], inc[:], [inc], [excb])
    bk = C.bank()
    C.mm(bk[:, 0:NF], K["ustrict"][:], flat(Aeb), True, False, [K["ustrict"], Aeb], [bk])
    C.mm(bk[:, 0:NF], onesb[:], flat(excb), False, True, [onesb, excb], [bk])
    C.stt(flat(posm), bk[:, 0:NF], 1.0, flat(Ae), ALU.add, ALU.mult, [bk, Ae], [posm])
    C.ts(posm[:], posm[:], -1.0, None, ALU.add, None, [posm], [posm])
    vals = C.tile([128, ntl, NE, 4], BF16)
    tv = C.tile([128, NT_ALL, 2], BF16)
    C.dma(tv[:], I["tvals"][:], (), [tv])
    affb = C.tile([128, ntl, NE], BF16)
    afr = C.tile([128, ntl, NE], F32)
    C.copy(affb[:], aff[:], [aff], [affb])
    C.tt(afr[:], aff[:], affb[:], ALU.subtract, [aff, affb], [afr])
    for j in range(2):
        C.copy(vals[:, :, :, j], bc_last(tv[:, tile0:tile0 + ntl, j], NE), [tv], [vals])
    C.copy(vals[:, :, :, 2], affb[:], [affb], [vals])
    C.copy(vals[:, :, :, 3], afr[:], [afr], [vals])
    iota = C.tile([128, 512], F32)
    C.dma(iota[:], I["iota"][:], (), [iota])
    rows = min(cap, 128)
    njt = cap // rows
    Pm = [C.tile([128, cap], BF16) for _ in range(4)]
    Rsb = [C.tile([4, cap], F32) for _ in range(2)]
    tq = C.tile([128, 4], F32)
    idf = C.tile([128, 1], F32)
    k = 0
    for e in range(NE):
        rb = C.bank()
        for i in range(ntl):
            Pt = Pm[k % 4]
            k += 1
            C.ts(Pt[:], iota[:, 0:cap], posm[:, e, i:i + 1], None, ALU.is_equal, None, [iota, posm], [Pt])
            C.mm(rb[0:4, 0:cap], vals[:, i, e, :], Pt[:], i == 0, i == ntl - 1, [vals, Pt], [rb])
        R = Rsb[e % 2]
        C.copy(R[:], rb[0:4, 0:cap], [rb], [R], eng="act")
        for jt in range(njt):
            tb = C.bank()
            C.tr(tb[0:rows, 0:4], R[:, jt * rows:(jt + 1) * rows], K["identf"][0:4, 0:4], [R, K["identf"]], [tb])
            C.copy(tq[0:rows, :], tb[0:rows, 0:4], [tb], [tq])
            C.stt(idf[0:rows, :], tq[0:rows, 0:1], 64.0, tq[0:rows, 1:2], ALU.mult, ALU.add, [tq], [idf])
            C.copy(rt["idx"][0:rows, e, jt:jt + 1], idf[0:rows, :], [idf], [rt["idx"]])
            C.tt(rt["gate"][0:rows, e, jt:jt + 1], tq[0:rows, 2:3], tq[0:rows, 3:4], ALU.add, [tq], [rt["gate"]])


def phase_experts(C, K, l, groups):
    C.new_phase()
    I, S = C.din, C.dscr
    ttiles, segs = [], []
    col = 0
    for r, cap, rt in groups:
        g2b = C.tile([128, D], F32)
        load_mod(C, g2b, l, r, 5)
        rows = min(cap, 128)
        for jt in range(cap // rows):
            ttiles.append((rt, jt, rows, col + jt * rows, g2b))
        segs.append((col, cap))
        col += cap
    NTOK = col
    ntt = len(ttiles)
    NBA, NBY = 3, 2
    W1 = [C.tile([128, 8, 512], BF16) for _ in range(NBA)]
    W3 = [C.tile([128, 8, 512], BF16) for _ in range(NBA)]
    W2 = [C.tile([128, 16, 512], BF16) for _ in range(NBY)]
    xe = [C.tile([128, D], BF16) for _ in range(2)]
    xeT = [C.tile([128, 8, NTOK], BF16) for _ in range(2)]
    gT = [C.tile([128, 16, NTOK], BF16) for _ in range(2)]
    sa = [C.tile([128, NTOK], BF16) for _ in range(2)]
    yo = [C.tile([128, ntt, D], F32) for _ in range(2)]
    pieces = []
    for e in range(NE):
        pieces += [(e, "a", fq) for fq in range(4)] + [(e, "y", dh) for dh in range(2)]
    cnt = {"a": 0, "y": 0}
    slot = {}

    def load_piece(pi):
        e, kind, j = pieces[pi]
        if kind == "a":
            s_ = cnt["a"] % NBA
            cnt["a"] += 1
            slot[pi] = s_
            C.dma(W1[s_][:], I["expert_w1"][l, e, :, j * 512:(j + 1) * 512].rearrange("(c p) n -> p c n", p=128), (), [W1[s_]], eng="pool")
            C.dma(W3[s_][:], I["expert_w3"][l, e, :, j * 512:(j + 1) * 512].rearrange("(c p) n -> p c n", p=128), (), [W3[s_]], eng="pool")
        else:
            s_ = cnt["y"] % NBY
            cnt["y"] += 1
            slot[pi] = s_
            C.dma(W2[s_][:], I["expert_w2"][l, e, :, j * 512:(j + 1) * 512].rearrange("(c p) n -> p c n", p=128), (), [W2[s_]], eng="pool")

    def gather(e):
        xT = xeT[e % 2]
        for ti, (rt, jt, rows, col0, _) in enumerate(ttiles):
            xg = xe[ti % 2]
            C.p.op("pool", lambda en, xg=xg, jt=jt, rt=rt, rows=rows: en.indirect_dma_start(
                out=xg[0:rows, :], out_offset=None, in_=S["hA"][:, :],
                in_offset=bass.IndirectOffsetOnAxis(ap=rt["idx"][0:rows, e, jt:jt + 1], axis=0)),
                [rt["idx"], S["hA"]], [xg], dma=True)
            transpose_1024(C, K, xg, xT, npart=rows, col0=col0)

    load_piece(0)
    load_piece(1)
    gather(0)
    for pi in range(len(pieces)):
        if pi + 2 < len(pieces):
            load_piece(pi + 2)
        e, kind, j = pieces[pi]
        s_ = slot[pi]
        xT, g_, y_ = xeT[e % 2], gT[e % 2], yo[e % 2]
        if kind == "a":
            for ft in range(4):
                f = j * 4 + ft
                sb_ = sa[f % 2]
                for c0, nc_ in segs:
                    ab, ub = C.bank(), C.bank()
                    for c in range(8):
                        C.mm(ab[:, 0:nc_], W1[s_][:, c, ft * 128:(ft + 1) * 128], xT[:, c, c0:c0 + nc_], c == 0, c == 7, [W1[s_], xT], [ab])
                    for c in range(8):
                        C.mm(ub[:, 0:nc_], W3[s_][:, c, ft * 128:(ft + 1) * 128], xT[:, c, c0:c0 + nc_], c == 0, c == 7, [W3[s_], xT], [ub])
                    C.act(sb_[:, c0:c0 + nc_], ab[:, 0:nc_], AF.Silu, [ab], [sb_])
                    C.tt(g_[:, f, c0:c0 + nc_], sb_[:, c0:c0 + nc_], ub[:, 0:nc_], ALU.mult, [sb_, ub], [g_])
        else:
            if j == 0 and e + 1 < NE:
                gather(e + 1)
            for ti, (rt, jt, rows, col0, g2b) in enumerate(ttiles):
                yb = C.bank()
                for fc in range(16):
                    C.mm(yb[0:rows, :], g_[:, fc, col0:col0 + rows], W2[s_][:, fc, :], fc == 0, fc == 15, [g_, W2[s_]], [yb])
                C.stt(y_[0:rows, ti, j * 512:(j + 1) * 512], yb[0:rows, :], rt["gate"][0:rows, e, jt:jt + 1], g2b[0:rows, j * 512:(j + 1) * 512],
                      ALU.mult, ALU.mult, [yb, rt["gate"], g2b], [y_])
            if j == 1:
                for ti, (rt, jt, rows, col0, g2b) in enumerate(ttiles):
                    C.p.op("pool", lambda en, y_=y_, jt=jt, e=e, rt=rt, rows=rows, ti=ti: en.indirect_dma_start(
                        out=S["xres"][:, :], out_offset=bass.IndirectOffsetOnAxis(ap=rt["idx"][0:rows, e, jt:jt + 1], axis=0),
                        in_=y_[0:rows, ti, :], in_offset=None, compute_op=ALU.add),
                        [rt["idx"], y_], [S["xres"]], dma=True)


def alloc_route(C, cap):
    rows = min(cap, 128)
    njt = cap // rows
    return dict(idx=C.tile([128, NE, njt], I32, persist=True), gate=C.tile([128, NE, njt], F32, persist=True))


import os
ATT_LEVEL = int(os.environ.get('ATT_LEVEL', '9'))
ATT_TILES = int(os.environ.get('ATT_TILES', str(NT_ALL)))


def phase_attention(C, K):
    C.new_phase()
    I, S = C.din, C.dscr
    alloc_norm_scratch(C, K)
    wqkv = C.tile([128, 8, 1536], BF16)
    load_w_bf16(C, wqkv, I["attn_w_qkv"][:], 1536, 8)
    w_out = C.tile([128, 8, D], BF16)
    load_w_bf16(C, w_out, I["attn_w_out"][:], D, 8)
    W = dict(wr=load_router_w(C, 1))
    idb = K["identb"]
    kT = C.tile([64, 4, NALL], BF16)
    vaug = C.tile([128, NT_ALL, 4, 80], BF16)
    C.memset(vaug[:], 1.0, [vaug])
    masks = C.tile([128, 2, 128], BF16)
    C.dma(masks[:], I["amask"][:], (), [masks])
    esink = C.tile([128, 16], F32)
    C.dma(esink[:], I["attn_sink"][:].partition_broadcast(128), (), [esink])
    C.act(esink[:], esink[:], AF.Exp, [esink], [esink])
    gs1, sh1 = C.tile([128, D], F32), C.tile([128, D], F32)
    load_mod(C, gs1, 1, 1, 1)
    load_mod(C, sh1, 1, 1, 0)
    g1b, gs2, sh2 = C.tile([128, D], F32), C.tile([128, D], F32), C.tile([128, D], F32)
    load_mod(C, g1b, 1, 0, 2)
    load_mod(C, gs2, 1, 0, 4)
    load_mod(C, sh2, 1, 0, 3)
    xt = [C.tile([128, D], F32) for _ in range(2)]
    xt2 = [C.tile([128, D], F32) for _ in range(1)]
    rc = [C.tile([128, 512], F32) for _ in range(2)]
    rs = [C.tile([128, 512], F32) for _ in range(2)]
    h_bf = C.tile([128, D], BF16)
    hT = C.tile([128, 8, 128], BF16)
    zs = C.tile([128, 1280], F32)
    t1 = C.tile([128, 20, 64], F32)
    t2 = C.tile([128, 20, 64], F32)
    qk = C.tile([128, 20, 64], BF16)
    qT = [C.tile([64, 16, 128], BF16) for _ in range(3)]
    pT = [C.tile([128, 512], BF16) for _ in range(5)]
    oat2 = [C.tile([128, 16, 64], BF16) for _ in range(2)]
    den = C.tile([128, 4], F32)
    oT = C.tile([128, 8, 128], BF16)
    h2 = C.tile([128, D], BF16)
    h2T = oT
    K2 = dict(K)
    K2["ntmp"] = C.tile([128, D], F32)
    K2["nss"] = C.tile([128, 1], F32)

    def load(i):
        b = i % 2
        C.dma(xt[b][:], S["xres"][i * 128:(i + 1) * 128, :], [S["xres"].sub(i)], [xt[b]])
        if i >= 2:
            C.dma(rc[b][:], I["rope_c"][(i - 2) * 128:(i - 1) * 128, :], (), [rc[b]])
            C.dma(rs[b][:], I["rope_s"][(i - 2) * 128:(i - 1) * 128, :], (), [rs[b]])

    def qkv(i):
        b = i % 2
        if i == 2:
            load_mod(C, gs1, 1, 0, 1)
            load_mod(C, sh1, 1, 0, 0)
        norm_mod_transpose(C, K, xt[b], gs1, sh1, h_bf, hT)
        yield
        zb = []
        for n in range(3):
            if i < 2 and n < 2:
                zb.append(None)
                continue
            bk = C.bank()
            for c in range(8):
                C.mm(bk[:], hT[:, c, :], wqkv[:, c, n * 512:(n + 1) * 512], c == 0, c == 7, [hT, wqkv], [bk])
            zb.append(bk)
        kvb = zb[2]
        if ATT_LEVEL < 1:
            return
        C.copy(vaug[:, i, :, 0:64], kvb[:, 256:512].rearrange("p (k d) -> p k d", k=4), [kvb], [vaug.sub(i)], eng="act")
        if ATT_LEVEL < 2:
            return
        if i < 2:
            C.copy(qk[:, 16:20, :], kvb[:, 0:256].rearrange("p (k d) -> p k d", k=4), [kvb], [qk], eng="act")
        else:
            srcs = [(zb[0], 512, 0), (zb[1], 512, 8), (kvb, 256, 16)]
            for bkk, w, h0 in srcs:
                nh = w // 64
                zsv = zs[:, h0 * 64:h0 * 64 + w]
                C.copy(zsv, bkk[:, 0:w], [bkk], [zs], eng="act")
                t1v = t1[:, h0:h0 + nh, :].rearrange("p h d -> p (h d)")
                C.tt(t1v, zsv, rc[b][:, 0:w], ALU.mult, [zs, rc[b]], [t1])
                s5 = zsv.rearrange("p (h u a w) -> p h u a w", u=2, a=2, w=16)
                d5 = t2[:, h0:h0 + nh, :].rearrange("p h (u a w) -> p h u a w", u=2, a=2)
                sn = rs[b][:, 0:w].rearrange("p (h u a w) -> p h u a w", u=2, a=2, w=16)
                for a in range(2):
                    C.tt(d5[:, :, :, a, :], s5[:, :, :, 1 - a, :], sn[:, :, :, a, :], ALU.mult, [zs, rs[b]], [t2])
            C.tt(qk[:], t1[:], t2[:], ALU.add, [t1, t2], [qk])
        yield
        if ATT_LEVEL < 3:
            return
        tb = C.bank()
        pb = tb[:].bitcast(BF16)
        for kv in range(4):
            C.tr(pb[0:64, kv * 128:(kv + 1) * 128], qk[:, 16 + kv, :], idb[:], [qk, idb], [tb])
        C.copy(kT[:, :, i * 128:(i + 1) * 128], pb[0:64, 0:512].rearrange("p (k t) -> p k t", k=4), [tb], [kT.sub(i)], eng="act")
        if i >= 2:
            q_ = qT[i % 3]
            for half in range(2):
                tb = C.bank()
                pb = tb[:].bitcast(BF16)
                for hh in range(8):
                    C.tr(pb[0:64, hh * 128:(hh + 1) * 128], qk[:, half * 8 + hh, :], idb[:], [qk, idb], [tb])
                C.copy(q_[:, half * 8:(half + 1) * 8, :], pb[0:64, :].rearrange("p (h t) -> p h t", h=8), [tb], [q_], eng="act")

    def attend(i):
        if ATT_LEVEL < 4:
            return
        b = i % 2
        q_ = qT[i % 3]
        oat = oat2[i % 2]
        oat_flat = oat.view(oat[:].rearrange("p h d -> p (h d)"))
        C.dma(xt2[0][:], S["xres"][i * 128:(i + 1) * 128, :], [S["xres"].sub(i)], [xt2[0]])
        kbl = [(0, None), (1, None)]
        if i - 1 >= 2:
            kbl.append((i - 1, 0))
        kbl.append((i, None))
        if i + 1 < NT_ALL:
            kbl.append((i + 1, 1))
        for kv in range(4):
            pts = []
            for j, (kb, mk) in enumerate(kbl):
                sb_ = C.bank()
                C.mm(sb_[:], kT[:, kv, kb * 128:(kb + 1) * 128], q_[:, 4 * kv:4 * kv + 4, :].rearrange("p h t -> p (h t)"), True, True,
                     [kT.sub(kb), q_], [sb_])
                pt = pT[j]
                C.act(pt[:], sb_[:], AF.Exp, [sb_], [pt], scale=0.125)
                if mk is not None:
                    C.tt(pt[:].rearrange("p (h t) -> p h t", h=4), pt[:].rearrange("p (h t) -> p h t", h=4),
                         bc_mid(masks[:, mk, :], 4), ALU.mult, [pt, masks], [pt])
                pts.append(pt)
            yield
            if ATT_LEVEL < 5:
                continue
            ob_ = C.bank()
            for g in range(4):
                for j, (kb, mk) in enumerate(kbl):
                    C.mm(ob_[:, g * 128:g * 128 + 65], pts[j][:, g * 128:(g + 1) * 128], vaug[:, kb, kv, 0:65], j == 0, j == len(kbl) - 1,
                         [pts[j], vaug.sub(kb)], [ob_])
            ov = ob_[:, :].rearrange("p (g d) -> p g d", g=4)
            C.tt(den[:], ov[:, :, 64], esink[:, 4 * kv:4 * kv + 4], ALU.add, [ob_, esink], [den])
            C.recip(den[:], den[:], [den], [den])
            C.tt(oat[:, 4 * kv:4 * kv + 4, :], ov[:, :, 0:64], bc_last(den[:], 64), ALU.mult, [ob_, den], [oat])
            yield
        if ATT_LEVEL < 6:
            return
        transpose_1024(C, K, oat_flat, oT)
        yield
        yb = []
        for n in range(2):
            bk = C.bank()
            for c in range(8):
                C.mm(bk[:], oT[:, c, :], w_out[:, c, n * 512:(n + 1) * 512], c == 0, c == 7, [oT, w_out], [bk])
            yb.append(bk)
        residual_norm2_router(C, K2, W, i, 1, 0, yb, xt2[0], (g1b, gs2, sh2), (None, h2, h2T))

    load(0)
    for i in range(ATT_TILES):
        if i + 1 < ATT_TILES:
            load(i + 1)
        interleave(qkv(i), attend(i - 2) if i - 2 >= 2 else None)
    if ATT_TILES == NT_ALL:
        interleave(attend(NT_ALL - 2))
        interleave(attend(NT_ALL - 1))


def phase_final(C, K):
    C.new_phase()
    I, S = C.din, C.dscr
    alloc_norm_scratch(C, K)
    gb = C.tile([128, D], F32)
    C.dma(gb[:], I["final_g"][:].partition_broadcast(128), (), [gb])
    xt = [C.tile([128, D], F32) for _ in range(2)]
    yt = [C.tile([128, D], F32) for _ in range(2)]
    out = C.dout

    def load(i):
        C.dma(xt[i % 2][:], S["xres"][(i + 2) * 128:(i + 3) * 128, :], [S["xres"]], [xt[i % 2]])

    load(0)
    for i in range(NLAT // 128):
        if i + 1 < NLAT // 128:
            load(i + 1)
        b = i % 2
        rms_rstd(C, xt[b][:], 128, D, K["ntmp"], K["nss"], [xt[b]])
        C.stt(yt[b][:], xt[b][:], K["nss"][:], gb[:], ALU.mult, ALU.mult, [xt[b], K["nss"], gb], [yt[b]])
        C.dma(out[i * 128:(i + 1) * 128, :], yt[b][:], [yt[b]], [C.dout_buf], eng="pool")


INPUT_SPECS = [
    ("x", [NLAT, D], F32), ("ctx", [NCTX, D], F32), ("c2", [2, D], F32),
    ("mod_w", [2, D, 6 * D], F32), ("mod_b", [2, 6 * D], F32), ("norm1_g", [2, D], F32), ("norm2_g", [2, D], F32),
    ("ab_w_in", [D, 3584], F32), ("ab_w_out", [D, D], F32), ("sgu_w", [4, 128, 128], F32), ("sgu_b", [4, 128], F32),
    ("hgrn_lb_logits", [2, 2, 512], F32), ("hgrn_norm_g", [4, 128], F32),
    ("attn_w_qkv", [D, 1536], F32), ("attn_w_out", [D, D], F32), ("attn_sink", [16], F32),
    ("router_w", [2, D, NE], F32), ("expert_w1", [2, NE, D, FF], F32), ("expert_w3", [2, NE, D, FF], F32),
    ("expert_w2", [2, NE, FF, D], F32), ("final_g", [D], F32),
    ("identb", [128, 128], BF16), ("identf", [128, 128], F32), ("onesb", [128, 128], BF16), ("ustrict", [128, 128], BF16),
    ("gla_m", [64, 2, 64], F32), ("gla_sel", [64, 2, 2], F32), ("gla_tri", [64, 2, 64], F32),
    ("amask", [128, 2, 128], BF16), ("rope_c", [NLAT, 512], F32), ("rope_s", [NLAT, 512], F32),
    ("tvals", [128, NT_ALL, 2], BF16), ("iota", [128, 512], F32),
]


def host_constants():
    bf = ml_dtypes.bfloat16
    c = {}
    c["identb"] = np.eye(128, dtype=np.float32).astype(bf)
    c["identf"] = np.eye(128, dtype=np.float32)
    c["onesb"] = np.ones((128, 128), np.float32).astype(bf)
    pp = np.arange(128)
    c["ustrict"] = (pp[:, None] < pp[None, :]).astype(np.float32).astype(bf)
    s = np.arange(64)[:, None]
    t = np.arange(64)[None, :]
    m = np.zeros((64, 2, 64), np.float32)
    m[:, 0, :] = (s <= t).astype(np.float32) - (s <= 31).astype(np.float32)
    m[:, 1, :] = (s >= t).astype(np.float32) - (s >= 32).astype(np.float32)
    c["gla_m"] = m
    sel = np.zeros((64, 2, 2), np.float32)
    sel[:, 0, 0] = (np.arange(64) <= 31)
    sel[:, 1, 0] = (np.arange(64) >= 32)
    sel[:, :, 1] = 1.0
    c["gla_sel"] = sel
    tri = np.zeros((64, 2, 64), np.float32)
    tri[:, 0, :] = (s <= t)
    tri[:, 1, :] = (s >= t)
    c["gla_tri"] = tri
    j = np.arange(128)[:, None]
    i = np.arange(128)[None, :]
    am = np.zeros((128, 2, 128), np.float32)
    am[:, 0, :] = (i <= j)
    am[:, 1, :] = (j <= i)
    c["amask"] = am.astype(bf)
    tt = np.arange(NLAT)
    inv = (10000.0 ** (-np.arange(16, dtype=np.float32) / 16)).astype(np.float32)
    ar = (tt // 64).astype(np.float32)[:, None] * inv[None, :]
    ac = (tt % 64).astype(np.float32)[:, None] * inv[None, :]
    c["rope_c"] = np.tile(np.concatenate([np.cos(ar), np.cos(ar), np.cos(ac), np.cos(ac)], axis=1).astype(np.float32), (1, 8))
    c["rope_s"] = np.tile(np.concatenate([-np.sin(ar), np.sin(ar), -np.sin(ac), np.sin(ac)], axis=1).astype(np.float32), (1, 8))
    rowid = np.arange(NT_ALL)[None, :] * 128 + np.arange(128)[:, None]
    c["tvals"] = np.stack([rowid // 64, rowid % 64], axis=-1).astype(np.float32).astype(bf)
    c["iota"] = np.broadcast_to(np.arange(512, dtype=np.float32), (128, 512)).copy()
    return c


SCRATCH_SPECS = [
    ("modv", [2, 2, 6 * D], F32), ("xres", [NALL, D], F32), ("hA", [NALL, D], BF16),
    ("ya", [NALL, 512], BF16), ("q", [NALL, 512], BF16), ("kk", [2, NALL, 512], BF16), ("gl", [2, NALL, 512], F32),
    ("v", [NALL, 512], BF16), ("og", [NALL, 512], BF16), ("o", [2, NALL, 512], F32),
]


def build_program(debug=(), stop_after=None, only=None):
    nc = bass.Bass("TRN2", target_bir_lowering=False)
    C = Ctx(nc, debug)
    K = {}
    for name, shape, dt in INPUT_SPECS:
        C.inp(name, shape, dt)
    for name, shape, dt in SCRATCH_SPECS:
        C.scr(name, shape, dt)
    C.dout_buf = Buf(nc.dram_tensor("out", [NLAT, D], F32, kind="ExternalOutput").ap(), "out")
    C.dout = C.dout_buf.t
    load_consts(C, K)
    rt_ctx = alloc_route(C, 32)
    rt_lat = alloc_route(C, 512)
    phases = [
        ("prep", lambda: phase_prep(C, K)),
        ("mixer_in", lambda: phase_mixer_in(C, K)),
        ("gla", lambda: phase_gla(C, K)),
        ("mixer_out", lambda: phase_mixer_out(C, K)),
        ("route_ctx0", lambda: phase_route(C, K, 0, 2, 32, rt_ctx)),
        ("route_lat0", lambda: phase_route(C, K, 2, 32, 512, rt_lat)),
        ("experts_lat0", lambda: phase_experts(C, K, 0, [(0, 512, rt_lat), (1, 32, rt_ctx)])),
        ("attention", lambda: phase_attention(C, K)),
        ("route_lat1", lambda: phase_route(C, K, 2, 32, 512, rt_lat)),
        ("experts_lat1", lambda: phase_experts(C, K, 1, [(0, 512, rt_lat)])),
        ("final", lambda: phase_final(C, K)),
    ]
    for name, fn in phases:
        if only is not None and name not in only:
            continue
        fn()
        if stop_after == name:
            break
    C.p.emit()
    return nc


def make_in_maps(inputs, cores):
    consts = host_constants()
    f = lambda a: np.ascontiguousarray(np.asarray(a, dtype=np.float32))
    shared = {
        "mod_w": f(inputs["mod_w"]), "mod_b": f(inputs["mod_b"]), "norm1_g": f(inputs["norm1_g"]), "norm2_g": f(inputs["norm2_g"]),
        "ab_w_in": f(inputs["ab_w_in"][0]), "ab_w_out": f(inputs["ab_w_out"][0]), "sgu_w": f(inputs["sgu_w"][0]), "sgu_b": f(inputs["sgu_b"][0]),
        "hgrn_lb_logits": f(inputs["hgrn_lb_logits"]), "hgrn_norm_g": f(inputs["hgrn_norm_g"][0]),
        "attn_w_qkv": f(inputs["attn_w_qkv"][0]), "attn_w_out": f(inputs["attn_w_out"][0]), "attn_sink": f(inputs["attn_sink"][0]),
        "router_w": f(inputs["router_w"]), "expert_w1": f(inputs["expert_w1"]), "expert_w3": f(inputs["expert_w3"]),
        "expert_w2": f(inputs["expert_w2"]), "final_g": f(inputs["final_g"]),
    }
    shared.update(consts)
    maps = []
    for b in cores:
        m = dict(shared)
        m["x"] = f(inputs["x"][b])
        m["ctx"] = f(inputs["ctx"][b])
        m["c2"] = np.ascontiguousarray(np.stack([f(inputs["c"][b]), f(inputs["c_ctx"])], axis=0))
        maps.append(m)
    return maps


def kernel(**inputs):
    nc = build_program()
    maps = make_in_maps(inputs, list(range(8)))
    res = run_bass_kernel_spmd(nc, maps, core_ids=list(range(8)))
    return np.stack([np.asarray(r["out"], dtype=np.float32) for r in res.results], axis=0)
```
